# Optimizing a Trainium2 kernel written in Bass

```python
import jax
import jax.numpy as jnp
from jax import lax
import numpy as np

D_MODEL = 2048
BATCH = 2
SEQ = 16384
DEPTH = 1

EPS = 1e-6
HEAD_DIM = 128

GLA_HEADS = 4
GLA_DK = 64
GLA_DV = 128
GLA_GATE_RANK = 16
GLA_TAU = 16.0
GLA_CHUNK = 64

DIL_HEADS = 12
DIL_PATTERNS = ((128, 1), (512, 4), (2048, 16))
ROPE_THETA = 500000.0
ROPE_DIM = HEAD_DIM // 4
MASK_VALUE = -1e30

PEER_HEADS = 8
PEER_NKEYS = 128
PEER_NEXPERTS = PEER_NKEYS * PEER_NKEYS
PEER_QDIM = 256
PEER_TOPK = 16
PEER_TOKEN_BLOCK = 128

GLA_QK_W = GLA_HEADS * GLA_DK
GLA_V_W = GLA_HEADS * GLA_DV
DIL_W = DIL_HEADS * HEAD_DIM
MIX_W = GLA_V_W + DIL_W
IN_SPLITS = (GLA_QK_W, GLA_QK_W, GLA_V_W, GLA_GATE_RANK, GLA_GATE_RANK, GLA_V_W, DIL_W, DIL_W, DIL_W)
IN_W = sum(IN_SPLITS)

kernel_name = 'hybrid_gla_dilated_peer_encoder'


def rms_norm(x, g):
    xf = x.astype(jnp.float32)
    y = xf * lax.rsqrt(jnp.mean(xf * xf, axis=-1, keepdims=True) + EPS)
    return (y * g.astype(jnp.float32)).astype(x.dtype)


def partial_rope(t, positions):
    half = ROPE_DIM // 2
    inv_freq = ROPE_THETA ** (-jnp.arange(half, dtype=jnp.float32) / half)
    ang = positions.astype(jnp.float32)[:, None] * inv_freq[None, :]
    cos = jnp.cos(ang)[None, :, None, :]
    sin = jnp.sin(ang)[None, :, None, :]
    tf = t.astype(jnp.float32)
    x1 = tf[..., :half]
    x2 = tf[..., half:ROPE_DIM]
    return jnp.concatenate([x1 * cos - x2 * sin, x2 * cos + x1 * sin, tf[..., ROPE_DIM:]], axis=-1)


def gla_direction(q, k, v, log_a, include_diag):
    Bsz, H, S, dk = q.shape
    dv = v.shape[-1]
    C = GLA_CHUNK
    N = S // C
    q = q.reshape(Bsz, H, N, C, dk)
    k = k.reshape(Bsz, H, N, C, dk)
    v = v.reshape(Bsz, H, N, C, dv)
    b = jnp.cumsum(log_a.reshape(Bsz, H, N, C, dk), axis=3)
    b_last = b[:, :, :, -1:, :]
    q_in = q * jnp.exp(b)
    k_in = k * jnp.exp(-b)
    scores = jnp.einsum('bhnik,bhnjk->bhnij', q_in, k_in)
    mask = jnp.tril(jnp.ones((C, C), dtype=bool), k=0 if include_diag else -1)
    o_intra = jnp.einsum('bhnij,bhnjv->bhniv', jnp.where(mask, scores, 0.0), v)
    k_state = k * jnp.exp(b_last - b)
    inc = jnp.einsum('bhnjk,bhnjv->bhnkv', k_state, v)
    decay = jnp.exp(b_last[:, :, :, 0, :])

    def step(state, inp):
        dec, add = inp
        return dec[..., None] * state + add, state

    s0 = jnp.zeros((Bsz, H, dk, dv), jnp.float32)
    _, s_before = lax.scan(step, s0, (jnp.moveaxis(decay, 2, 0), jnp.moveaxis(inc, 2, 0)))
    s_before = jnp.moveaxis(s_before, 0, 2)
    o_inter = jnp.einsum('bhnik,bhnkv->bhniv', q_in, s_before)
    return (o_intra + o_inter).reshape(Bsz, H, S, dv)


def gla_mixer(q, k, v, gate_down_f, gate_down_b, r, up_f, bias_f, up_b, bias_b, out_g):
    Bsz, S, _ = q.shape
    f32 = jnp.float32

    def heads(t, d):
        return t.astype(f32).reshape(Bsz, S, GLA_HEADS, d).transpose(0, 2, 1, 3)

    qh = heads(q, GLA_DK) * (GLA_DK ** -0.5)
    kh = heads(k, GLA_DK)
    vh = heads(v, GLA_DV)
    la_f = heads(jax.nn.log_sigmoid(gate_down_f.astype(f32) @ up_f.astype(f32) + bias_f.astype(f32)) / GLA_TAU, GLA_DK)
    la_b = heads(jax.nn.log_sigmoid(gate_down_b.astype(f32) @ up_b.astype(f32) + bias_b.astype(f32)) / GLA_TAU, GLA_DK)

    def rev(t):
        return jnp.flip(t, axis=2)

    o_fwd = gla_direction(qh, kh, vh, la_f, True)
    o_bwd = rev(gla_direction(rev(qh), rev(kh), rev(vh), rev(la_b), False))
    o = (o_fwd + o_bwd).transpose(0, 2, 1, 3)
    o = rms_norm(o, out_g.reshape(GLA_HEADS, GLA_DV))
    return o.reshape(Bsz, S, GLA_V_W) * jax.nn.silu(r.astype(f32))


def dilated_window_attention(q, k, v, window, dilation):
    Bsz, S, H, hd = q.shape
    half = window // (2 * dilation)
    L = S // dilation
    nb = -(-L // half)
    Lp = nb * half

    def to_blocks(t):
        t = t.reshape(Bsz, L, dilation, H, hd)
        t = jnp.pad(t, ((0, 0), (0, Lp - L), (0, 0), (0, 0), (0, 0)))
        return t.reshape(Bsz, nb, half, dilation, H, hd)

    def with_neighbours(t):
        tp = jnp.pad(t, ((0, 0), (1, 1), (0, 0), (0, 0), (0, 0), (0, 0)))
        return jnp.concatenate([tp[:, :-2], tp[:, 1:-1], tp[:, 2:]], axis=2)

    qb = to_blocks(q)
    kn = with_neighbours(to_blocks(k))
    vn = with_neighbours(to_blocks(v))
    s = jnp.einsum('bnqrhe,bnkrhe->bnrhqk', qb, kn)
    qpos = jnp.arange(nb)[:, None] * half + jnp.arange(half)[None, :]
    kpos = jnp.arange(nb)[:, None] * half - half + jnp.arange(3 * half)[None, :]
    rel = kpos[:, None, :] - qpos[:, :, None]
    valid = (jnp.abs(rel) <= half) & (kpos[:, None, :] >= 0) & (kpos[:, None, :] < L)
    s = jnp.where(valid[None, :, None, None], s, MASK_VALUE)
    m = jnp.max(s, axis=-1)
    p = jnp.exp(s - m[..., None])
    den = jnp.sum(p, axis=-1)
    m_t = jnp.moveaxis(m, (2, 3, 4), (3, 4, 2))
    den_t = jnp.moveaxis(den, (2, 3, 4), (3, 4, 2))
    o = jnp.einsum('bnrhqk,bnkrhe->bnqrhe', p, vn) / den_t[..., None]

    def back(t):
        t = t.reshape((Bsz, Lp, dilation, H) + t.shape[5:])[:, :L]
        return t.reshape((Bsz, S, H) + t.shape[4:])

    return back(o), back(m_t), back(den_t)


def dilated_mixer(q, k, v, q_norm_g, k_norm_g, positions):
    Bsz, S, _ = q.shape
    shp = (Bsz, S, DIL_HEADS, HEAD_DIM)
    qh = partial_rope(rms_norm(q.reshape(shp), q_norm_g), positions) * (HEAD_DIM ** -0.5)
    kh = partial_rope(rms_norm(k.reshape(shp), k_norm_g), positions)
    vh = v.reshape(shp).astype(jnp.float32)
    outs, maxes, dens = [], [], []
    for window, dilation in DIL_PATTERNS:
        o, m, d = dilated_window_attention(qh, kh, vh, window, dilation)
        outs.append(o)
        maxes.append(m)
        dens.append(d)
    m_all = jnp.stack(maxes)
    w = jnp.stack(dens) * jnp.exp(m_all - jnp.max(m_all, axis=0, keepdims=True))
    w = w / jnp.sum(w, axis=0, keepdims=True)
    o = jnp.sum(w[..., None] * jnp.stack(outs), axis=0)
    return o.reshape(Bsz, S, DIL_W)


def peer_ffn(xn, w_query, sub_keys, expert_down, expert_up):
    Bsz, S, D = xn.shape
    f32 = jnp.float32
    T = Bsz * S
    xt = xn.reshape(T, D)
    qry = (xt @ w_query).reshape(T, PEER_HEADS, 2, PEER_QDIM // 2)
    sc = jnp.einsum('thcd,hckd->thck', qry.astype(f32), sub_keys.astype(f32))
    top_s, top_i = lax.top_k(sc, PEER_TOPK)
    kk = PEER_TOPK * PEER_TOPK
    cand_s = (top_s[:, :, 0, :, None] + top_s[:, :, 1, None, :]).reshape(T, PEER_HEADS, kk)
    cand_i = (top_i[:, :, 0, :, None] * PEER_NKEYS + top_i[:, :, 1, None, :]).reshape(T, PEER_HEADS, kk)
    best_s, best_pos = lax.top_k(cand_s, PEER_TOPK)
    idx = jnp.take_along_axis(cand_i, best_pos, axis=-1)
    gates = jax.nn.softmax(best_s, axis=-1)
    K = PEER_HEADS * PEER_TOPK
    nblk = T // PEER_TOKEN_BLOCK

    def block(args):
        xb, ib, gb = args
        u = expert_down[ib]
        act = jax.nn.gelu(jnp.einsum('tkd,td->tk', u, xb).astype(f32))
        vv = expert_up[ib]
        return jnp.einsum('tk,tkd->td', (gb * act).astype(vv.dtype), vv)

    out = lax.map(block, (xt.reshape(nblk, PEER_TOKEN_BLOCK, D),
                          idx.reshape(nblk, PEER_TOKEN_BLOCK, K),
                          gates.reshape(nblk, PEER_TOKEN_BLOCK, K)))
    return out.reshape(Bsz, S, D).astype(xn.dtype)


def setup_inputs(seed: int = 0) -> dict:
    key = jax.random.key(seed)
    ks = jax.random.split(key, 16)
    f32 = jnp.float32

    def nrm(k, shape, scale):
        return jax.random.normal(k, shape, f32) * scale

    def gain(k, shape):
        return 1.0 + 0.02 * jax.random.normal(k, shape, f32)

    L = DEPTH
    return {
        'x': nrm(ks[0], (BATCH, SEQ, D_MODEL), 1.0),
        'norm1_g': gain(ks[1], (L, D_MODEL)),
        'w_in': nrm(ks[2], (L, D_MODEL, IN_W), D_MODEL ** -0.5),
        'gla_up_f': nrm(ks[3], (L, GLA_GATE_RANK, GLA_QK_W), GLA_GATE_RANK ** -0.5),
        'gla_bias_f': nrm(ks[4], (L, GLA_QK_W), 0.1),
        'gla_up_b': nrm(ks[5], (L, GLA_GATE_RANK, GLA_QK_W), GLA_GATE_RANK ** -0.5),
        'gla_bias_b': nrm(ks[6], (L, GLA_QK_W), 0.1),
        'gla_out_g': gain(ks[7], (L, GLA_V_W)),
        'q_norm_g': gain(ks[8], (L, HEAD_DIM)),
        'k_norm_g': gain(ks[9], (L, HEAD_DIM)),
        'w_out': nrm(ks[10], (L, MIX_W, D_MODEL), MIX_W ** -0.5),
        'norm2_g': gain(ks[11], (L, D_MODEL)),
        'peer_w_query': nrm(ks[12], (L, D_MODEL, PEER_HEADS * PEER_QDIM), D_MODEL ** -0.5),
        'peer_sub_keys': nrm(ks[13], (L, PEER_HEADS, 2, PEER_NKEYS, PEER_QDIM // 2), (PEER_QDIM // 2) ** -0.5),
        'peer_down': nrm(ks[14], (L, PEER_NEXPERTS, D_MODEL), D_MODEL ** -0.5),
        'peer_up': nrm(ks[15], (L, PEER_NEXPERTS, D_MODEL), PEER_HEADS ** -0.5),
    }


def reference(x, norm1_g, w_in, gla_up_f, gla_bias_f, gla_up_b, gla_bias_b, gla_out_g, q_norm_g, k_norm_g, w_out, norm2_g, peer_w_query, peer_sub_keys, peer_down, peer_up):
    positions = jnp.arange(x.shape[1], dtype=jnp.int32)
    split_points = [int(c) for c in np.cumsum(IN_SPLITS)[:-1]]
    for l in range(DEPTH):
        xn = rms_norm(x, norm1_g[l])
        h = xn @ w_in[l]
        qa, ka, va, gdf, gdb, ra, qd, kd, vd = jnp.split(h, split_points, axis=-1)
        oa = gla_mixer(qa, ka, va, gdf, gdb, ra, gla_up_f[l], gla_bias_f[l], gla_up_b[l], gla_bias_b[l], gla_out_g[l])
        od = dilated_mixer(qd, kd, vd, q_norm_g[l], k_norm_g[l], positions)
        mixed = jnp.concatenate([oa, od], axis=-1).astype(x.dtype)
        x = x + mixed @ w_out[l]
        x = x + peer_ffn(rms_norm(x, norm2_g[l]), peer_w_query[l], peer_sub_keys[l], peer_down[l], peer_up[l])
    return x
```

```python
import numpy as np
from contextlib import ExitStack
import concourse.bass as bass
import concourse.mybir as mybir
from concourse.bass_utils import run_bass_kernel_spmd

F32 = mybir.dt.float32
BF16 = mybir.dt.bfloat16
U32 = mybir.dt.uint32
AF = mybir.ActivationFunctionType
ALU = mybir.AluOpType
AX = mybir.AxisListType

D_MODEL = 2048
SEQ = 16384
NTOK = 4096
HALO = 1024
NEXT = NTOK + 2 * HALO
NT_EXT = NEXT // 128
NT_OWN = NTOK // 128
HT = HALO // 128
IN_W = 6176
EPS = 1e-6
NEG = -1e30

PHASES = ("w_in", "tables", "stage1", "gla", "dil", "wout", "gmat", "peer")
DEBUG_OUTS = ()


class SemRec:
    def __init__(self, sem):
        self.sem = sem
        self.cnt = 0


class Ev:
    __slots__ = ("rec", "val", "eng")

    def __init__(self, rec, val, eng):
        self.rec = rec
        self.val = val
        self.eng = eng


class Buf:
    def __init__(self, name, t=None):
        self.name = name
        self.t = t
        self.last_w = None
        self.readers = []
        self.dma = None

    def __getitem__(self, k):
        return self.t[k]


class Eng:
    def __init__(self, fw, name, obj, sem):
        self.fw = fw
        self.name = name
        self.obj = obj
        self.rec = SemRec(sem)
        self.waited = {}

    def wait_ev(self, ev):
        rec = ev.rec
        if ev.val is None:
            val = rec.cnt * 16
        else:
            val = ev.val
            if ev.eng is self and (self.name == "pe" or not self.fw.same_engine_sync):
                return
        if val <= 0:
            return
        if self.waited.get(id(rec), 0) >= val:
            return
        self.waited[id(rec)] = val
        self.obj.wait_ge(rec.sem, val)
        self.fw.n_waits += 1


class FW:
    def __init__(self, nc, n_dma_sems=88, same_engine_sync=True):
        self.nc = nc
        self.same_engine_sync = same_engine_sync
        self.n_waits = 0
        self.n_inst = 0
        self.pe = Eng(self, "pe", nc.tensor, nc.alloc_semaphore("sem_pe"))
        self.act = Eng(self, "act", nc.scalar, nc.alloc_semaphore("sem_act"))
        self.dve = Eng(self, "dve", nc.vector, nc.alloc_semaphore("sem_dve"))
        self.pool = Eng(self, "pool", nc.gpsimd, nc.alloc_semaphore("sem_pool"))
        self.sp = Eng(self, "sp", nc.sync, nc.alloc_semaphore("sem_sp"))
        self.engs = [self.pe, self.act, self.dve, self.pool, self.sp]
        self.free_dma = [SemRec(nc.alloc_semaphore(f"sem_d{i}")) for i in range(n_dma_sems)]
        self.all_dma = list(self.free_dma)
        self.phase_bufs = []

    def sbuf(self, es, name, shape, dtype):
        t = es.enter_context(self.nc.sbuf_tensor("sb_" + name, list(shape), dtype))
        b = Buf(name, t)
        self.phase_bufs.append(b)
        return b

    def psum(self, es, name, shape, dtype=F32):
        t = es.enter_context(self.nc.psum_tensor("ps_" + name, list(shape), dtype))
        b = Buf(name, t)
        self.phase_bufs.append(b)
        return b

    def _deps(self, reads, writes):
        deps = []
        for b in reads:
            if b.last_w is not None:
                deps.append(b.last_w)
        for b in writes:
            if b.last_w is not None:
                deps.append(b.last_w)
            deps.extend(b.readers)
        return deps

    def _record(self, ev, reads, writes):
        for b in writes:
            b.last_w = ev
            b.readers = []
        for b in reads:
            if b in writes:
                continue
            b.readers = [r for r in b.readers if r.rec is not ev.rec]
            b.readers.append(ev)

    def I(self, eng, fn, *args, reads=(), writes=(), **kw):
        for ev in self._deps(reads, writes):
            eng.wait_ev(ev)
        inst = fn(*args, **kw)
        eng.rec.cnt += 1
        inst.then_inc(eng.rec.sem, 1)
        self.n_inst += 1
        self._record(Ev(eng.rec, eng.rec.cnt, eng), reads, writes)
        return inst

    def D(self, eng, out, in_, sb, reads=(), writes=(), **kw):
        for ev in self._deps(reads, writes):
            eng.wait_ev(ev)
        if sb.dma is None:
            sb.dma = self.free_dma.pop()
        rec = sb.dma
        inst = eng.obj.dma_start(out=out, in_=in_, **kw)
        inst.then_inc(rec.sem, 16)
        rec.cnt += 1
        self.n_inst += 1
        self._record(Ev(rec, None, eng), reads, writes)
        return inst

    def barrier(self):
        for e in self.engs:
            for o in self.engs:
                if o is e or o.rec.cnt == 0:
                    continue
                e.wait_ev(Ev(o.rec, o.rec.cnt, o))
            for rec in self.all_dma:
                if rec.cnt:
                    e.wait_ev(Ev(rec, None, None))

    def end_phase(self):
        self.barrier()
        for b in self.phase_bufs:
            if b.dma is not None:
                self.free_dma.append(b.dma)
                b.dma = None
        self.phase_bufs = []


class Ring:
    def __init__(self, fw, es, name, n, shape, dtype, psum=False):
        mk = fw.psum if psum else fw.sbuf
        self.bufs = [mk(es, f"{name}{i}", shape, dtype) for i in range(n)]
        self.i = 0

    def next(self):
        b = self.bufs[self.i % len(self.bufs)]
        self.i += 1
        return b


def bc(ap, steps):
    return bass.AP(ap.tensor, ap.offset, [list(ap.ap[0])] + [[s, c] for (s, c) in steps])


C_LINCL, C_USTRICT, C_UINCL, C_LSTRICT, C_IDENT, C_IOTA, C_BM, C_ONES, C_CMASK = [k * 128 for k in range(9)]
C_COLS = 8 * 128 + 17 * 128


def host_consts():
    j = np.arange(128)[:, None]
    i = np.arange(128)[None, :]
    c = np.zeros((128, C_COLS), np.float32)
    c[:, C_LINCL:C_LINCL + 128] = (j <= i)
    c[:, C_USTRICT:C_USTRICT + 128] = (j > i)
    c[:, C_UINCL:C_UINCL + 128] = (j >= i)
    c[:, C_LSTRICT:C_LSTRICT + 128] = (j < i)
    c[:, C_IDENT:C_IDENT + 128] = (j == i)
    c[:, C_IOTA:C_IOTA + 128] = np.broadcast_to(i, (128, 128))
    c[:, C_BM:C_BM + 128] = ((j // 16) == (i // 16))
    c[:, C_ONES:C_ONES + 128] = 1.0
    for t in range(17):
        off = (t - 8) * 128 + j - i
        m = (np.abs(off) <= 64).astype(np.float32)
        m += ((off % 4 == 0) & (np.abs(off) <= 256))
        m += ((off % 16 == 0) & (np.abs(off) <= 1024))
        c[:, C_CMASK + t * 128:C_CMASK + (t + 1) * 128] = m
    return c


def host_aux(p):
    pos = np.arange(NEXT) + p * NTOK - HALO
    half = 16
    inv_freq = (np.float32(500000.0) ** (-np.arange(half, dtype=np.float32) / np.float32(half))).astype(np.float32)
    ang = pos.astype(np.float32)[:, None] * inv_freq[None, :]
    a = np.zeros((NEXT, 33), np.float32)
    a[:, 0:16] = np.cos(ang)
    a[:, 16:32] = np.sin(ang)
    a[:, 32] = ((pos >= 0) & (pos < SEQ))
    return a


def build_program(phases=PHASES, debug_outs=DEBUG_OUTS):
    nc = bass.Bass("TRN2", target_bir_lowering=False)
    fw = FW(nc)
    I, D = fw.I, fw.D
    PE, ACT, DVE, POOL, SP = fw.pe, fw.act, fw.dve, fw.pool, fw.sp
    T = {}

    def din(name, shape, dt=F32):
        T[name] = nc.dram_tensor(name, list(shape), dt, kind="ExternalInput").ap()

    def dscr(name, shape, dt):
        kind = "ExternalOutput" if name in debug_outs else "Internal"
        T[name] = nc.dram_tensor(name, list(shape), dt, kind=kind).ap()

    din("xe", [NEXT, D_MODEL])
    din("cst", [128, C_COLS])
    din("aux", [NEXT, 33])
    din("g1", [128, 16])
    din("g2", [128, 16])
    din("w_in", [D_MODEL, IN_W])
    din("up_f", [16, 256]); din("up_b", [16, 256]); din("bias_f", [1, 256]); din("bias_b", [1, 256])
    din("out_g", [1, 512]); din("gq", [1, 128]); din("gk", [1, 128])
    din("w_out", [D_MODEL, D_MODEL]); din("wq", [D_MODEL, D_MODEL])
    din("subk", [16, 128, 128])
    if "tables" in phases:
        din("down", [128 * 128, D_MODEL]); din("up", [128 * 128, D_MODEL])
    T["out"] = nc.dram_tensor("out", [NTOK, D_MODEL], F32, kind="ExternalOutput").ap()

    dscr("wi_s", [128, 16, IN_W], BF16)
    dscr("qdT_s", [12, 128, NTOK], BF16)
    dscr("kdT_s", [12, 128, NEXT], BF16)
    dscr("vd_s", [NEXT, 12 * 129], BF16)
    dscr("glaT_s", [NT_EXT, 128, 8 * 128], BF16)
    dscr("kst_s", [NEXT, 512], BF16)
    dscr("va_s", [NEXT, 512], BF16)
    dscr("dec_s", [NT_EXT, 128, 4], F32)
    dscr("rs_s", [NTOK, 512], BF16)
    dscr("mixed_s", [NTOK, D_MODEL], BF16)
    dscr("x1_s", [NTOK, D_MODEL], F32)
    dscr("xn2T_s", [128, 16, NTOK], BF16)
    dscr("r3_s", [NTOK, 384], F32)
    dscr("G_s", [NT_OWN, 128, 128 * 128], BF16)
    if "tables" in phases:
        dscr("dT_s", [128, 128, 2048], BF16)
        dscr("up_s", [128, 128, 2048], BF16)

    with ExitStack() as es0:
        cst = fw.sbuf(es0, "cst", [128, C_COLS], F32)
        identb = fw.sbuf(es0, "identb", [128, 128], BF16)
        trib = fw.sbuf(es0, "trib", [128, 256], BF16)
        D(SP, cst[:], T["cst"], cst, writes=[cst])
        I(DVE, nc.vector.tensor_copy, identb[:], cst[:, C_IDENT:C_IDENT + 128], reads=[cst], writes=[identb])
        I(DVE, nc.vector.tensor_copy, trib[:], cst[:, C_LINCL:C_LINCL + 256], reads=[cst], writes=[trib])
        identf = cst
        fw.end_phase()
        fw.phase_bufs = []

        def rstd_from_ss(ss_ap, n, ss_buf, out_buf, out_ap, eng_recip=DVE):
            I(ACT, nc.scalar.activation, out_ap, ss_ap, AF.Sqrt, scale=1.0 / n, bias=eps_col[:, 0:1],
              reads=[ss_buf, eps_col], writes=[out_buf])
            I(eng_recip, nc.vector.reciprocal, out_ap, out_ap, reads=[out_buf], writes=[out_buf])

        eps_col = fw.sbuf(es0, "eps_col", [128, 1], F32)
        I(DVE, nc.vector.memset, eps_col[:], EPS, writes=[eps_col])

        if "w_in" in phases:
            with ExitStack() as es:
                st = Ring(fw, es, "wst", 2, [128, IN_W], F32)
                bf = Ring(fw, es, "wbf", 2, [128, IN_W], BF16)
                for c in range(16):
                    s = st.next(); b = bf.next()
                    D(SP, s[:], T["w_in"][c * 128:(c + 1) * 128, :], s, writes=[s])
                    if c % 2 == 0:
                        I(ACT, nc.scalar.copy, b[:], s[:], reads=[s], writes=[b])
                    else:
                        I(DVE, nc.vector.tensor_copy, b[:], s[:], reads=[s], writes=[b])
                    D(SP, T["wi_s"][:, c, :], b[:], b, reads=[b])
                fw.end_phase()

        def tables_gen(es):
            dn_r = Ring(fw, es, "dn", 3, [128, 2048], F32)
            up_r = Ring(fw, es, "upf", 3, [128, 2048], F32)
            dnb_r = Ring(fw, es, "dnb", 3, [128, 2048], BF16)
            upb_r = Ring(fw, es, "upb", 2, [128, 2048], BF16)
            dT_r = Ring(fw, es, "dTt", 2, [128, 2048], BF16)
            pT_r = Ring(fw, es, "pTt", 1, [128, 2048], BF16, psum=True)
            down_v = T["down"].rearrange("(i j) d -> j i d", j=128)
            up_v = T["up"].rearrange("(i j) d -> j i d", j=128)
            loaded = {}

            def tload(j):
                dn = dn_r.next(); upf = up_r.next()
                D(SP, dn[:], down_v[j], dn, writes=[dn])
                D(SP, upf[:], up_v[j], upf, writes=[upf])
                loaded[j] = (dn, upf)

            tload(0); tload(1)
            prev = None
            for j in range(129):
                cur = None
                if j < 128:
                    if j + 2 < 128:
                        tload(j + 2)
                    dn, upf = loaded.pop(j)
                    dnb = dnb_r.next(); upb = upb_r.next()
                    I(POOL, nc.gpsimd.tensor_copy, dnb[:], dn[:], reads=[dn], writes=[dnb])
                    I(ACT, nc.scalar.copy, upb[:], upf[:], reads=[upf], writes=[upb])
                    D(SP, T["up_s"][j], upb[:], upb, reads=[upb])
                    cur = (j, dnb)
                if prev is not None:
                    pj, pdnb = prev
                    pT = pT_r.next()
                    for c in range(16):
                        I(PE, nc.tensor.transpose, pT[:, c * 128:(c + 1) * 128], pdnb[:, c * 128:(c + 1) * 128], identb[:],
                          reads=[pdnb, identb], writes=[pT])
                    dT = dT_r.next()
                    I(ACT, nc.scalar.copy, dT[:], pT[:], reads=[pT], writes=[dT])
                    D(SP, T["dT_s"][pj], dT[:], dT, reads=[dT])
                prev = cur
                yield

        if "tables" in phases and "dil" not in phases:
            with ExitStack() as es:
                for _ in tables_gen(es):
                    pass
                fw.end_phase()

        if "stage1" in phases:
            with ExitStack() as es:
                g1col = fw.sbuf(es, "g1col", [128, 16], F32)
                gq_b = fw.sbuf(es, "gq_b", [128, 128], F32)
                gk_b = fw.sbuf(es, "gk_b", [128, 128], F32)
                upb = fw.sbuf(es, "upbx", [33, 512], F32)
                D(SP, g1col[:], T["g1"], g1col, writes=[g1col])
                D(SP, gq_b[:], T["gq"].partition_broadcast(128), gq_b, writes=[gq_b])
                D(SP, gk_b[:], T["gk"].partition_broadcast(128), gk_b, writes=[gk_b])
                I(ACT, nc.scalar.mul, gq_b[:], gq_b[:], 128.0 ** -0.5, reads=[gq_b], writes=[gq_b])
                I(DVE, nc.vector.memset, upb[:], 0.0, writes=[upb])
                D(SP, upb[0:16, 0:256], T["up_f"], upb, writes=[upb])
                D(SP, upb[16:32, 256:512], T["up_b"], upb, writes=[upb])
                D(SP, upb[32:33, 0:256], T["bias_f"], upb, writes=[upb])
                D(SP, upb[32:33, 256:512], T["bias_b"], upb, writes=[upb])

                xr = Ring(fw, es, "x", 2, [128, 2048], F32)
                junk = fw.sbuf(es, "junk", [128, 2048], BF16)
                xbr = Ring(fw, es, "xb", 2, [128, 2048], BF16)
                ssr = Ring(fw, es, "ss", 4, [128, 1], F32)
                xnTr = Ring(fw, es, "xnT", 2, [128, 16, 512], BF16)
                slabr = Ring(fw, es, "slab", 2, [128, 16, 512], BF16)
                gTr = Ring(fw, es, "gT", 2, [33, 512], F32)
                for b in gTr.bufs:
                    I(DVE, nc.vector.memset, b[32:33, :], 1.0, writes=[b])
                auxr = Ring(fw, es, "aux", 8, [128, 33], F32)
                tabr = Ring(fw, es, "tab", 8, [128, 2, 2, 32], F32)
                Er = Ring(fw, es, "E", 4, [128, 3, 512], F32)
                vdr = Ring(fw, es, "vd", 3, [128, 4, 129], BF16)
                sqr = Ring(fw, es, "sq", 2, [128, 512], F32)
                ss4r = Ring(fw, es, "ss4", 2, [128, 4], F32)
                qnr = Ring(fw, es, "qn", 2, [128, 4, 128], F32)
                qsmr = Ring(fw, es, "qsm", 2, [128, 4, 32], F32)
                rtr = Ring(fw, es, "rt", 2, [128, 4, 4, 16], F32)
                qrr = Ring(fw, es, "qr", 3, [128, 4, 128], BF16)
                TTr = Ring(fw, es, "TT", 2, [128, 4, 128], BF16)
                e1r = Ring(fw, es, "e1", 2, [128, 512], F32)
                spr = Ring(fw, es, "sp", 2, [128, 512], F32)
                decr = Ring(fw, es, "dec", 2, [128, 4], F32)
                QKr = Ring(fw, es, "QK", 3, [128, 4, 256], BF16)
                kstr = Ring(fw, es, "kst", 2, [128, 2, 256], BF16)
                var = Ring(fw, es, "va", 2, [128, 512], BF16)
                rsr = Ring(fw, es, "rs", 2, [128, 512], BF16)
                GTr = Ring(fw, es, "GT", 2, [128, 8, 128], BF16)
                pTx = Ring(fw, es, "pTx", 1, [128, 2048], BF16, psum=True)
                main = Ring(fw, es, "mp", 4, [128, 512], F32, psum=True)
                pTs = Ring(fw, es, "pTs", 2, [128, 1024], BF16, psum=True)

                def load_slab(c0, n):
                    s = slabr.next()
                    D(SP, s[:, :, 0:n], T["wi_s"][:, :, c0:c0 + n], s, writes=[s])
                    return s

                pending = []

                def defer(fn):
                    pending.append(fn)

                def flush(keep=0):
                    while len(pending) > keep:
                        pending.pop(0)()

                def proj(slab, xnT, k, n=512):
                    ps = main.next()
                    for c in range(16):
                        I(PE, nc.tensor.matmul, ps[:, 0:n], xnT[:, c, k * 128:(k + 1) * 128], slab[:, c, 0:n],
                          start=(c == 0), stop=(c == 15), reads=[xnT, slab], writes=[ps])
                    flush(keep=1)
                    return ps

                for g in range(NT_EXT // 4):
                    tiles = [4 * g + k for k in range(4)]
                    own = (HT <= tiles[0] < HT + NT_OWN)
                    xnT = xnTr.next()
                    auxs = []
                    tabs = []
                    for k, te in enumerate(tiles):
                        x = xr.next(); aux = auxr.next(); auxs.append(aux)
                        D(SP, x[:], T["xe"][te * 128:(te + 1) * 128, :], x, writes=[x])
                        D(POOL, aux[:], T["aux"][te * 128:(te + 1) * 128, :], aux, writes=[aux])
                        tb = tabr.next(); tabs.append(tb)
                        for wi_, g_b_ in enumerate((gq_b, gk_b)):
                            I(POOL, nc.gpsimd.tensor_tensor, tb[:, wi_, 0, :].rearrange("p (a b) -> p a b", a=2),
                              bc(aux[:, 0:16], [(0, 2), (1, 16)]), g_b_[:, 0:32].rearrange("p (a b) -> p a b", a=2), ALU.mult,
                              reads=[aux, g_b_], writes=[tb])
                            I(DVE, nc.vector.scalar_tensor_tensor, tb[:, wi_, 1, 0:16], aux[:, 16:32], -1.0, g_b_[:, 16:32],
                              ALU.mult, ALU.mult, reads=[aux, g_b_], writes=[tb])
                            I(POOL, nc.gpsimd.tensor_tensor, tb[:, wi_, 1, 16:32], aux[:, 16:32], g_b_[:, 0:16], ALU.mult,
                              reads=[aux, g_b_], writes=[tb])
                        ss = ssr.next()
                        I(ACT, nc.scalar.activation, junk[:], x[:], AF.Square, accum_out=ss[:, 0:1],
                          reads=[x], writes=[junk, ss])
                        rstd_from_ss(ss[:, 0:1], 2048.0, ss, ss, ss[:, 0:1])
                        xb = xbr.next()
                        I(DVE, nc.vector.tensor_scalar, xb[:], x[:], ss[:, 0:1], None, ALU.mult,
                          reads=[x, ss], writes=[xb])
                        pT = pTx.next()
                        for c in range(16):
                            I(PE, nc.tensor.transpose, pT[:, c * 128:(c + 1) * 128], xb[:, c * 128:(c + 1) * 128],
                              identb[:], reads=[xb, identb], writes=[pT])
                        I(DVE, nc.vector.tensor_tensor, xnT[:, :, k * 128:(k + 1) * 128],
                          pT[:].rearrange("p (c t) -> p c t", c=16), bc(g1col[:], [(1, 16), (0, 128)]), ALU.mult,
                          reads=[pT, g1col], writes=[xnT])

                    slab = load_slab(1024, 32)
                    gps = main.next()
                    for c in range(16):
                        I(PE, nc.tensor.matmul, gps[0:32, :], slab[:, c, 0:32], xnT[:, c, :],
                          start=(c == 0), stop=(c == 15), reads=[xnT, slab], writes=[gps])
                    gT = gTr.next()
                    I(ACT, nc.scalar.copy, gT[0:32, :], gps[0:32, :], reads=[gps], writes=[gT])
                    Es = []
                    for k, te in enumerate(tiles):
                        zps = main.next()
                        I(PE, nc.tensor.matmul, zps[:], gT[0:33, k * 128:(k + 1) * 128], upb[0:33, :],
                          start=True, stop=True, reads=[gT, upb], writes=[zps])
                        e1 = e1r.next(); sp = spr.next()
                        I(ACT, nc.scalar.activation, e1[:], zps[:], AF.Exp, scale=-1.0, reads=[zps], writes=[e1])
                        I(ACT, nc.scalar.activation, sp[:], e1[:], AF.Ln, bias=1.0, reads=[e1], writes=[sp])
                        cum = main.next()
                        I(PE, nc.tensor.matmul, cum[:, 0:256], cst[:, C_LINCL:C_LINCL + 128], sp[:, 0:256],
                          start=True, stop=True, reads=[cst, sp], writes=[cum])
                        I(PE, nc.tensor.matmul, cum[:, 256:512], cst[:, C_UINCL:C_UINCL + 128], sp[:, 256:512],
                          start=True, stop=True, reads=[cst, sp], writes=[cum])
                        rem = main.next()
                        I(PE, nc.tensor.matmul, rem[:, 0:256], cst[:, C_USTRICT:C_USTRICT + 128], sp[:, 0:256],
                          start=True, stop=True, reads=[cst, sp], writes=[rem])
                        I(PE, nc.tensor.matmul, rem[:, 256:512], cst[:, C_LSTRICT:C_LSTRICT + 128], sp[:, 256:512],
                          start=True, stop=True, reads=[cst, sp], writes=[rem])
                        E = Er.next(); Es.append(E)
                        I(ACT, nc.scalar.activation, E[:, 0, :], cum[:], AF.Exp, scale=-1.0 / 16, reads=[cum], writes=[E])
                        I(ACT, nc.scalar.activation, E[:, 1, :], cum[:], AF.Exp, scale=1.0 / 16, reads=[cum], writes=[E])
                        I(ACT, nc.scalar.activation, E[:, 2, :], rem[:], AF.Exp, scale=-1.0 / 16, reads=[rem], writes=[E])
                        tot = main.next()
                        for cc in range(4):
                            I(PE, nc.tensor.matmul, tot[:, cc:cc + 1], sp[:, cc * 128:(cc + 1) * 128],
                              cst[:, C_ONES:C_ONES + 1], start=True, stop=True, reads=[cst, sp], writes=[tot])
                        dec = decr.next()
                        I(ACT, nc.scalar.activation, dec[:], tot[:, 0:4], AF.Exp, scale=-1.0 / 16, reads=[tot], writes=[dec])
                        D(ACT, T["dec_s"][te], dec[:], dec, reads=[dec])

                    slab = load_slab(0, 512)
                    for k, te in enumerate(tiles):
                        ps = proj(slab, xnT, k)
                        E = Es[k]
                        QK = QKr.next(); kst = kstr.next()
                        qa = bc(ps[:, 0:256], [(0, 2), (1, 256)])
                        ka = bc(ps[:, 256:512], [(0, 2), (1, 256)])
                        I(DVE, nc.vector.scalar_tensor_tensor, QK[:, 0:2, :], qa, 0.125,
                          E[:, 0, :].rearrange("p (a b) -> p a b", a=2), ALU.mult, ALU.mult,
                          reads=[ps, E], writes=[QK])
                        I(DVE, nc.vector.tensor_tensor, QK[:, 2:4, :], ka, E[:, 1, :].rearrange("p (a b) -> p a b", a=2),
                          ALU.mult, reads=[ps, E], writes=[QK])
                        I(DVE, nc.vector.tensor_tensor, kst[:], ka, E[:, 2, :].rearrange("p (a b) -> p a b", a=2),
                          ALU.mult, reads=[ps, E], writes=[kst])
                        D(POOL, T["kst_s"][te * 128:(te + 1) * 128, :], kst[:].rearrange("p a b -> p (a b)"), kst, reads=[kst])
                        def tail_qk(QK=QK, te=te):
                            pT = pTs.next()
                            QKf = QK[:].rearrange("p a b -> p (a b)")
                            for b8 in range(8):
                                I(PE, nc.tensor.transpose, pT[:, b8 * 128:(b8 + 1) * 128], QKf[:, b8 * 128:(b8 + 1) * 128],
                                  identb[:], reads=[QK, identb], writes=[pT])
                            GT = GTr.next()
                            I(ACT, nc.scalar.copy, GT[:].rearrange("p a b -> p (a b)"), pT[:], reads=[pT], writes=[GT])
                            D(ACT, T["glaT_s"][te], GT[:].rearrange("p a b -> p (a b)"), GT, reads=[GT])
                        defer(tail_qk)

                    slab = load_slab(512, 512)
                    for k, te in enumerate(tiles):
                        ps = proj(slab, xnT, k)
                        va = var.next()
                        I(ACT, nc.scalar.copy, va[:], ps[:], reads=[ps], writes=[va])
                        D(ACT, T["va_s"][te * 128:(te + 1) * 128, :], va[:], va, reads=[va])

                    if own:
                        slab = load_slab(1056, 512)
                        for k, te in enumerate(tiles):
                            ps = proj(slab, xnT, k)
                            rs = rsr.next()
                            I(ACT, nc.scalar.activation, rs[:], ps[:], AF.Silu, reads=[ps], writes=[rs])
                            to = te - HT
                            D(ACT, T["rs_s"][to * 128:(to + 1) * 128, :], rs[:], rs, reads=[rs])

                    for which in ("q", "k"):
                        if which == "q" and not own:
                            continue
                        base = 1568 if which == "q" else 3104
                        g_b = gq_b if which == "q" else gk_b
                        for hg in range(3):
                            slab = load_slab(base + hg * 512, 512)
                            for k, te in enumerate(tiles):
                                ps = proj(slab, xnT, k)
                                ps3 = ps[:].rearrange("p (h d) -> p h d", h=4)
                                sq = sqr.next(); ss4 = ss4r.next()
                                I(ACT, nc.scalar.activation, sq[:], ps[:], AF.Square, reads=[ps], writes=[sq])
                                I(DVE, nc.vector.tensor_reduce, ss4[:], sq[:].rearrange("p (h d) -> p h d", h=4), AX.X, ALU.add,
                                  reads=[sq], writes=[ss4])
                                rstd_from_ss(ss4[:], 128.0, ss4, ss4, ss4[:])
                                qn = qnr.next(); qr = qrr.next(); qsm = qsmr.next(); rt = rtr.next()
                                I(DVE, nc.vector.tensor_tensor, qn[:], ps3, bc(ss4[:], [(1, 4), (0, 128)]), ALU.mult,
                                  reads=[ps, ss4], writes=[qn])
                                I(DVE, nc.vector.tensor_tensor, qr[:], qn[:], bc(g_b[:], [(0, 4), (1, 128)]), ALU.mult,
                                  reads=[qn, g_b], writes=[qr])
                                tb = tabs[k]
                                wi_ = 0 if which == "q" else 1
                                TA = tb[:, wi_, 0, :]; TB = tb[:, wi_, 1, :]
                                I(DVE, nc.vector.tensor_tensor, qsm[:], qn[:, :, 0:32], bc(TA, [(0, 4), (1, 32)]), ALU.mult,
                                  reads=[qn, tb], writes=[qsm])
                                I(POOL, nc.gpsimd.tensor_tensor, rt[:, :, 0, :], qn[:, :, 16:32], bc(TB[:, 0:16], [(0, 4), (1, 16)]), ALU.mult,
                                  reads=[qn, tb], writes=[rt])
                                I(POOL, nc.gpsimd.tensor_tensor, rt[:, :, 1, :], qn[:, :, 0:16], bc(TB[:, 16:32], [(0, 4), (1, 16)]), ALU.mult,
                                  reads=[qn, tb], writes=[rt])
                                I(DVE, nc.vector.tensor_tensor, qr[:, :, 0:32].rearrange("p h (a b) -> p h a b", a=2),
                                  qsm[:].rearrange("p h (a b) -> p h a b", a=2), rt[:, :, 0:2, :], ALU.add,
                                  reads=[qsm, rt], writes=[qr])
                                def tail_d(qr=qr, te=te, hg=hg, which=which):
                                    pT = pTs.next()
                                    for h in range(4):
                                        I(PE, nc.tensor.transpose, pT[:, h * 128:(h + 1) * 128], qr[:, h, :], identb[:],
                                          reads=[qr, identb], writes=[pT])
                                    TT = TTr.next()
                                    I(ACT, nc.scalar.copy, TT[:].rearrange("p a b -> p (a b)"), pT[:, 0:512], reads=[pT], writes=[TT])
                                    if which == "q":
                                        to = te - HT
                                        dst = T["qdT_s"][hg * 4:(hg + 1) * 4, :, to * 128:(to + 1) * 128]
                                    else:
                                        dst = T["kdT_s"][hg * 4:(hg + 1) * 4, :, te * 128:(te + 1) * 128]
                                    D(ACT, dst.rearrange("h p t -> p h t"), TT[:], TT, reads=[TT])
                                defer(tail_d)

                    for hg in range(3):
                        slab = load_slab(4640 + hg * 512, 512)
                        for k, te in enumerate(tiles):
                            ps = proj(slab, xnT, k)
                            vd = vdr.next()
                            I(ACT, nc.scalar.copy, vd[:, :, 0:128], ps[:].rearrange("p (h d) -> p h d", h=4),
                              reads=[ps], writes=[vd])
                            I(DVE, nc.vector.tensor_copy, vd[:, :, 128:129], bc(auxs[k][:, 32:33], [(0, 4), (1, 1)]),
                              reads=[auxs[k]], writes=[vd])
                            D(POOL, T["vd_s"][te * 128:(te + 1) * 128, hg * 516:(hg + 1) * 516], vd[:].rearrange("p h d -> p (h d)"),
                              vd, reads=[vd])
                flush()
                fw.end_phase()

        if "gla" in phases:
            with ExitStack() as es:
                og_b = fw.sbuf(es, "og_b", [128, 512], F32)
                D(SP, og_b[:], T["out_g"].partition_broadcast(128), og_b, writes=[og_b])
                st_f = fw.sbuf(es, "st_f", [128, 2, 128], F32)
                st_fb = fw.sbuf(es, "st_fb", [128, 2, 128], BF16)
                st_b = fw.sbuf(es, "st_b", [128, 2, 128], F32)
                st_bb = fw.sbuf(es, "st_bb", [128, 2, 128], BF16)
                SR = fw.sbuf(es, "SR", [128, NT_OWN, 2, 128], BF16)
                for b in (st_f, st_fb, st_b, st_bb):
                    I(DVE, nc.vector.memset, b[:], 0.0, writes=[b])
                kstr = Ring(fw, es, "gkst", 3, [128, 512], BF16)
                var = Ring(fw, es, "gva", 3, [128, 512], BF16)
                decr = Ring(fw, es, "gdec", 3, [128, 4], F32)
                GTr = Ring(fw, es, "gGT", 2, [128, 8, 128], BF16)
                rsr = Ring(fw, es, "grs", 2, [128, 512], BF16)
                Smr = Ring(fw, es, "Sm", 2, [128, 8, 128], BF16)
                sqr = Ring(fw, es, "gsq", 2, [128, 512], F32)
                ss4r = Ring(fw, es, "gss4", 2, [128, 4], F32)
                onr = Ring(fw, es, "gon", 2, [128, 512], F32)
                mixr = Ring(fw, es, "gmix", 2, [128, 512], BF16)
                inc_r = Ring(fw, es, "inc", 2, [128, 2, 128], F32, psum=True)
                S_r = Ring(fw, es, "Sps", 1, [128, 8, 128], F32, psum=True)
                o_r = Ring(fw, es, "ops", 2, [128, 4, 128], F32, psum=True)
                dummy = fw.psum(es, "dummy", [128, 128], F32)

                def load_tile(te, need_out):
                    kst = kstr.next(); va = var.next(); dec = decr.next()
                    D(SP, kst[:], T["kst_s"][te * 128:(te + 1) * 128, :], kst, writes=[kst])
                    D(SP, va[:], T["va_s"][te * 128:(te + 1) * 128, :], va, writes=[va])
                    D(SP, dec[:], T["dec_s"][te], dec, writes=[dec])
                    GT = rs = None
                    if need_out:
                        GT = GTr.next(); rs = rsr.next()
                        to = te - HT
                        D(SP, GT[:].rearrange("p a b -> p (a b)"), T["glaT_s"][te], GT, writes=[GT])
                        D(SP, rs[:], T["rs_s"][to * 128:(to + 1) * 128, :], rs, writes=[rs])
                    return kst, va, dec, GT, rs

                def state_update(st, stb, kst, va, dec, d):
                    inc = inc_r.next()
                    for h in range(4):
                        c, hh = h // 2, h % 2
                        I(PE, nc.tensor.matmul, inc[hh * 64:(hh + 1) * 64, c, :],
                          kst[:, d * 256 + h * 64:d * 256 + (h + 1) * 64], va[:, h * 128:(h + 1) * 128],
                          start=True, stop=True, reads=[kst, va], writes=[inc])
                    for c in range(2):
                        I(DVE, nc.vector.scalar_tensor_tensor, st[:, c, :], st[:, c, :], dec[:, 2 * d + c:2 * d + c + 1],
                          inc[:, c, :], ALU.mult, ALU.add, reads=[st, dec, inc], writes=[st])
                    I(ACT, nc.scalar.copy, stb[:], st[:], reads=[st], writes=[stb])

                for te in range(NT_EXT - 1, HT - 1, -1):
                    kst, va, dec, _, _ = load_tile(te, False)
                    if te < HT + NT_OWN:
                        I(ACT, nc.scalar.copy, SR[:, te - HT, :, :], st_bb[:], reads=[st_bb], writes=[SR])
                    state_update(st_b, st_bb, kst, va, dec, 1)

                for te in range(0, HT + NT_OWN):
                    need = te >= HT
                    kst, va, dec, GT, rs = load_tile(te, need)
                    if need:
                        to = te - HT
                        Sps = S_r.next()
                        for hh in range(2):
                            if hh == 1:
                                I(PE, nc.tensor.matmul, dummy[:], GT[:, 0, :], GT[:, 1, :], start=True, stop=True,
                                  reads=[GT], writes=[dummy])
                            for d in range(2):
                                for c in range(2):
                                    h = 2 * c + hh
                                    kT = GT[hh * 64:(hh + 1) * 64, (2 + d) * 2 + c, :]
                                    qT = GT[hh * 64:(hh + 1) * 64, d * 2 + c, :]
                                    I(PE, nc.tensor.matmul, Sps[:, d * 4 + h, :], kT, qT, start=True, stop=True,
                                      reads=[GT], writes=[Sps])
                        Sm = Smr.next()
                        for d in range(2):
                            I(DVE, nc.vector.tensor_tensor, Sm[:, d * 4:(d + 1) * 4, :], Sps[:, d * 4:(d + 1) * 4, :],
                              bc(trib[:, d * 128:(d + 1) * 128], [(0, 4), (1, 128)]), ALU.mult,
                              reads=[Sps, trib], writes=[Sm])
                        ops = o_r.next()
                        for h in range(4):
                            c, hh = h // 2, h % 2
                            vh = va[:, h * 128:(h + 1) * 128]
                            I(PE, nc.tensor.matmul, ops[:, h, :], Sm[:, h, :], vh, start=True, stop=False,
                              reads=[Sm, va], writes=[ops])
                            I(PE, nc.tensor.matmul, ops[:, h, :], Sm[:, 4 + h, :], vh, start=False, stop=False,
                              reads=[Sm, va], writes=[ops])
                            I(PE, nc.tensor.matmul, ops[:, h, :], GT[hh * 64:(hh + 1) * 64, 0 * 2 + c, :],
                              st_fb[hh * 64:(hh + 1) * 64, c, :], start=False, stop=False,
                              reads=[GT, st_fb], writes=[ops])
                            I(PE, nc.tensor.matmul, ops[:, h, :], GT[hh * 64:(hh + 1) * 64, 1 * 2 + c, :],
                              SR[hh * 64:(hh + 1) * 64, to, c, :], start=False, stop=True,
                              reads=[GT, SR], writes=[ops])
                        opf = ops[:].rearrange("p h d -> p (h d)")
                        sq = sqr.next(); ss4 = ss4r.next(); on = onr.next(); mix = mixr.next()
                        I(ACT, nc.scalar.activation, sq[:], opf, AF.Square, reads=[ops], writes=[sq])
                        I(DVE, nc.vector.tensor_reduce, ss4[:], sq[:].rearrange("p (h d) -> p h d", h=4), AX.X, ALU.add,
                          reads=[sq], writes=[ss4])
                        rstd_from_ss(ss4[:], 128.0, ss4, ss4, ss4[:])
                        I(DVE, nc.vector.tensor_tensor, on[:].rearrange("p (h d) -> p h d", h=4), ops[:],
                          bc(ss4[:], [(1, 4), (0, 128)]), ALU.mult, reads=[ops, ss4], writes=[on])
                        I(DVE, nc.vector.tensor_tensor, on[:], on[:], og_b[:], ALU.mult, reads=[on, og_b], writes=[on])
                        I(DVE, nc.vector.tensor_tensor, mix[:], on[:], rs[:], ALU.mult, reads=[on, rs], writes=[mix])
                        D(SP, T["mixed_s"][to * 128:(to + 1) * 128, 0:512], mix[:], mix, reads=[mix])
                    state_update(st_f, st_fb, kst, va, dec, 0)
                fw.end_phase()

        if "dil" in phases:
            with ExitStack() as es:
                cmaskb = fw.sbuf(es, "cmaskb", [128, 17 * 128], BF16)
                I(DVE, nc.vector.tensor_copy, cmaskb[:], cst[:, C_CMASK:C_CMASK + 17 * 128], reads=[cst], writes=[cmaskb])
                kTr = Ring(fw, es, "dkT", 2, [128, NEXT], BF16)
                vr = Ring(fw, es, "dv", 2, [128, NT_EXT, 129], BF16)
                qTr = Ring(fw, es, "dqT", 2, [128, NTOK], BF16)
                exr = Ring(fw, es, "dex", 4, [128, 512], BF16)
                pmr = Ring(fw, es, "dpm", 5, [128, 512], BF16)
                rdr = Ring(fw, es, "drd", 3, [128, 1], F32)
                oor = Ring(fw, es, "doo", 3, [128, 128], BF16)
                S_r = Ring(fw, es, "dS", 3, [128, 512], F32, psum=True)
                o_r = Ring(fw, es, "dO", 2, [128, 129], F32, psum=True)
                tg = tables_gen(es) if "tables" in phases else None
                dstep = 0
                vd_v = T["vd_s"].rearrange("(t p) (h d) -> h p t d", p=128, h=12)
                def dil_load(h):
                    kT = kTr.next(); v = vr.next(); qT = qTr.next()
                    D(SP, kT[:], T["kdT_s"][h], kT, writes=[kT])
                    D(SP, qT[:], T["qdT_s"][h], qT, writes=[qT])
                    for v4 in range(4):
                        D(SP, v[:, v4 * 12:(v4 + 1) * 12, :], vd_v[h][:, v4 * 12:(v4 + 1) * 12, :], v, writes=[v])
                    return kT, v, qT
                nxt = dil_load(0)
                dpend = []
                for h in range(12):
                    kT, v, qT = nxt
                    while dpend:
                        dpend.pop(0)()
                    if h + 1 < 12:
                        nxt = dil_load(h + 1)
                    for qi in range(NT_OWN):
                        dstep += 1
                        if tg is not None and dstep % 3 == 0:
                            next(tg, None)
                        ops = o_r.next()
                        kts = list(range(qi, qi + 17))
                        for g0 in range(0, 17, 4):
                            grp = kts[g0:g0 + 4]
                            n = len(grp)
                            Sps = S_r.next()
                            for k, kt in enumerate(grp):
                                I(PE, nc.tensor.matmul, Sps[:, k * 128:(k + 1) * 128], kT[:, kt * 128:(kt + 1) * 128],
                                  qT[:, qi * 128:(qi + 1) * 128], start=True, stop=True, reads=[kT, qT], writes=[Sps])
                            ex = exr.next(); pm = pmr.next()
                            I(ACT, nc.scalar.activation, ex[:, 0:n * 128], Sps[:, 0:n * 128], AF.Exp, reads=[Sps], writes=[ex])
                            I(DVE, nc.vector.tensor_tensor, pm[:, 0:n * 128], ex[:, 0:n * 128],
                              cmaskb[:, g0 * 128:(g0 + n) * 128], ALU.mult, reads=[ex, cmaskb], writes=[pm])

                            def pv(grp=grp, g0=g0, pm=pm, ops=ops, v=v, qi=qi, h=h):
                                for k, kt in enumerate(grp):
                                    I(PE, nc.tensor.matmul, ops[:, 0:129], pm[:, k * 128:(k + 1) * 128], v[:, kt, :],
                                      start=(g0 + k == 0), stop=(g0 + k == 16), reads=[pm, v], writes=[ops])
                                if g0 + len(grp) == 17:
                                    rd = rdr.next(); oo = oor.next()
                                    I(DVE, nc.vector.reciprocal, rd[:], ops[:, 128:129], reads=[ops], writes=[rd])
                                    I(DVE, nc.vector.tensor_scalar, oo[:], ops[:, 0:128], rd[:, 0:1], None, ALU.mult,
                                      reads=[ops, rd], writes=[oo])
                                    D(POOL, T["mixed_s"][qi * 128:(qi + 1) * 128, 512 + h * 128:512 + (h + 1) * 128], oo[:], oo,
                                      reads=[oo])
                            dpend.append(pv)
                            while len(dpend) > 2:
                                dpend.pop(0)()
                while dpend:
                    dpend.pop(0)()
                if tg is not None:
                    for _ in tg:
                        pass
                fw.end_phase()

        if "wout" in phases:
            with ExitStack() as es:
                Wo = fw.sbuf(es, "Wo", [128, 16, 2048], BF16)
                g2col = fw.sbuf(es, "g2col", [128, 16], F32)
                D(SP, g2col[:], T["g2"], g2col, writes=[g2col])
                with ExitStack() as es2:
                    wst = Ring(fw, es2, "wst2", 2, [128, 2048], F32)
                    for c in range(16):
                        s_ = wst.next()
                        D(SP, s_[:], T["w_out"][c * 128:(c + 1) * 128, :], s_, writes=[s_])
                        if c % 2 == 0:
                            I(ACT, nc.scalar.copy, Wo[:, c, :], s_[:], reads=[s_], writes=[Wo])
                        else:
                            I(DVE, nc.vector.tensor_copy, Wo[:, c, :], s_[:], reads=[s_], writes=[Wo])
                    fw.barrier()
                mxr = Ring(fw, es, "mx", 3, [128, 2048], BF16)
                mTr = Ring(fw, es, "mT", 3, [128, 16, 128], BF16)
                xr = Ring(fw, es, "x5", 3, [128, 2048], F32)
                ssr = Ring(fw, es, "ss5", 3, [128, 1], F32)
                xb2r = Ring(fw, es, "xb2", 2, [128, 2048], BF16)
                xnTr = Ring(fw, es, "xn2T", 2, [128, 16, 128], BF16)
                pTx = Ring(fw, es, "pT5", 2, [128, 2048], BF16, psum=True)
                main = Ring(fw, es, "mp5", 4, [128, 512], F32, psum=True)
                st = {}

                def stepA(to):
                    te = to + HT
                    mx = mxr.next(); x = xr.next()
                    D(SP, mx[:], T["mixed_s"][to * 128:(to + 1) * 128, :], mx, writes=[mx])
                    D(SP, x[:], T["xe"][te * 128:(te + 1) * 128, :], x, writes=[x])
                    pT = pTx.next()
                    for c in range(16):
                        I(PE, nc.tensor.transpose, pT[:, c * 128:(c + 1) * 128], mx[:, c * 128:(c + 1) * 128], identb[:],
                          reads=[mx, identb], writes=[pT])
                    mT = mTr.next()
                    I(ACT, nc.scalar.copy, mT[:].rearrange("p a b -> p (a b)"), pT[:], reads=[pT], writes=[mT])
                    st[to] = (mT, x)

                def stepB(to):
                    mT, x1 = st[to]
                    for q in range(4):
                        ps = main.next()
                        for c in range(16):
                            I(PE, nc.tensor.matmul, ps[:], mT[:, c, :], Wo[:, c, q * 512:(q + 1) * 512],
                              start=(c == 0), stop=(c == 15), reads=[mT, Wo], writes=[ps])
                        I(DVE, nc.vector.tensor_tensor, x1[:, q * 512:(q + 1) * 512], ps[:], x1[:, q * 512:(q + 1) * 512], ALU.add,
                          reads=[ps, x1], writes=[x1])
                    D(ACT, T["x1_s"][to * 128:(to + 1) * 128, :], x1[:], x1, reads=[x1])
                    ss = ssr.next()
                    xb2 = xb2r.next()
                    I(ACT, nc.scalar.activation, xb2[:], x1[:], AF.Square, accum_out=ss[:, 0:1], reads=[x1], writes=[xb2, ss])
                    rstd_from_ss(ss[:, 0:1], 2048.0, ss, ss, ss[:, 0:1])
                    I(ACT, nc.scalar.activation, xb2[:], x1[:], AF.Copy, scale=ss[:, 0:1], reads=[x1, ss], writes=[xb2])
                    st[to] = (xb2,)

                def stepC(to):
                    (xb2,) = st.pop(to)
                    pT = pTx.next()
                    for c in range(16):
                        I(PE, nc.tensor.transpose, pT[:, c * 128:(c + 1) * 128], xb2[:, c * 128:(c + 1) * 128], identb[:],
                          reads=[xb2, identb], writes=[pT])
                    xnT = xnTr.next()
                    I(DVE, nc.vector.tensor_tensor, xnT[:], pT[:].rearrange("p (c t) -> p c t", c=16),
                      bc(g2col[:], [(1, 16), (0, 128)]), ALU.mult, reads=[pT, g2col], writes=[xnT])
                    D(ACT, T["xn2T_s"][:, :, to * 128:(to + 1) * 128], xnT[:], xnT, reads=[xnT])

                for n in range(NT_OWN + 2):
                    if n < NT_OWN:
                        stepA(n)
                    if 0 <= n - 1 < NT_OWN:
                        stepB(n - 1)
                    if 0 <= n - 2 < NT_OWN:
                        stepC(n - 2)
                fw.end_phase()

            with ExitStack() as es:
                Wq = fw.sbuf(es, "Wq", [128, 16, 2048], BF16)
                KT = fw.sbuf(es, "KT", [128, 16, 128], F32)
                with ExitStack() as es2:
                    wst = Ring(fw, es2, "wst3", 2, [128, 2048], F32)
                    for c in range(16):
                        s_ = wst.next()
                        D(SP, s_[:], T["wq"][c * 128:(c + 1) * 128, :], s_, writes=[s_])
                        if c % 2 == 0:
                            I(ACT, nc.scalar.copy, Wq[:, c, :], s_[:], reads=[s_], writes=[Wq])
                        else:
                            I(DVE, nc.vector.tensor_copy, Wq[:, c, :], s_[:], reads=[s_], writes=[Wq])
                    kps = fw.psum(es2, "kps", [128, 512], F32)
                    for g4 in range(4):
                        s_ = wst.next()
                        D(SP, s_[:, 0:512].rearrange("p (a b) -> p a b", a=4),
                          T["subk"][g4 * 4:(g4 + 1) * 4].rearrange("a k d -> k a d"), s_, writes=[s_])
                        for a_ in range(4):
                            I(PE, nc.tensor.transpose, kps[:, a_ * 128:(a_ + 1) * 128], s_[:, a_ * 128:(a_ + 1) * 128],
                              identf[:, C_IDENT:C_IDENT + 128], reads=[s_, cst], writes=[kps])
                        I(ACT, nc.scalar.copy, KT[:, g4 * 4:(g4 + 1) * 4, :].rearrange("p a b -> p (a b)"), kps[:],
                          reads=[kps], writes=[KT])
                    fw.barrier()
                xn4r = Ring(fw, es, "xn4", 2, [128, 16, 512], BF16)
                qryTr = Ring(fw, es, "qryT", 1, [128, 16, 512], F32)
                scr = Ring(fw, es, "sc", 2, [128, 16, 128], F32)
                topv = fw.sbuf(es, "topv", [128, 16, 16], F32)
                topi = fw.sbuf(es, "topi", [128, 16, 16], U32)
                topif_r = Ring(fw, es, "topif", 2, [128, 16, 16], F32)
                wkA = fw.sbuf(es, "wkA", [128, 128], F32)
                wkB = fw.sbuf(es, "wkB", [128, 128], F32)
                cand = fw.sbuf(es, "cand", [128, 8, 16, 16], F32)
                wk2A = fw.sbuf(es, "wk2A", [128, 256], F32)
                wk2B = fw.sbuf(es, "wk2B", [128, 256], F32)
                tvb = [Buf(f"tv{i}") for i in range(16)]
                tib = [Buf(f"ti{i}") for i in range(16)]
                cmb = [Buf(f"cm{i}") for i in range(8)]
                cm = fw.sbuf(es, "cm", [128, 8, 16], F32)
                cpos = fw.sbuf(es, "cpos", [128, 8, 16], U32)
                cpb = [Buf(f"cp{i}") for i in range(8)]
                abu = fw.sbuf(es, "abu", [128, 2, 8, 16], U32)
                ab = fw.sbuf(es, "ab", [128, 2, 8, 16], F32)
                eq = fw.sbuf(es, "eq", [128, 8, 16, 16], F32)
                gts = fw.sbuf(es, "gts", [128, 8, 16], F32)
                R_r = Ring(fw, es, "R3", 2, [128, 3, 128], F32)
                Zs = fw.sbuf(es, "Zs", [128, 8], F32)
                main = Ring(fw, es, "mp6", 6, [128, 512], F32, psum=True)
                for g in range(NT_OWN // 4):
                    xn4 = xn4r.next()
                    D(SP, xn4[:], T["xn2T_s"][:, :, g * 512:(g + 1) * 512], xn4, writes=[xn4])
                    qryT = qryTr.next()
                    for hc in range(16):
                        ps = main.next()
                        for c in range(16):
                            I(PE, nc.tensor.matmul, ps[:], Wq[:, c, hc * 128:(hc + 1) * 128], xn4[:, c, :],
                              start=(c == 0), stop=(c == 15), reads=[Wq, xn4], writes=[ps])
                        I(ACT, nc.scalar.copy, qryT[:, hc, :], ps[:], reads=[ps], writes=[qryT])
                    for k in range(4):
                        to = g * 4 + k
                        sc = scr.next()
                        for g4 in range(4):
                            ps = main.next()
                            for a_ in range(4):
                                hc = g4 * 4 + a_
                                I(PE, nc.tensor.matmul, ps[:, a_ * 128:(a_ + 1) * 128], qryT[:, hc, k * 128:(k + 1) * 128], KT[:, hc, :],
                                  start=True, stop=True, reads=[qryT, KT], writes=[ps])
                            I(ACT, nc.scalar.copy, sc[:, g4 * 4:(g4 + 1) * 4, :].rearrange("p a b -> p (a b)"), ps[:],
                              reads=[ps], writes=[sc])
                        for hp in range(0, 16, 2):
                            pr = [(hp, wkA, tvb[hp], tib[hp]), (hp + 1, wkB, tvb[hp + 1], tib[hp + 1])]
                            for (hc, wk_, tv_, ti_) in pr:
                                I(DVE, nc.vector.max, topv[:, hc, 0:8], sc[:, hc, :], reads=[sc], writes=[tv_])
                            for (hc, wk_, tv_, ti_) in pr:
                                I(DVE, nc.vector.max_index, topi[:, hc, 0:8], topv[:, hc, 0:8], sc[:, hc, :], reads=[sc, tv_], writes=[ti_])
                            for (hc, wk_, tv_, ti_) in pr:
                                I(DVE, nc.vector.match_replace, wk_[:], topv[:, hc, 0:8], sc[:, hc, :], NEG, reads=[sc, tv_], writes=[wk_])
                            for (hc, wk_, tv_, ti_) in pr:
                                I(DVE, nc.vector.max, topv[:, hc, 8:16], wk_[:], reads=[wk_], writes=[tv_])
                            for (hc, wk_, tv_, ti_) in pr:
                                I(DVE, nc.vector.max_index, topi[:, hc, 8:16], topv[:, hc, 8:16], wk_[:], reads=[wk_, tv_], writes=[ti_])
                        topif = topif_r.next()
                        I(DVE, nc.vector.tensor_copy, topif[:], topi[:], reads=tib, writes=[topif])
                        tv = topv[:]
                        v1 = bass.AP(tv.tensor, tv.offset, [list(tv.ap[0]), [32, 8], [1, 16], [0, 16]])
                        v2 = bass.AP(tv.tensor, tv.offset + 16, [list(tv.ap[0]), [32, 8], [0, 16], [1, 16]])
                        I(POOL, nc.gpsimd.tensor_tensor, cand[:], v1, v2, ALU.add, reads=tvb, writes=[cand])
                        for h2 in range(0, 8, 2):
                            pr = [(h2, wk2A, cmb[h2]), (h2 + 1, wk2B, cmb[h2 + 1])]
                            chs = {h: cand[:, h, :, :].rearrange("p a b -> p (a b)") for (h, _, _) in pr}
                            for (h, w2, cb) in pr:
                                I(DVE, nc.vector.max, cm[:, h, 0:8], chs[h], reads=[cand], writes=[cb])
                            for (h, w2, cb) in pr:
                                I(DVE, nc.vector.max_index, cpos[:, h, 0:8], cm[:, h, 0:8], chs[h], reads=[cand, cb], writes=[cpb[h]])
                            for (h, w2, cb) in pr:
                                I(DVE, nc.vector.match_replace, w2[:], cm[:, h, 0:8], chs[h], NEG, reads=[cand, cb], writes=[w2])
                            for (h, w2, cb) in pr:
                                I(DVE, nc.vector.max, cm[:, h, 8:16], w2[:], reads=[w2], writes=[cb])
                            for (h, w2, cb) in pr:
                                I(DVE, nc.vector.max_index, cpos[:, h, 8:16], cm[:, h, 8:16], w2[:], reads=[w2, cb], writes=[cpb[h]])
                        I(DVE, nc.vector.tensor_scalar, abu[:, 0, :, :], cpos[:], 4, None, ALU.logical_shift_right, reads=cpb, writes=[abu])
                        I(DVE, nc.vector.tensor_scalar, abu[:, 1, :, :], cpos[:], 15, None, ALU.bitwise_and, reads=cpb, writes=[abu])
                        I(DVE, nc.vector.tensor_copy, ab[:], abu[:], reads=[abu], writes=[ab])
                        R = R_r.next()
                        tfv = topif[:]
                        io16 = bc(cst[:, C_IOTA:C_IOTA + 16], [(0, 8), (0, 16), (1, 16)])
                        for c_ in range(2):
                            abv = ab[:, c_, :, :]
                            I(DVE, nc.vector.tensor_tensor, eq[:], io16, bc(abv, [(16, 8), (1, 16), (0, 16)]), ALU.is_equal,
                              reads=[cst, ab], writes=[eq])
                            idx_b = bass.AP(tfv.tensor, tfv.offset + 16 * c_, [list(tfv.ap[0]), [32, 8], [0, 16], [1, 16]])
                            I(POOL, nc.gpsimd.tensor_tensor, eq[:], eq[:], idx_b, ALU.mult, reads=[eq, topif], writes=[eq])
                            I(DVE, nc.vector.tensor_reduce, R[:, c_, :].rearrange("p (h r) -> p h r", h=8), eq[:], AX.X, ALU.add,
                              reads=[eq], writes=[R])
                        I(POOL, nc.gpsimd.tensor_tensor, gts[:], cm[:], bc(cm[:, :, 0:1], [(16, 8), (0, 16)]), ALU.subtract,
                          reads=cmb, writes=[gts])
                        I(ACT, nc.scalar.activation, gts[:], gts[:], AF.Exp, reads=[gts], writes=[gts])
                        I(DVE, nc.vector.tensor_reduce, Zs[:], gts[:], AX.X, ALU.add, reads=[gts], writes=[Zs])
                        I(DVE, nc.vector.reciprocal, Zs[:], Zs[:], reads=[Zs], writes=[Zs])
                        I(DVE, nc.vector.tensor_tensor, R[:, 2, :].rearrange("p (h r) -> p h r", h=8), gts[:],
                          bc(Zs[:], [(1, 8), (0, 16)]), ALU.mult, reads=[gts, Zs], writes=[R])
                        D(ACT, T["r3_s"][to * 128:(to + 1) * 128, :], R[:].rearrange("p a b -> p (a b)"), R, reads=[R])
                fw.end_phase()

        if "gmat" in phases:
            with ExitStack() as es:
                Rr = Ring(fw, es, "gR", 3, [128, 3, 128], F32)
                RT_r = Ring(fw, es, "gRT", 2, [128, 3, 128], F32)
                gtb_r = Ring(fw, es, "gtb", 2, [128, 128], BF16)
                RTb_r = Ring(fw, es, "gRTb", 2, [128, 2, 128], BF16)
                iotab = fw.sbuf(es, "iotab", [128, 128], BF16)
                I(DVE, nc.vector.tensor_copy, iotab[:], cst[:, C_IOTA:C_IOTA + 128], reads=[cst], writes=[iotab])
                OI_r = Ring(fw, es, "OI", 2, [128, 64, 128], BF16)
                OJ_r = Ring(fw, es, "OJ", 2, [128, 64, 128], BF16)
                OJg_r = Ring(fw, es, "OJg", 2, [128, 64, 128], BF16)
                Gr = Ring(fw, es, "Gall", 2, [128, 128, 128], BF16)
                pR = fw.psum(es, "pR", [128, 3, 128], F32)
                cps = Ring(fw, es, "cps", 4, [128, 4, 128], F32, psum=True)
                iota = cst[:, C_IOTA:C_IOTA + 128]

                Rl = {}

                def rload(to):
                    R = Rr.next()
                    D(SP, R[:].rearrange("p a b -> p (a b)"), T["r3_s"][to * 128:(to + 1) * 128, :], R, writes=[R])
                    Rl[to] = R

                def front(to):
                    R = Rl.pop(to); RT = RT_r.next(); gtb = gtb_r.next()
                    for c in range(3):
                        I(PE, nc.tensor.transpose, pR[:, c, :], R[:, c, :], identf[:, C_IDENT:C_IDENT + 128],
                          reads=[R, cst], writes=[pR])
                    I(ACT, nc.scalar.copy, RT[:].rearrange("p a b -> p (a b)"), pR[:].rearrange("p a b -> p (a b)"),
                      reads=[pR], writes=[RT])
                    I(ACT, nc.scalar.copy, gtb[:], RT[:, 2, :], reads=[RT], writes=[gtb])
                    RTb = RTb_r.next()
                    I(ACT, nc.scalar.copy, RTb[:].rearrange("p a b -> p (a b)"), RT[:, 0:2, :].rearrange("p a b -> p (a b)"),
                      reads=[RT], writes=[RTb])
                    return RTb, gtb

                def gens(hf, RT, gtb):
                    t0h = hf * 64
                    OI = OI_r.next(); OJ = OJ_r.next(); OJg = OJg_r.next()
                    I(DVE, nc.vector.tensor_tensor, OI[:], bc(iotab[:], [(0, 64), (1, 128)]),
                      bc(RT[:, 0, t0h:t0h + 64], [(1, 64), (0, 128)]), ALU.is_equal, reads=[iotab, RT], writes=[OI])
                    I(DVE, nc.vector.tensor_tensor, OJ[:], bc(iotab[:], [(0, 64), (1, 128)]),
                      bc(RT[:, 1, t0h:t0h + 64], [(1, 64), (0, 128)]), ALU.is_equal, reads=[iotab, RT], writes=[OJ])
                    I(POOL, nc.gpsimd.tensor_tensor, OJg[:], OJ[:], bc(gtb[:, t0h:t0h + 64], [(1, 64), (0, 128)]), ALU.mult,
                      reads=[OJ, gtb], writes=[OJg])
                    return OI, OJg

                def gpart(to, hf, OI, OJg, G, g_b):
                    t0h = hf * 64
                    for t4 in range(16):
                        ps = cps.next()
                        for k in range(4):
                            t = t4 * 4 + k
                            I(PE, nc.tensor.matmul, ps[:, k, :], OI[:, t, :], OJg[:, t, :], start=True, stop=True,
                              reads=[OI, OJg], writes=[ps])
                        gv = G[:]
                        g_out = bass.AP(gv.tensor, gv.offset + t0h + t4 * 4, [list(gv.ap[0]), [128, 128], [1, 4]])
                        ps_jk = ps[:].rearrange("p k j -> p j k")
                        gsl = g_b[hf * 16 + t4]
                        I(ACT, nc.scalar.copy, g_out, ps_jk, reads=[ps], writes=[gsl])
                    if hf == 1:
                        D(SP, T["G_s"][to], G[:].rearrange("p j t -> p (j t)"), G, reads=g_b, writes=[G])

                rload(0); rload(1)
                fr = {0: front(0)}
                gn = {0: gens(0, *fr[0])}
                Gs = {}
                for n in range(2 * NT_OWN):
                    to, hf = n // 2, n % 2
                    if hf == 0:
                        if to + 2 < NT_OWN:
                            rload(to + 2)
                        G = Gr.next()
                        g_b = [Buf("gsl") for _ in range(32)]
                        for gb_ in g_b:
                            gb_.last_w = G.last_w; gb_.readers = list(G.readers)
                        Gs[to] = (G, g_b)
                    if n + 1 < 2 * NT_OWN:
                        to1, hf1 = (n + 1) // 2, (n + 1) % 2
                        if hf1 == 0:
                            fr[to1] = front(to1)
                        gn[n + 1] = gens(hf1, *fr[to1])
                    OI, OJg = gn.pop(n)
                    G, g_b = Gs[to]
                    gpart(to, hf, OI, OJg, G, g_b)
                    if hf == 1:
                        Gs.pop(to); fr.pop(to)
                fw.end_phase()

        if "peer" in phases:
            with ExitStack() as es:
                GP = fw.sbuf(es, "GP", [128, 128, 512], BF16)
                xnT = fw.sbuf(es, "pxnT", [128, 16, 512], BF16)
                dTr = Ring(fw, es, "pdT", 5, [128, 16, 128], BF16)
                actr = Ring(fw, es, "pact", 3, [128, 512], BF16)
                upr = Ring(fw, es, "pup", 4, [128, 4, 512], BF16)
                x1r = Ring(fw, es, "px1", 2, [128, 512], F32)
                outr = Ring(fw, es, "pout", 2, [128, 512], F32)
                bank = Ring(fw, es, "pb", 8, [128, 512], F32, psum=True)
                gp_b = [Buf(f"gp{i}") for i in range(32)]
                fw.phase_bufs.extend(gp_b)
                NP = NTOK // 512

                def gload(ps_, jg, eng):
                    for k in range(4):
                        gsrc = T["G_s"][ps_ * 4 + k].rearrange("p (j t) -> p j t", j=128)
                        D(eng, GP[:, jg * 4:(jg + 1) * 4, k * 128:(k + 1) * 128], gsrc[:, jg * 4:(jg + 1) * 4, :], gp_b[jg],
                          writes=[gp_b[jg]])

                D(SP, xnT[:], T["xn2T_s"][:, :, 0:512], xnT, writes=[xnT])
                for jg in range(32):
                    gload(0, jg, SP if jg % 2 == 0 else POOL)
                for ps_ in range(NP):
                    for j in range(128):
                        dT = dTr.next()
                        D(SP, dT[:].rearrange("p a b -> p (a b)"), T["dT_s"][j], dT, writes=[dT])
                        hp = bank.next()
                        for c in range(16):
                            I(PE, nc.tensor.matmul, hp[:], dT[:, c, :], xnT[:, c, :], start=(c == 0), stop=(c == 15),
                              reads=[dT, xnT], writes=[hp])
                        a = actr.next()
                        I(ACT, nc.scalar.activation, a[:], hp[:], AF.Gelu_apprx_tanh, reads=[hp], writes=[a])
                        I(DVE, nc.vector.tensor_tensor, GP[:, j, :], GP[:, j, :], a[:], ALU.mult, reads=[a, gp_b[j // 4]],
                          writes=[gp_b[j // 4]])
                    if ps_ + 1 < NP:
                        D(SP, xnT[:], T["xn2T_s"][:, :, (ps_ + 1) * 512:(ps_ + 2) * 512], xnT, writes=[xnT])
                    for q in range(4):
                        accs = [bank.next() for _ in range(4)]
                        for jg in range(32):
                            ut = upr.next()
                            D(SP, ut[:], T["up_s"][jg * 4:(jg + 1) * 4, :, q * 512:(q + 1) * 512].rearrange("j p d -> p j d"),
                              ut, writes=[ut])
                            for jj in range(4):
                                j = jg * 4 + jj
                                for k in range(4):
                                    I(PE, nc.tensor.matmul, accs[k][:], GP[:, j, k * 128:(k + 1) * 128], ut[:, jj, :],
                                      start=(j == 0), stop=(j == 127), reads=[gp_b[jg], ut], writes=[accs[k]])
                            if q == 3 and ps_ + 1 < NP:
                                gload(ps_ + 1, jg, POOL)
                        for k in range(4):
                            r0 = ps_ * 512 + k * 128
                            x1 = x1r.next(); o = outr.next()
                            D(SP, x1[:], T["x1_s"][r0:r0 + 128, q * 512:(q + 1) * 512], x1, writes=[x1])
                            I(DVE, nc.vector.tensor_tensor, o[:], accs[k][:], x1[:], ALU.add, reads=[accs[k], x1], writes=[o])
                            D(SP, T["out"][r0:r0 + 128, q * 512:(q + 1) * 512], o[:], o, reads=[o])
                fw.end_phase()
        fw.barrier()
    return nc, fw


_CACHE = {}


def make_in_maps(inp, ncores=8, phases=PHASES):
    f = lambda a: np.ascontiguousarray(np.asarray(a, dtype=np.float32))
    x = f(inp["x"])
    cst = host_consts()
    shared = {
        "cst": cst,
        "g1": f(inp["norm1_g"][0].reshape(16, 128).T),
        "g2": f(inp["norm2_g"][0].reshape(16, 128).T),
        "w_in": f(inp["w_in"][0]),
        "up_f": f(inp["gla_up_f"][0]), "up_b": f(inp["gla_up_b"][0]),
        "bias_f": f(inp["gla_bias_f"][0].reshape(1, 256)), "bias_b": f(inp["gla_bias_b"][0].reshape(1, 256)),
        "out_g": f(inp["gla_out_g"][0].reshape(1, 512)),
        "gq": f(inp["q_norm_g"][0].reshape(1, 128)), "gk": f(inp["k_norm_g"][0].reshape(1, 128)),
        "w_out": f(inp["w_out"][0]), "wq": f(inp["peer_w_query"][0]),
        "subk": f(inp["peer_sub_keys"][0].reshape(16, 128, 128)),
    }
    if "tables" in phases:
        shared["down"] = f(inp["peer_down"][0])
        shared["up"] = f(inp["peer_up"][0])
    maps = []
    for c in range(ncores):
        b, p = c // 4, c % 4
        xe = np.zeros((NEXT, D_MODEL), np.float32)
        lo = p * NTOK - HALO
        hi = lo + NEXT
        slo, shi = max(lo, 0), min(hi, SEQ)
        xe[slo - lo:shi - lo] = x[b, slo:shi]
        m = dict(shared)
        m["xe"] = xe
        m["aux"] = host_aux(p)
        maps.append(m)
    return maps


def kernel(**inputs):
    if "nc" not in _CACHE:
        _CACHE["nc"] = build_program()[0]
    nc = _CACHE["nc"]
    maps = make_in_maps(inputs)
    res = run_bass_kernel_spmd(nc, maps, core_ids=list(range(8)))
    out = np.zeros((2, SEQ, D_MODEL), np.float32)
    for c in range(8):
        b, p = c // 4, c % 4
        out[b, p * NTOK:(p + 1) * NTOK] = np.asarray(res.results[c]["out"], dtype=np.float32)
    return out
```

```python
import numpy as np
from contextlib import ExitStack
import concourse.bass as bass
import concourse.mybir as mybir
from concourse.bass_utils import run_bass_kernel_spmd

F32 = mybir.dt.float32
BF16 = mybir.dt.bfloat16
U32 = mybir.dt.uint32
AF = mybir.ActivationFunctionType
ALU = mybir.AluOpType
AX = mybir.AxisListType

D_MODEL = 2048
SEQ = 16384
NTOK = 4096
HALO = 1024
NEXT = NTOK + 2 * HALO
NT_EXT = NEXT // 128
NT_OWN = NTOK // 128
HT = HALO // 128
IN_W = 6176
EPS = 1e-6
NEG = -1e30

PHASES = ("w_in", "tables", "stage1", "gla", "dil", "wout", "gmat", "peer")
DEBUG_OUTS = ()


class SemRec:
    def __init__(self, sem):
        self.sem = sem
        self.cnt = 0


class Ev:
    __slots__ = ("rec", "val", "eng")

    def __init__(self, rec, val, eng):
        self.rec = rec
        self.val = val
        self.eng = eng


class Buf:
    def __init__(self, name, t=None):
        self.name = name
        self.t = t
        self.last_w = None
        self.readers = []
        self.dma = None

    def __getitem__(self, k):
        return self.t[k]


class Eng:
    def __init__(self, fw, name, obj, sem):
        self.fw = fw
        self.name = name
        self.obj = obj
        self.rec = SemRec(sem)
        self.waited = {}

    def wait_ev(self, ev):
        rec = ev.rec
        if ev.val is None:
            val = rec.cnt * 16
        else:
            val = ev.val
            if ev.eng is self and (self.name == "pe" or not self.fw.same_engine_sync):
                return
        if val <= 0:
            return
        if self.waited.get(id(rec), 0) >= val:
            return
        self.waited[id(rec)] = val
        self.obj.wait_ge(rec.sem, val)
        self.fw.n_waits += 1


class FW:
    def __init__(self, nc, n_dma_sems=88, same_engine_sync=True):
        self.nc = nc
        self.same_engine_sync = same_engine_sync
        self.n_waits = 0
        self.n_inst = 0
        self.pe = Eng(self, "pe", nc.tensor, nc.alloc_semaphore("sem_pe"))
        self.act = Eng(self, "act", nc.scalar, nc.alloc_semaphore("sem_act"))
        self.dve = Eng(self, "dve", nc.vector, nc.alloc_semaphore("sem_dve"))
        self.pool = Eng(self, "pool", nc.gpsimd, nc.alloc_semaphore("sem_pool"))
        self.sp = Eng(self, "sp", nc.sync, nc.alloc_semaphore("sem_sp"))
        self.engs = [self.pe, self.act, self.dve, self.pool, self.sp]
        self.free_dma = [SemRec(nc.alloc_semaphore(f"sem_d{i}")) for i in range(n_dma_sems)]
        self.all_dma = list(self.free_dma)
        self.phase_bufs = []

    def sbuf(self, es, name, shape, dtype):
        t = es.enter_context(self.nc.sbuf_tensor("sb_" + name, list(shape), dtype))
        b = Buf(name, t)
        self.phase_bufs.append(b)
        return b

    def psum(self, es, name, shape, dtype=F32):
        t = es.enter_context(self.nc.psum_tensor("ps_" + name, list(shape), dtype))
        b = Buf(name, t)
        self.phase_bufs.append(b)
        return b

    def _deps(self, reads, writes):
        deps = []
        for b in reads:
            if b.last_w is not None:
                deps.append(b.last_w)
        for b in writes:
            if b.last_w is not None:
                deps.append(b.last_w)
            deps.extend(b.readers)
        return deps

    def _record(self, ev, reads, writes):
        for b in writes:
            b.last_w = ev
            b.readers = []
        for b in reads:
            if b in writes:
                continue
            b.readers = [r for r in b.readers if r.rec is not ev.rec]
            b.readers.append(ev)

    def I(self, eng, fn, *args, reads=(), writes=(), **kw):
        for ev in self._deps(reads, writes):
            eng.wait_ev(ev)
        inst = fn(*args, **kw)
        eng.rec.cnt += 1
        inst.then_inc(eng.rec.sem, 1)
        self.n_inst += 1
        self._record(Ev(eng.rec, eng.rec.cnt, eng), reads, writes)
        return inst

    def D(self, eng, out, in_, sb, reads=(), writes=(), **kw):
        for ev in self._deps(reads, writes):
            eng.wait_ev(ev)
        if sb.dma is None:
            sb.dma = self.free_dma.pop()
        rec = sb.dma
        inst = eng.obj.dma_start(out=out, in_=in_, **kw)
        inst.then_inc(rec.sem, 16)
        rec.cnt += 1
        self.n_inst += 1
        self._record(Ev(rec, None, eng), reads, writes)
        return inst

    def barrier(self):
        for e in self.engs:
            for o in self.engs:
                if o is e or o.rec.cnt == 0:
                    continue
                e.wait_ev(Ev(o.rec, o.rec.cnt, o))
            for rec in self.all_dma:
                if rec.cnt:
                    e.wait_ev(Ev(rec, None, None))

    def end_phase(self):
        self.barrier()
        for b in self.phase_bufs:
            if b.dma is not None:
                self.free_dma.append(b.dma)
                b.dma = None
        self.phase_bufs = []


class Ring:
    def __init__(self, fw, es, name, n, shape, dtype, psum=False):
        mk = fw.psum if psum else fw.sbuf
        self.bufs = [mk(es, f"{name}{i}", shape, dtype) for i in range(n)]
        self.i = 0

    def next(self):
        b = self.bufs[self.i % len(self.bufs)]
        self.i += 1
        return b


def bc(ap, steps):
    return bass.AP(ap.tensor, ap.offset, [list(ap.ap[0])] + [[s, c] for (s, c) in steps])


C_LINCL, C_USTRICT, C_UINCL, C_LSTRICT, C_IDENT, C_IOTA, C_BM, C_ONES, C_CMASK = [k * 128 for k in range(9)]
C_COLS = 8 * 128 + 17 * 128


def host_consts():
    j = np.arange(128)[:, None]
    i = np.arange(128)[None, :]
    c = np.zeros((128, C_COLS), np.float32)
    c[:, C_LINCL:C_LINCL + 128] = (j <= i)
    c[:, C_USTRICT:C_USTRICT + 128] = (j > i)
    c[:, C_UINCL:C_UINCL + 128] = (j >= i)
    c[:, C_LSTRICT:C_LSTRICT + 128] = (j < i)
    c[:, C_IDENT:C_IDENT + 128] = (j == i)
    c[:, C_IOTA:C_IOTA + 128] = np.broadcast_to(i, (128, 128))
    c[:, C_BM:C_BM + 128] = ((j // 16) == (i // 16))
    c[:, C_ONES:C_ONES + 128] = 1.0
    for t in range(17):
        off = (t - 8) * 128 + j - i
        m = (np.abs(off) <= 64).astype(np.float32)
        m += ((off % 4 == 0) & (np.abs(off) <= 256))
        m += ((off % 16 == 0) & (np.abs(off) <= 1024))
        c[:, C_CMASK + t * 128:C_CMASK + (t + 1) * 128] = m
    return c


def host_aux(p):
    pos = np.arange(NEXT) + p * NTOK - HALO
    half = 16
    inv_freq = (np.float32(500000.0) ** (-np.arange(half, dtype=np.float32) / np.float32(half))).astype(np.float32)
    ang = pos.astype(np.float32)[:, None] * inv_freq[None, :]
    a = np.zeros((NEXT, 33), np.float32)
    a[:, 0:16] = np.cos(ang)
    a[:, 16:32] = np.sin(ang)
    a[:, 32] = ((pos >= 0) & (pos < SEQ))
    return a


def build_program(phases=PHASES, debug_outs=DEBUG_OUTS):
    nc = bass.Bass("TRN2", target_bir_lowering=False)
    fw = FW(nc)
    I, D = fw.I, fw.D
    PE, ACT, DVE, POOL, SP = fw.pe, fw.act, fw.dve, fw.pool, fw.sp
    T = {}

    def din(name, shape, dt=F32):
        T[name] = nc.dram_tensor(name, list(shape), dt, kind="ExternalInput").ap()

    def dscr(name, shape, dt):
        kind = "ExternalOutput" if name in debug_outs else "Internal"
        T[name] = nc.dram_tensor(name, list(shape), dt, kind=kind).ap()

    din("xe", [NEXT, D_MODEL])
    din("cst", [128, C_COLS])
    din("aux", [NEXT, 33])
    din("g1", [128, 16])
    din("g2", [128, 16])
    din("w_in", [D_MODEL, IN_W])
    din("up_f", [16, 256]); din("up_b", [16, 256]); din("bias_f", [1, 256]); din("bias_b", [1, 256])
    din("out_g", [1, 512]); din("gq", [1, 128]); din("gk", [1, 128])
    din("w_out", [D_MODEL, D_MODEL]); din("wq", [D_MODEL, D_MODEL])
    din("subk", [16, 128, 128])
    if "tables" in phases:
        din("down", [128 * 128, D_MODEL]); din("up", [128 * 128, D_MODEL])
    T["out"] = nc.dram_tensor("out", [NTOK, D_MODEL], F32, kind="ExternalOutput").ap()

    dscr("wi_s", [128, 16, IN_W], BF16)
    dscr("qdT_s", [12, 128, NTOK], BF16)
    dscr("kdT_s", [12, 128, NEXT], BF16)
    dscr("vd_s", [NEXT, 12 * 129], BF16)
    dscr("glaT_s", [NT_EXT, 128, 8 * 128], BF16)
    dscr("kst_s", [NEXT, 512], BF16)
    dscr("va_s", [NEXT, 512], BF16)
    dscr("dec_s", [NT_EXT, 128, 4], F32)
    dscr("rs_s", [NTOK, 512], BF16)
    dscr("mixed_s", [NTOK, D_MODEL], BF16)
    dscr("x1_s", [NTOK, D_MODEL], F32)
    dscr("xn2T_s", [128, 16, NTOK], BF16)
    dscr("r3_s", [NTOK, 384], F32)
    dscr("G_s", [NT_OWN, 128, 128 * 128], BF16)
    if "tables" in phases:
        dscr("dT_s", [128, 128, 2048], BF16)
        dscr("up_s", [128, 128, 2048], BF16)

    with ExitStack() as es0:
        cst = fw.sbuf(es0, "cst", [128, C_COLS], F32)
        identb = fw.sbuf(es0, "identb", [128, 128], BF16)
        trib = fw.sbuf(es0, "trib", [128, 256], BF16)
        D(SP, cst[:], T["cst"], cst, writes=[cst])
        I(DVE, nc.vector.tensor_copy, identb[:], cst[:, C_IDENT:C_IDENT + 128], reads=[cst], writes=[identb])
        I(DVE, nc.vector.tensor_copy, trib[:], cst[:, C_LINCL:C_LINCL + 256], reads=[cst], writes=[trib])
        identf = cst
        fw.end_phase()
        fw.phase_bufs = []

        def rstd_from_ss(ss_ap, n, ss_buf, out_buf, out_ap, eng_recip=DVE):
            I(ACT, nc.scalar.activation, out_ap, ss_ap, AF.Sqrt, scale=1.0 / n, bias=eps_col[:, 0:1],
              reads=[ss_buf, eps_col], writes=[out_buf])
            I(eng_recip, nc.vector.reciprocal, out_ap, out_ap, reads=[out_buf], writes=[out_buf])

        eps_col = fw.sbuf(es0, "eps_col", [128, 1], F32)
        I(DVE, nc.vector.memset, eps_col[:], EPS, writes=[eps_col])

        if "w_in" in phases:
            with ExitStack() as es:
                st = Ring(fw, es, "wst", 2, [128, IN_W], F32)
                bf = Ring(fw, es, "wbf", 2, [128, IN_W], BF16)
                for c in range(16):
                    s = st.next(); b = bf.next()
                    D(SP, s[:], T["w_in"][c * 128:(c + 1) * 128, :], s, writes=[s])
                    if c % 2 == 0:
                        I(ACT, nc.scalar.copy, b[:], s[:], reads=[s], writes=[b])
                    else:
                        I(DVE, nc.vector.tensor_copy, b[:], s[:], reads=[s], writes=[b])
                    D(SP, T["wi_s"][:, c, :], b[:], b, reads=[b])
                fw.end_phase()

        def tables_gen(es):
            dn_r = Ring(fw, es, "dn", 3, [128, 2048], F32)
            up_r = Ring(fw, es, "upf", 3, [128, 2048], F32)
            dnb_r = Ring(fw, es, "dnb", 3, [128, 2048], BF16)
            upb_r = Ring(fw, es, "upb", 2, [128, 2048], BF16)
            dT_r = Ring(fw, es, "dTt", 2, [128, 2048], BF16)
            pT_r = Ring(fw, es, "pTt", 1, [128, 2048], BF16, psum=True)
            down_v = T["down"].rearrange("(i j) d -> j i d", j=128)
            up_v = T["up"].rearrange("(i j) d -> j i d", j=128)
            loaded = {}

            def tload(j):
                dn = dn_r.next(); upf = up_r.next()
                D(SP, dn[:], down_v[j], dn, writes=[dn])
                D(SP, upf[:], up_v[j], upf, writes=[upf])
                loaded[j] = (dn, upf)

            tload(0); tload(1)
            prev = None
            for j in range(129):
                cur = None
                if j < 128:
                    if j + 2 < 128:
                        tload(j + 2)
                    dn, upf = loaded.pop(j)
                    dnb = dnb_r.next(); upb = upb_r.next()
                    I(POOL, nc.gpsimd.tensor_copy, dnb[:], dn[:], reads=[dn], writes=[dnb])
                    I(ACT, nc.scalar.copy, upb[:], upf[:], reads=[upf], writes=[upb])
                    D(SP, T["up_s"][j], upb[:], upb, reads=[upb])
                    cur = (j, dnb)
                if prev is not None:
                    pj, pdnb = prev
                    pT = pT_r.next()
                    for c in range(16):
                        I(PE, nc.tensor.transpose, pT[:, c * 128:(c + 1) * 128], pdnb[:, c * 128:(c + 1) * 128], identb[:],
                          reads=[pdnb, identb], writes=[pT])
                    dT = dT_r.next()
                    I(ACT, nc.scalar.copy, dT[:], pT[:], reads=[pT], writes=[dT])
                    D(SP, T["dT_s"][pj], dT[:], dT, reads=[dT])
                prev = cur
                yield

        if "tables" in phases and "dil" not in phases:
            with ExitStack() as es:
                for _ in tables_gen(es):
                    pass
                fw.end_phase()

        if "stage1" in phases:
            with ExitStack() as es:
                g1col = fw.sbuf(es, "g1col", [128, 16], F32)
                gq_b = fw.sbuf(es, "gq_b", [128, 128], F32)
                gk_b = fw.sbuf(es, "gk_b", [128, 128], F32)
                upb = fw.sbuf(es, "upbx", [33, 512], F32)
                D(SP, g1col[:], T["g1"], g1col, writes=[g1col])
                D(SP, gq_b[:], T["gq"].partition_broadcast(128), gq_b, writes=[gq_b])
                D(SP, gk_b[:], T["gk"].partition_broadcast(128), gk_b, writes=[gk_b])
                I(ACT, nc.scalar.mul, gq_b[:], gq_b[:], 128.0 ** -0.5, reads=[gq_b], writes=[gq_b])
                I(DVE, nc.vector.memset, upb[:], 0.0, writes=[upb])
                D(SP, upb[0:16, 0:256], T["up_f"], upb, writes=[upb])
                D(SP, upb[16:32, 256:512], T["up_b"], upb, writes=[upb])
                D(SP, upb[32:33, 0:256], T["bias_f"], upb, writes=[upb])
                D(SP, upb[32:33, 256:512], T["bias_b"], upb, writes=[upb])

                xr = Ring(fw, es, "x", 2, [128, 2048], F32)
                junk = fw.sbuf(es, "junk", [128, 2048], BF16)
                xbr = Ring(fw, es, "xb", 2, [128, 2048], BF16)
                ssr = Ring(fw, es, "ss", 4, [128, 1], F32)
                xnTr = Ring(fw, es, "xnT", 2, [128, 16, 512], BF16)
                slabr = Ring(fw, es, "slab", 2, [128, 16, 512], BF16)
                gTr = Ring(fw, es, "gT", 2, [33, 512], F32)
                for b in gTr.bufs:
                    I(DVE, nc.vector.memset, b[32:33, :], 1.0, writes=[b])
                auxr = Ring(fw, es, "aux", 8, [128, 33], F32)
                tabr = Ring(fw, es, "tab", 8, [128, 2, 2, 32], F32)
                Er = Ring(fw, es, "E", 4, [128, 3, 512], F32)
                vdr = Ring(fw, es, "vd", 3, [128, 4, 129], BF16)
                sqr = Ring(fw, es, "sq", 2, [128, 512], F32)
                ss4r = Ring(fw, es, "ss4", 2, [128, 4], F32)
                qnr = Ring(fw, es, "qn", 2, [128, 4, 128], F32)
                qsmr = Ring(fw, es, "qsm", 2, [128, 4, 32], F32)
                rtr = Ring(fw, es, "rt", 2, [128, 4, 4, 16], F32)
                qrr = Ring(fw, es, "qr", 3, [128, 4, 128], BF16)
                TTr = Ring(fw, es, "TT", 2, [128, 4, 128], BF16)
                e1r = Ring(fw, es, "e1", 2, [128, 512], F32)
                spr = Ring(fw, es, "sp", 2, [128, 512], F32)
                decr = Ring(fw, es, "dec", 2, [128, 4], F32)
                QKr = Ring(fw, es, "QK", 3, [128, 4, 256], BF16)
                kstr = Ring(fw, es, "kst", 2, [128, 2, 256], BF16)
                var = Ring(fw, es, "va", 2, [128, 512], BF16)
                rsr = Ring(fw, es, "rs", 2, [128, 512], BF16)
                GTr = Ring(fw, es, "GT", 2, [128, 8, 128], BF16)
                pTx = Ring(fw, es, "pTx", 1, [128, 2048], BF16, psum=True)
                main = Ring(fw, es, "mp", 4, [128, 512], F32, psum=True)
                pTs = Ring(fw, es, "pTs", 2, [128, 1024], BF16, psum=True)

                def load_slab(c0, n):
                    s = slabr.next()
                    D(SP, s[:, :, 0:n], T["wi_s"][:, :, c0:c0 + n], s, writes=[s])
                    return s

                pending = []

                def defer(fn):
                    pending.append(fn)

                def flush(keep=0):
                    while len(pending) > keep:
                        pending.pop(0)()

                def proj(slab, xnT, k, n=512):
                    ps = main.next()
                    for c in range(16):
                        I(PE, nc.tensor.matmul, ps[:, 0:n], xnT[:, c, k * 128:(k + 1) * 128], slab[:, c, 0:n],
                          start=(c == 0), stop=(c == 15), reads=[xnT, slab], writes=[ps])
                    flush(keep=1)
                    return ps

                for g in range(NT_EXT // 4):
                    tiles = [4 * g + k for k in range(4)]
                    own = (HT <= tiles[0] < HT + NT_OWN)
                    xnT = xnTr.next()
                    auxs = []
                    tabs = []
                    for k, te in enumerate(tiles):
                        x = xr.next(); aux = auxr.next(); auxs.append(aux)
                        D(SP, x[:], T["xe"][te * 128:(te + 1) * 128, :], x, writes=[x])
                        D(POOL, aux[:], T["aux"][te * 128:(te + 1) * 128, :], aux, writes=[aux])
                        tb = tabr.next(); tabs.append(tb)
                        for wi_, g_b_ in enumerate((gq_b, gk_b)):
                            I(POOL, nc.gpsimd.tensor_tensor, tb[:, wi_, 0, :].rearrange("p (a b) -> p a b", a=2),
                              bc(aux[:, 0:16], [(0, 2), (1, 16)]), g_b_[:, 0:32].rearrange("p (a b) -> p a b", a=2), ALU.mult,
                              reads=[aux, g_b_], writes=[tb])
                            I(DVE, nc.vector.scalar_tensor_tensor, tb[:, wi_, 1, 0:16], aux[:, 16:32], -1.0, g_b_[:, 16:32],
                              ALU.mult, ALU.mult, reads=[aux, g_b_], writes=[tb])
                            I(POOL, nc.gpsimd.tensor_tensor, tb[:, wi_, 1, 16:32], aux[:, 16:32], g_b_[:, 0:16], ALU.mult,
                              reads=[aux, g_b_], writes=[tb])
                        ss = ssr.next()
                        I(ACT, nc.scalar.activation, junk[:], x[:], AF.Square, accum_out=ss[:, 0:1],
                          reads=[x], writes=[junk, ss])
                        rstd_from_ss(ss[:, 0:1], 2048.0, ss, ss, ss[:, 0:1])
                        xb = xbr.next()
                        I(DVE, nc.vector.tensor_scalar, xb[:], x[:], ss[:, 0:1], None, ALU.mult,
                          reads=[x, ss], writes=[xb])
                        pT = pTx.next()
                        for c in range(16):
                            I(PE, nc.tensor.transpose, pT[:, c * 128:(c + 1) * 128], xb[:, c * 128:(c + 1) * 128],
                              identb[:], reads=[xb, identb], writes=[pT])
                        I(DVE, nc.vector.tensor_tensor, xnT[:, :, k * 128:(k + 1) * 128],
                          pT[:].rearrange("p (c t) -> p c t", c=16), bc(g1col[:], [(1, 16), (0, 128)]), ALU.mult,
                          reads=[pT, g1col], writes=[xnT])

                    slab = load_slab(1024, 32)
                    gps = main.next()
                    for c in range(16):
                        I(PE, nc.tensor.matmul, gps[0:32, :], slab[:, c, 0:32], xnT[:, c, :],
                          start=(c == 0), stop=(c == 15), reads=[xnT, slab], writes=[gps])
                    gT = gTr.next()
                    I(ACT, nc.scalar.copy, gT[0:32, :], gps[0:32, :], reads=[gps], writes=[gT])
                    Es = []
                    for k, te in enumerate(tiles):
                        zps = main.next()
                        I(PE, nc.tensor.matmul, zps[:], gT[0:33, k * 128:(k + 1) * 128], upb[0:33, :],
                          start=True, stop=True, reads=[gT, upb], writes=[zps])
                        e1 = e1r.next(); sp = spr.next()
                        I(ACT, nc.scalar.activation, e1[:], zps[:], AF.Exp, scale=-1.0, reads=[zps], writes=[e1])
                        I(ACT, nc.scalar.activation, sp[:], e1[:], AF.Ln, bias=1.0, reads=[e1], writes=[sp])
                        cum = main.next()
                        I(PE, nc.tensor.matmul, cum[:, 0:256], cst[:, C_LINCL:C_LINCL + 128], sp[:, 0:256],
                          start=True, stop=True, reads=[cst, sp], writes=[cum])
                        I(PE, nc.tensor.matmul, cum[:, 256:512], cst[:, C_UINCL:C_UINCL + 128], sp[:, 256:512],
                          start=True, stop=True, reads=[cst, sp], writes=[cum])
                        rem = main.next()
                        I(PE, nc.tensor.matmul, rem[:, 0:256], cst[:, C_USTRICT:C_USTRICT + 128], sp[:, 0:256],
                          start=True, stop=True, reads=[cst, sp], writes=[rem])
                        I(PE, nc.tensor.matmul, rem[:, 256:512], cst[:, C_LSTRICT:C_LSTRICT + 128], sp[:, 256:512],
                          start=True, stop=True, reads=[cst, sp], writes=[rem])
                        E = Er.next(); Es.append(E)
                        I(ACT, nc.scalar.activation, E[:, 0, :], cum[:], AF.Exp, scale=-1.0 / 16, reads=[cum], writes=[E])
                        I(ACT, nc.scalar.activation, E[:, 1, :], cum[:], AF.Exp, scale=1.0 / 16, reads=[cum], writes=[E])
                        I(ACT, nc.scalar.activation, E[:, 2, :], rem[:], AF.Exp, scale=-1.0 / 16, reads=[rem], writes=[E])
                        tot = main.next()
                        for cc in range(4):
                            I(PE, nc.tensor.matmul, tot[:, cc:cc + 1], sp[:, cc * 128:(cc + 1) * 128],
                              cst[:, C_ONES:C_ONES + 1], start=True, stop=True, reads=[cst, sp], writes=[tot])
                        dec = decr.next()
                        I(ACT, nc.scalar.activation, dec[:], tot[:, 0:4], AF.Exp, scale=-1.0 / 16, reads=[tot], writes=[dec])
                        D(ACT, T["dec_s"][te], dec[:], dec, reads=[dec])

                    slab = load_slab(0, 512)
                    for k, te in enumerate(tiles):
                        ps = proj(slab, xnT, k)
                        E = Es[k]
                        QK = QKr.next(); kst = kstr.next()
                        qa = bc(ps[:, 0:256], [(0, 2), (1, 256)])
                        ka = bc(ps[:, 256:512], [(0, 2), (1, 256)])
                        I(DVE, nc.vector.scalar_tensor_tensor, QK[:, 0:2, :], qa, 0.125,
                          E[:, 0, :].rearrange("p (a b) -> p a b", a=2), ALU.mult, ALU.mult,
                          reads=[ps, E], writes=[QK])
                        I(DVE, nc.vector.tensor_tensor, QK[:, 2:4, :], ka, E[:, 1, :].rearrange("p (a b) -> p a b", a=2),
                          ALU.mult, reads=[ps, E], writes=[QK])
                        I(DVE, nc.vector.tensor_tensor, kst[:], ka, E[:, 2, :].rearrange("p (a b) -> p a b", a=2),
                          ALU.mult, reads=[ps, E], writes=[kst])
                        D(POOL, T["kst_s"][te * 128:(te + 1) * 128, :], kst[:].rearrange("p a b -> p (a b)"), kst, reads=[kst])
                        def tail_qk(QK=QK, te=te):
                            pT = pTs.next()
                            QKf = QK[:].rearrange("p a b -> p (a b)")
                            for b8 in range(8):
                                I(PE, nc.tensor.transpose, pT[:, b8 * 128:(b8 + 1) * 128], QKf[:, b8 * 128:(b8 + 1) * 128],
                                  identb[:], reads=[QK, identb], writes=[pT])
                            GT = GTr.next()
                            I(ACT, nc.scalar.copy, GT[:].rearrange("p a b -> p (a b)"), pT[:], reads=[pT], writes=[GT])
                            D(ACT, T["glaT_s"][te], GT[:].rearrange("p a b -> p (a b)"), GT, reads=[GT])
                        defer(tail_qk)

                    slab = load_slab(512, 512)
                    for k, te in enumerate(tiles):
                        ps = proj(slab, xnT, k)
                        va = var.next()
                        I(ACT, nc.scalar.copy, va[:], ps[:], reads=[ps], writes=[va])
                        D(ACT, T["va_s"][te * 128:(te + 1) * 128, :], va[:], va, reads=[va])

                    if own:
                        slab = load_slab(1056, 512)
                        for k, te in enumerate(tiles):
                            ps = proj(slab, xnT, k)
                            rs = rsr.next()
                            I(ACT, nc.scalar.activation, rs[:], ps[:], AF.Silu, reads=[ps], writes=[rs])
                            to = te - HT
                            D(ACT, T["rs_s"][to * 128:(to + 1) * 128, :], rs[:], rs, reads=[rs])

                    for which in ("q", "k"):
                        if which == "q" and not own:
                            continue
                        base = 1568 if which == "q" else 3104
                        g_b = gq_b if which == "q" else gk_b
                        for hg in range(3):
                            slab = load_slab(base + hg * 512, 512)
                            for k, te in enumerate(tiles):
                                ps = proj(slab, xnT, k)
                                ps3 = ps[:].rearrange("p (h d) -> p h d", h=4)
                                sq = sqr.next(); ss4 = ss4r.next()
                                I(ACT, nc.scalar.activation, sq[:], ps[:], AF.Square, reads=[ps], writes=[sq])
                                I(DVE, nc.vector.tensor_reduce, ss4[:], sq[:].rearrange("p (h d) -> p h d", h=4), AX.X, ALU.add,
                                  reads=[sq], writes=[ss4])
                                rstd_from_ss(ss4[:], 128.0, ss4, ss4, ss4[:])
                                qn = qnr.next(); qr = qrr.next(); qsm = qsmr.next(); rt = rtr.next()
                                I(DVE, nc.vector.tensor_tensor, qn[:], ps3, bc(ss4[:], [(1, 4), (0, 128)]), ALU.mult,
                                  reads=[ps, ss4], writes=[qn])
                                I(DVE, nc.vector.tensor_tensor, qr[:], qn[:], bc(g_b[:], [(0, 4), (1, 128)]), ALU.mult,
                                  reads=[qn, g_b], writes=[qr])
                                tb = tabs[k]
                                wi_ = 0 if which == "q" else 1
                                TA = tb[:, wi_, 0, :]; TB = tb[:, wi_, 1, :]
                                I(DVE, nc.vector.tensor_tensor, qsm[:], qn[:, :, 0:32], bc(TA, [(0, 4), (1, 32)]), ALU.mult,
                                  reads=[qn, tb], writes=[qsm])
                                I(POOL, nc.gpsimd.tensor_tensor, rt[:, :, 0, :], qn[:, :, 16:32], bc(TB[:, 0:16], [(0, 4), (1, 16)]), ALU.mult,
                                  reads=[qn, tb], writes=[rt])
                                I(POOL, nc.gpsimd.tensor_tensor, rt[:, :, 1, :], qn[:, :, 0:16], bc(TB[:, 16:32], [(0, 4), (1, 16)]), ALU.mult,
                                  reads=[qn, tb], writes=[rt])
                                I(DVE, nc.vector.tensor_tensor, qr[:, :, 0:32].rearrange("p h (a b) -> p h a b", a=2),
                                  qsm[:].rearrange("p h (a b) -> p h a b", a=2), rt[:, :, 0:2, :], ALU.add,
                                  reads=[qsm, rt], writes=[qr])
                                def tail_d(qr=qr, te=te, hg=hg, which=which):
                                    pT = pTs.next()
                                    for h in range(4):
                                        I(PE, nc.tensor.transpose, pT[:, h * 128:(h + 1) * 128], qr[:, h, :], identb[:],
                                          reads=[qr, identb], writes=[pT])
                                    TT = TTr.next()
                                    I(ACT, nc.scalar.copy, TT[:].rearrange("p a b -> p (a b)"), pT[:, 0:512], reads=[pT], writes=[TT])
                                    if which == "q":
                                        to = te - HT
                                        dst = T["qdT_s"][hg * 4:(hg + 1) * 4, :, to * 128:(to + 1) * 128]
                                    else:
                                        dst = T["kdT_s"][hg * 4:(hg + 1) * 4, :, te * 128:(te + 1) * 128]
                                    D(ACT, dst.rearrange("h p t -> p h t"), TT[:], TT, reads=[TT])
                                defer(tail_d)

                    for hg in range(3):
                        slab = load_slab(4640 + hg * 512, 512)
                        for k, te in enumerate(tiles):
                            ps = proj(slab, xnT, k)
                            vd = vdr.next()
                            I(ACT, nc.scalar.copy, vd[:, :, 0:128], ps[:].rearrange("p (h d) -> p h d", h=4),
                              reads=[ps], writes=[vd])
                            I(DVE, nc.vector.tensor_copy, vd[:, :, 128:129], bc(auxs[k][:, 32:33], [(0, 4), (1, 1)]),
                              reads=[auxs[k]], writes=[vd])
                            D(POOL, T["vd_s"][te * 128:(te + 1) * 128, hg * 516:(hg + 1) * 516], vd[:].rearrange("p h d -> p (h d)"),
                              vd, reads=[vd])
                flush()
                fw.end_phase()

        if "gla" in phases:
            with ExitStack() as es:
                og_b = fw.sbuf(es, "og_b", [128, 512], F32)
                D(SP, og_b[:], T["out_g"].partition_broadcast(128), og_b, writes=[og_b])
                st_f = fw.sbuf(es, "st_f", [128, 2, 128], F32)
                st_fb = fw.sbuf(es, "st_fb", [128, 2, 128], BF16)
                st_b = fw.sbuf(es, "st_b", [128, 2, 128], F32)
                st_bb = fw.sbuf(es, "st_bb", [128, 2, 128], BF16)
                SR = fw.sbuf(es, "SR", [128, NT_OWN, 2, 128], BF16)
                for b in (st_f, st_fb, st_b, st_bb):
                    I(DVE, nc.vector.memset, b[:], 0.0, writes=[b])
                kstr = Ring(fw, es, "gkst", 3, [128, 512], BF16)
                var = Ring(fw, es, "gva", 3, [128, 512], BF16)
                decr = Ring(fw, es, "gdec", 3, [128, 4], F32)
                GTr = Ring(fw, es, "gGT", 2, [128, 8, 128], BF16)
                rsr = Ring(fw, es, "grs", 2, [128, 512], BF16)
                Smr = Ring(fw, es, "Sm", 2, [128, 8, 128], BF16)
                sqr = Ring(fw, es, "gsq", 2, [128, 512], F32)
                ss4r = Ring(fw, es, "gss4", 2, [128, 4], F32)
                onr = Ring(fw, es, "gon", 2, [128, 512], F32)
                mixr = Ring(fw, es, "gmix", 2, [128, 512], BF16)
                inc_r = Ring(fw, es, "inc", 2, [128, 2, 128], F32, psum=True)
                S_r = Ring(fw, es, "Sps", 1, [128, 8, 128], F32, psum=True)
                o_r = Ring(fw, es, "ops", 2, [128, 4, 128], F32, psum=True)
                dummy = fw.psum(es, "dummy", [128, 128], F32)

                def load_tile(te, need_out):
                    kst = kstr.next(); va = var.next(); dec = decr.next()
                    D(SP, kst[:], T["kst_s"][te * 128:(te + 1) * 128, :], kst, writes=[kst])
                    D(SP, va[:], T["va_s"][te * 128:(te + 1) * 128, :], va, writes=[va])
                    D(SP, dec[:], T["dec_s"][te], dec, writes=[dec])
                    GT = rs = None
                    if need_out:
                        GT = GTr.next(); rs = rsr.next()
                        to = te - HT
                        D(SP, GT[:].rearrange("p a b -> p (a b)"), T["glaT_s"][te], GT, writes=[GT])
                        D(SP, rs[:], T["rs_s"][to * 128:(to + 1) * 128, :], rs, writes=[rs])
                    return kst, va, dec, GT, rs

                def state_update(st, stb, kst, va, dec, d):
                    inc = inc_r.next()
                    for h in range(4):
                        c, hh = h // 2, h % 2
                        I(PE, nc.tensor.matmul, inc[hh * 64:(hh + 1) * 64, c, :],
                          kst[:, d * 256 + h * 64:d * 256 + (h + 1) * 64], va[:, h * 128:(h + 1) * 128],
                          start=True, stop=True, reads=[kst, va], writes=[inc])
                    for c in range(2):
                        I(DVE, nc.vector.scalar_tensor_tensor, st[:, c, :], st[:, c, :], dec[:, 2 * d + c:2 * d + c + 1],
                          inc[:, c, :], ALU.mult, ALU.add, reads=[st, dec, inc], writes=[st])
                    I(ACT, nc.scalar.copy, stb[:], st[:], reads=[st], writes=[stb])

                for te in range(NT_EXT - 1, HT - 1, -1):
                    kst, va, dec, _, _ = load_tile(te, False)
                    if te < HT + NT_OWN:
                        I(ACT, nc.scalar.copy, SR[:, te - HT, :, :], st_bb[:], reads=[st_bb], writes=[SR])
                    state_update(st_b, st_bb, kst, va, dec, 1)

                for te in range(0, HT + NT_OWN):
                    need = te >= HT
                    kst, va, dec, GT, rs = load_tile(te, need)
                    if need:
                        to = te - HT
                        Sps = S_r.next()
                        for hh in range(2):
                            if hh == 1:
                                I(PE, nc.tensor.matmul, dummy[:], GT[:, 0, :], GT[:, 1, :], start=True, stop=True,
                                  reads=[GT], writes=[dummy])
                            for d in range(2):
                                for c in range(2):
                                    h = 2 * c + hh
                                    kT = GT[hh * 64:(hh + 1) * 64, (2 + d) * 2 + c, :]
                                    qT = GT[hh * 64:(hh + 1) * 64, d * 2 + c, :]
                                    I(PE, nc.tensor.matmul, Sps[:, d * 4 + h, :], kT, qT, start=True, stop=True,
                                      reads=[GT], writes=[Sps])
                        Sm = Smr.next()
                        for d in range(2):
                            I(DVE, nc.vector.tensor_tensor, Sm[:, d * 4:(d + 1) * 4, :], Sps[:, d * 4:(d + 1) * 4, :],
                              bc(trib[:, d * 128:(d + 1) * 128], [(0, 4), (1, 128)]), ALU.mult,
                              reads=[Sps, trib], writes=[Sm])
                        ops = o_r.next()
                        for h in range(4):
                            c, hh = h // 2, h % 2
                            vh = va[:, h * 128:(h + 1) * 128]
                            I(PE, nc.tensor.matmul, ops[:, h, :], Sm[:, h, :], vh, start=True, stop=False,
                              reads=[Sm, va], writes=[ops])
                            I(PE, nc.tensor.matmul, ops[:, h, :], Sm[:, 4 + h, :], vh, start=False, stop=False,
                              reads=[Sm, va], writes=[ops])
                            I(PE, nc.tensor.matmul, ops[:, h, :], GT[hh * 64:(hh + 1) * 64, 0 * 2 + c, :],
                              st_fb[hh * 64:(hh + 1) * 64, c, :], start=False, stop=False,
                              reads=[GT, st_fb], writes=[ops])
                            I(PE, nc.tensor.matmul, ops[:, h, :], GT[hh * 64:(hh + 1) * 64, 1 * 2 + c, :],
                              SR[hh * 64:(hh + 1) * 64, to, c, :], start=False, stop=True,
                              reads=[GT, SR], writes=[ops])
                        opf = ops[:].rearrange("p h d -> p (h d)")
                        sq = sqr.next(); ss4 = ss4r.next(); on = onr.next(); mix = mixr.next()
                        I(ACT, nc.scalar.activation, sq[:], opf, AF.Square, reads=[ops], writes=[sq])
                        I(DVE, nc.vector.tensor_reduce, ss4[:], sq[:].rearrange("p (h d) -> p h d", h=4), AX.X, ALU.add,
                          reads=[sq], writes=[ss4])
                        rstd_from_ss(ss4[:], 128.0, ss4, ss4, ss4[:])
                        I(DVE, nc.vector.tensor_tensor, on[:].rearrange("p (h d) -> p h d", h=4), ops[:],
                          bc(ss4[:], [(1, 4), (0, 128)]), ALU.mult, reads=[ops, ss4], writes=[on])
                        I(DVE, nc.vector.tensor_tensor, on[:], on[:], og_b[:], ALU.mult, reads=[on, og_b], writes=[on])
                        I(DVE, nc.vector.tensor_tensor, mix[:], on[:], rs[:], ALU.mult, reads=[on, rs], writes=[mix])
                        D(SP, T["mixed_s"][to * 128:(to + 1) * 128, 0:512], mix[:], mix, reads=[mix])
                    state_update(st_f, st_fb, kst, va, dec, 0)
                fw.end_phase()

        if "dil" in phases:
            with ExitStack() as es:
                cmaskb = fw.sbuf(es, "cmaskb", [128, 17 * 128], BF16)
                I(DVE, nc.vector.tensor_copy, cmaskb[:], cst[:, C_CMASK:C_CMASK + 17 * 128], reads=[cst], writes=[cmaskb])
                kTr = Ring(fw, es, "dkT", 2, [128, NEXT], BF16)
                vr = Ring(fw, es, "dv", 2, [128, NT_EXT, 129], BF16)
                qTr = Ring(fw, es, "dqT", 2, [128, NTOK], BF16)
                exr = Ring(fw, es, "dex", 4, [128, 512], BF16)
                pmr = Ring(fw, es, "dpm", 5, [128, 512], BF16)
                rdr = Ring(fw, es, "drd", 3, [128, 1], F32)
                oor = Ring(fw, es, "doo", 3, [128, 128], BF16)
                S_r = Ring(fw, es, "dS", 3, [128, 512], F32, psum=True)
                o_r = Ring(fw, es, "dO", 2, [128, 129], F32, psum=True)
                tg = tables_gen(es) if "tables" in phases else None
                dstep = 0
                vd_v = T["vd_s"].rearrange("(t p) (h d) -> h p t d", p=128, h=12)
                def dil_load(h):
                    kT = kTr.next(); v = vr.next(); qT = qTr.next()
                    D(SP, kT[:], T["kdT_s"][h], kT, writes=[kT])
                    D(SP, qT[:], T["qdT_s"][h], qT, writes=[qT])
                    for v4 in range(4):
                        D(SP, v[:, v4 * 12:(v4 + 1) * 12, :], vd_v[h][:, v4 * 12:(v4 + 1) * 12, :], v, writes=[v])
                    return kT, v, qT
                nxt = dil_load(0)
                dpend = []
                for h in range(12):
                    kT, v, qT = nxt
                    while dpend:
                        dpend.pop(0)()
                    if h + 1 < 12:
                        nxt = dil_load(h + 1)
                    for qi in range(NT_OWN):
                        dstep += 1
                        if tg is not None and dstep % 3 == 0:
                            next(tg, None)
                        ops = o_r.next()
                        kts = list(range(qi, qi + 17))
                        for g0 in range(0, 17, 4):
                            grp = kts[g0:g0 + 4]
                            n = len(grp)
                            Sps = S_r.next()
                            for k, kt in enumerate(grp):
                                I(PE, nc.tensor.matmul, Sps[:, k * 128:(k + 1) * 128], kT[:, kt * 128:(kt + 1) * 128],
                                  qT[:, qi * 128:(qi + 1) * 128], start=True, stop=True, reads=[kT, qT], writes=[Sps])
                            ex = exr.next(); pm = pmr.next()
                            I(ACT, nc.scalar.activation, ex[:, 0:n * 128], Sps[:, 0:n * 128], AF.Exp, reads=[Sps], writes=[ex])
                            I(DVE, nc.vector.tensor_tensor, pm[:, 0:n * 128], ex[:, 0:n * 128],
                              cmaskb[:, g0 * 128:(g0 + n) * 128], ALU.mult, reads=[ex, cmaskb], writes=[pm])

                            def pv(grp=grp, g0=g0, pm=pm, ops=ops, v=v, qi=qi, h=h):
                                for k, kt in enumerate(grp):
                                    I(PE, nc.tensor.matmul, ops[:, 0:129], pm[:, k * 128:(k + 1) * 128], v[:, kt, :],
                                      start=(g0 + k == 0), stop=(g0 + k == 16), reads=[pm, v], writes=[ops])
                                if g0 + len(grp) == 17:
                                    rd = rdr.next(); oo = oor.next()
                                    I(DVE, nc.vector.reciprocal, rd[:], ops[:, 128:129], reads=[ops], writes=[rd])
                                    I(DVE, nc.vector.tensor_scalar, oo[:], ops[:, 0:128], rd[:, 0:1], None, ALU.mult,
                                      reads=[ops, rd], writes=[oo])
                                    D(POOL, T["mixed_s"][qi * 128:(qi + 1) * 128, 512 + h * 128:512 + (h + 1) * 128], oo[:], oo,
                                      reads=[oo])
                            dpend.append(pv)
                            while len(dpend) > 2:
                                dpend.pop(0)()
                while dpend:
                    dpend.pop(0)()
                if tg is not None:
                    for _ in tg:
                        pass
                fw.end_phase()

        if "wout" in phases:
            with ExitStack() as es:
                Wo = fw.sbuf(es, "Wo", [128, 16, 2048], BF16)
                g2col = fw.sbuf(es, "g2col", [128, 16], F32)
                D(SP, g2col[:], T["g2"], g2col, writes=[g2col])
                with ExitStack() as es2:
                    wst = Ring(fw, es2, "wst2", 2, [128, 2048], F32)
                    for c in range(16):
                        s_ = wst.next()
                        D(SP, s_[:], T["w_out"][c * 128:(c + 1) * 128, :], s_, writes=[s_])
                        if c % 2 == 0:
                            I(ACT, nc.scalar.copy, Wo[:, c, :], s_[:], reads=[s_], writes=[Wo])
                        else:
                            I(DVE, nc.vector.tensor_copy, Wo[:, c, :], s_[:], reads=[s_], writes=[Wo])
                    fw.barrier()
                mxr = Ring(fw, es, "mx", 3, [128, 2048], BF16)
                mTr = Ring(fw, es, "mT", 3, [128, 16, 128], BF16)
                xr = Ring(fw, es, "x5", 3, [128, 2048], F32)
                ssr = Ring(fw, es, "ss5", 3, [128, 1], F32)
                xb2r = Ring(fw, es, "xb2", 2, [128, 2048], BF16)
                xnTr = Ring(fw, es, "xn2T", 2, [128, 16, 128], BF16)
                pTx = Ring(fw, es, "pT5", 2, [128, 2048], BF16, psum=True)
                main = Ring(fw, es, "mp5", 4, [128, 512], F32, psum=True)
                st = {}

                def stepA(to):
                    te = to + HT
                    mx = mxr.next(); x = xr.next()
                    D(SP, mx[:], T["mixed_s"][to * 128:(to + 1) * 128, :], mx, writes=[mx])
                    D(SP, x[:], T["xe"][te * 128:(te + 1) * 128, :], x, writes=[x])
                    pT = pTx.next()
                    for c in range(16):
                        I(PE, nc.tensor.transpose, pT[:, c * 128:(c + 1) * 128], mx[:, c * 128:(c + 1) * 128], identb[:],
                          reads=[mx, identb], writes=[pT])
                    mT = mTr.next()
                    I(ACT, nc.scalar.copy, mT[:].rearrange("p a b -> p (a b)"), pT[:], reads=[pT], writes=[mT])
                    st[to] = (mT, x)

                def stepB(to):
                    mT, x1 = st[to]
                    for q in range(4):
                        ps = main.next()
                        for c in range(16):
                            I(PE, nc.tensor.matmul, ps[:], mT[:, c, :], Wo[:, c, q * 512:(q + 1) * 512],
                              start=(c == 0), stop=(c == 15), reads=[mT, Wo], writes=[ps])
                        I(DVE, nc.vector.tensor_tensor, x1[:, q * 512:(q + 1) * 512], ps[:], x1[:, q * 512:(q + 1) * 512], ALU.add,
                          reads=[ps, x1], writes=[x1])
                    D(ACT, T["x1_s"][to * 128:(to + 1) * 128, :], x1[:], x1, reads=[x1])
                    ss = ssr.next()
                    xb2 = xb2r.next()
                    I(ACT, nc.scalar.activation, xb2[:], x1[:], AF.Square, accum_out=ss[:, 0:1], reads=[x1], writes=[xb2, ss])
                    rstd_from_ss(ss[:, 0:1], 2048.0, ss, ss, ss[:, 0:1])
                    I(ACT, nc.scalar.activation, xb2[:], x1[:], AF.Copy, scale=ss[:, 0:1], reads=[x1, ss], writes=[xb2])
                    st[to] = (xb2,)

                def stepC(to):
                    (xb2,) = st.pop(to)
                    pT = pTx.next()
                    for c in range(16):
                        I(PE, nc.tensor.transpose, pT[:, c * 128:(c + 1) * 128], xb2[:, c * 128:(c + 1) * 128], identb[:],
                          reads=[xb2, identb], writes=[pT])
                    xnT = xnTr.next()
                    I(DVE, nc.vector.tensor_tensor, xnT[:], pT[:].rearrange("p (c t) -> p c t", c=16),
                      bc(g2col[:], [(1, 16), (0, 128)]), ALU.mult, reads=[pT, g2col], writes=[xnT])
                    D(ACT, T["xn2T_s"][:, :, to * 128:(to + 1) * 128], xnT[:], xnT, reads=[xnT])

                for n in range(NT_OWN + 2):
                    if n < NT_OWN:
                        stepA(n)
                    if 0 <= n - 1 < NT_OWN:
                        stepB(n - 1)
                    if 0 <= n - 2 < NT_OWN:
                        stepC(n - 2)
                fw.end_phase()

            with ExitStack() as es:
                Wq = fw.sbuf(es, "Wq", [128, 16, 2048], BF16)
                KT = fw.sbuf(es, "KT", [128, 16, 128], F32)
                with ExitStack() as es2:
                    wst = Ring(fw, es2, "wst3", 2, [128, 2048], F32)
                    for c in range(16):
                        s_ = wst.next()
                        D(SP, s_[:], T["wq"][c * 128:(c + 1) * 128, :], s_, writes=[s_])
                        if c % 2 == 0:
                            I(ACT, nc.scalar.copy, Wq[:, c, :], s_[:], reads=[s_], writes=[Wq])
                        else:
                            I(DVE, nc.vector.tensor_copy, Wq[:, c, :], s_[:], reads=[s_], writes=[Wq])
                    kps = fw.psum(es2, "kps", [128, 512], F32)
                    for g4 in range(4):
                        s_ = wst.next()
                        D(SP, s_[:, 0:512].rearrange("p (a b) -> p a b", a=4),
                          T["subk"][g4 * 4:(g4 + 1) * 4].rearrange("a k d -> k a d"), s_, writes=[s_])
                        for a_ in range(4):
                            I(PE, nc.tensor.transpose, kps[:, a_ * 128:(a_ + 1) * 128], s_[:, a_ * 128:(a_ + 1) * 128],
                              identf[:, C_IDENT:C_IDENT + 128], reads=[s_, cst], writes=[kps])
                        I(ACT, nc.scalar.copy, KT[:, g4 * 4:(g4 + 1) * 4, :].rearrange("p a b -> p (a b)"), kps[:],
                          reads=[kps], writes=[KT])
                    fw.barrier()
                xn4r = Ring(fw, es, "xn4", 2, [128, 16, 512], BF16)
                qryTr = Ring(fw, es, "qryT", 1, [128, 16, 512], F32)
                scr = Ring(fw, es, "sc", 2, [128, 16, 128], F32)
                topv = fw.sbuf(es, "topv", [128, 16, 16], F32)
                topi = fw.sbuf(es, "topi", [128, 16, 16], U32)
                topif_r = Ring(fw, es, "topif", 2, [128, 16, 16], F32)
                wkA = fw.sbuf(es, "wkA", [128, 128], F32)
                wkB = fw.sbuf(es, "wkB", [128, 128], F32)
                cand = fw.sbuf(es, "cand", [128, 8, 16, 16], F32)
                wk2A = fw.sbuf(es, "wk2A", [128, 256], F32)
                wk2B = fw.sbuf(es, "wk2B", [128, 256], F32)
                tvb = [Buf(f"tv{i}") for i in range(16)]
                tib = [Buf(f"ti{i}") for i in range(16)]
                cmb = [Buf(f"cm{i}") for i in range(8)]
                cm = fw.sbuf(es, "cm", [128, 8, 16], F32)
                cpos = fw.sbuf(es, "cpos", [128, 8, 16], U32)
                cpb = [Buf(f"cp{i}") for i in range(8)]
                abu = fw.sbuf(es, "abu", [128, 2, 8, 16], U32)
                ab = fw.sbuf(es, "ab", [128, 2, 8, 16], F32)
                eq = fw.sbuf(es, "eq", [128, 8, 16, 16], F32)
                gts = fw.sbuf(es, "gts", [128, 8, 16], F32)
                R_r = Ring(fw, es, "R3", 2, [128, 3, 128], F32)
                Zs = fw.sbuf(es, "Zs", [128, 8], F32)
                main = Ring(fw, es, "mp6", 6, [128, 512], F32, psum=True)
                for g in range(NT_OWN // 4):
                    xn4 = xn4r.next()
                    D(SP, xn4[:], T["xn2T_s"][:, :, g * 512:(g + 1) * 512], xn4, writes=[xn4])
                    qryT = qryTr.next()
                    for hc in range(16):
                        ps = main.next()
                        for c in range(16):
                            I(PE, nc.tensor.matmul, ps[:], Wq[:, c, hc * 128:(hc + 1) * 128], xn4[:, c, :],
                              start=(c == 0), stop=(c == 15), reads=[Wq, xn4], writes=[ps])
                        I(ACT, nc.scalar.copy, qryT[:, hc, :], ps[:], reads=[ps], writes=[qryT])
                    for k in range(4):
                        to = g * 4 + k
                        sc = scr.next()
                        for g4 in range(4):
                            ps = main.next()
                            for a_ in range(4):
                                hc = g4 * 4 + a_
                                I(PE, nc.tensor.matmul, ps[:, a_ * 128:(a_ + 1) * 128], qryT[:, hc, k * 128:(k + 1) * 128], KT[:, hc, :],
                                  start=True, stop=True, reads=[qryT, KT], writes=[ps])
                            I(ACT, nc.scalar.copy, sc[:, g4 * 4:(g4 + 1) * 4, :].rearrange("p a b -> p (a b)"), ps[:],
                              reads=[ps], writes=[sc])
                        for hp in range(0, 16, 2):
                            pr = [(hp, wkA, tvb[hp], tib[hp]), (hp + 1, wkB, tvb[hp + 1], tib[hp + 1])]
                            for (hc, wk_, tv_, ti_) in pr:
                                I(DVE, nc.vector.max, topv[:, hc, 0:8], sc[:, hc, :], reads=[sc], writes=[tv_])
                            for (hc, wk_, tv_, ti_) in pr:
                                I(DVE, nc.vector.max_index, topi[:, hc, 0:8], topv[:, hc, 0:8], sc[:, hc, :], reads=[sc, tv_], writes=[ti_])
                            for (hc, wk_, tv_, ti_) in pr:
                                I(DVE, nc.vector.match_replace, wk_[:], topv[:, hc, 0:8], sc[:, hc, :], NEG, reads=[sc, tv_], writes=[wk_])
                            for (hc, wk_, tv_, ti_) in pr:
                                I(DVE, nc.vector.max, topv[:, hc, 8:16], wk_[:], reads=[wk_], writes=[tv_])
                            for (hc, wk_, tv_, ti_) in pr:
                                I(DVE, nc.vector.max_index, topi[:, hc, 8:16], topv[:, hc, 8:16], wk_[:], reads=[wk_, tv_], writes=[ti_])
                        topif = topif_r.next()
                        I(DVE, nc.vector.tensor_copy, topif[:], topi[:], reads=tib, writes=[topif])
                        tv = topv[:]
                        v1 = bass.AP(tv.tensor, tv.offset, [list(tv.ap[0]), [32, 8], [1, 16], [0, 16]])
                        v2 = bass.AP(tv.tensor, tv.offset + 16, [list(tv.ap[0]), [32, 8], [0, 16], [1, 16]])
                        I(POOL, nc.gpsimd.tensor_tensor, cand[:], v1, v2, ALU.add, reads=tvb, writes=[cand])
                        for h2 in range(0, 8, 2):
                            pr = [(h2, wk2A, cmb[h2]), (h2 + 1, wk2B, cmb[h2 + 1])]
                            chs = {h: cand[:, h, :, :].rearrange("p a b -> p (a b)") for (h, _, _) in pr}
                            for (h, w2, cb) in pr:
                                I(DVE, nc.vector.max, cm[:, h, 0:8], chs[h], reads=[cand], writes=[cb])
                            for (h, w2, cb) in pr:
                                I(DVE, nc.vector.max_index, cpos[:, h, 0:8], cm[:, h, 0:8], chs[h], reads=[cand, cb], writes=[cpb[h]])
                            for (h, w2, cb) in pr:
                                I(DVE, nc.vector.match_replace, w2[:], cm[:, h, 0:8], chs[h], NEG, reads=[cand, cb], writes=[w2])
                            for (h, w2, cb) in pr:
                                I(DVE, nc.vector.max, cm[:, h, 8:16], w2[:], reads=[w2], writes=[cb])
                            for (h, w2, cb) in pr:
                                I(DVE, nc.vector.max_index, cpos[:, h, 8:16], cm[:, h, 8:16], w2[:], reads=[w2, cb], writes=[cpb[h]])
                        I(DVE, nc.vector.tensor_scalar, abu[:, 0, :, :], cpos[:], 4, None, ALU.logical_shift_right, reads=cpb, writes=[abu])
                        I(DVE, nc.vector.tensor_scalar, abu[:, 1, :, :], cpos[:], 15, None, ALU.bitwise_and, reads=cpb, writes=[abu])
                        I(DVE, nc.vector.tensor_copy, ab[:], abu[:], reads=[abu], writes=[ab])
                        R = R_r.next()
                        tfv = topif[:]
                        io16 = bc(cst[:, C_IOTA:C_IOTA + 16], [(0, 8), (0, 16), (1, 16)])
                        for c_ in range(2):
                            abv = ab[:, c_, :, :]
                            I(DVE, nc.vector.tensor_tensor, eq[:], io16, bc(abv, [(16, 8), (1, 16), (0, 16)]), ALU.is_equal,
                              reads=[cst, ab], writes=[eq])
                            idx_b = bass.AP(tfv.tensor, tfv.offset + 16 * c_, [list(tfv.ap[0]), [32, 8], [0, 16], [1, 16]])
                            I(POOL, nc.gpsimd.tensor_tensor, eq[:], eq[:], idx_b, ALU.mult, reads=[eq, topif], writes=[eq])
                            I(DVE, nc.vector.tensor_reduce, R[:, c_, :].rearrange("p (h r) -> p h r", h=8), eq[:], AX.X, ALU.add,
                              reads=[eq], writes=[R])
                        I(POOL, nc.gpsimd.tensor_tensor, gts[:], cm[:], bc(cm[:, :, 0:1], [(16, 8), (0, 16)]), ALU.subtract,
                          reads=cmb, writes=[gts])
                        I(ACT, nc.scalar.activation, gts[:], gts[:], AF.Exp, reads=[gts], writes=[gts])
                        I(DVE, nc.vector.tensor_reduce, Zs[:], gts[:], AX.X, ALU.add, reads=[gts], writes=[Zs])
                        I(DVE, nc.vector.reciprocal, Zs[:], Zs[:], reads=[Zs], writes=[Zs])
                        I(DVE, nc.vector.tensor_tensor, R[:, 2, :].rearrange("p (h r) -> p h r", h=8), gts[:],
                          bc(Zs[:], [(1, 8), (0, 16)]), ALU.mult, reads=[gts, Zs], writes=[R])
                        D(ACT, T["r3_s"][to * 128:(to + 1) * 128, :], R[:].rearrange("p a b -> p (a b)"), R, reads=[R])
                fw.end_phase()

        if "gmat" in phases:
            with ExitStack() as es:
                Rr = Ring(fw, es, "gR", 3, [128, 3, 128], F32)
                RT_r = Ring(fw, es, "gRT", 2, [128, 3, 128], F32)
                gtb_r = Ring(fw, es, "gtb", 2, [128, 128], BF16)
                RTb_r = Ring(fw, es, "gRTb", 2, [128, 2, 128], BF16)
                iotab = fw.sbuf(es, "iotab", [128, 128], BF16)
                I(DVE, nc.vector.tensor_copy, iotab[:], cst[:, C_IOTA:C_IOTA + 128], reads=[cst], writes=[iotab])
                OI_r = Ring(fw, es, "OI", 2, [128, 64, 128], BF16)
                OJ_r = Ring(fw, es, "OJ", 2, [128, 64, 128], BF16)
                OJg_r = Ring(fw, es, "OJg", 2, [128, 64, 128], BF16)
                Gr = Ring(fw, es, "Gall", 2, [128, 128, 128], BF16)
                pR = fw.psum(es, "pR", [128, 3, 128], F32)
                cps = Ring(fw, es, "cps", 4, [128, 4, 128], F32, psum=True)
                iota = cst[:, C_IOTA:C_IOTA + 128]

                Rl = {}

                def rload(to):
                    R = Rr.next()
                    D(SP, R[:].rearrange("p a b -> p (a b)"), T["r3_s"][to * 128:(to + 1) * 128, :], R, writes=[R])
                    Rl[to] = R

                def front(to):
                    R = Rl.pop(to); RT = RT_r.next(); gtb = gtb_r.next()
                    for c in range(3):
                        I(PE, nc.tensor.transpose, pR[:, c, :], R[:, c, :], identf[:, C_IDENT:C_IDENT + 128],
                          reads=[R, cst], writes=[pR])
                    I(ACT, nc.scalar.copy, RT[:].rearrange("p a b -> p (a b)"), pR[:].rearrange("p a b -> p (a b)"),
                      reads=[pR], writes=[RT])
                    I(ACT, nc.scalar.copy, gtb[:], RT[:, 2, :], reads=[RT], writes=[gtb])
                    RTb = RTb_r.next()
                    I(ACT, nc.scalar.copy, RTb[:].rearrange("p a b -> p (a b)"), RT[:, 0:2, :].rearrange("p a b -> p (a b)"),
                      reads=[RT], writes=[RTb])
                    return RTb, gtb

                def gens(hf, RT, gtb):
                    t0h = hf * 64
                    OI = OI_r.next(); OJ = OJ_r.next(); OJg = OJg_r.next()
                    I(DVE, nc.vector.tensor_tensor, OI[:], bc(iotab[:], [(0, 64), (1, 128)]),
                      bc(RT[:, 0, t0h:t0h + 64], [(1, 64), (0, 128)]), ALU.is_equal, reads=[iotab, RT], writes=[OI])
                    I(DVE, nc.vector.tensor_tensor, OJ[:], bc(iotab[:], [(0, 64), (1, 128)]),
                      bc(RT[:, 1, t0h:t0h + 64], [(1, 64), (0, 128)]), ALU.is_equal, reads=[iotab, RT], writes=[OJ])
                    I(POOL, nc.gpsimd.tensor_tensor, OJg[:], OJ[:], bc(gtb[:, t0h:t0h + 64], [(1, 64), (0, 128)]), ALU.mult,
                      reads=[OJ, gtb], writes=[OJg])
                    return OI, OJg

                def gpart(to, hf, OI, OJg, G, g_b):
                    t0h = hf * 64
                    for t4 in range(16):
                        ps = cps.next()
                        for k in range(4):
                            t = t4 * 4 + k
                            I(PE, nc.tensor.matmul, ps[:, k, :], OI[:, t, :], OJg[:, t, :], start=True, stop=True,
                              reads=[OI, OJg], writes=[ps])
                        gv = G[:]
                        g_out = bass.AP(gv.tensor, gv.offset + t0h + t4 * 4, [list(gv.ap[0]), [128, 128], [1, 4]])
                        ps_jk = ps[:].rearrange("p k j -> p j k")
                        gsl = g_b[hf * 16 + t4]
                        I(ACT, nc.scalar.copy, g_out, ps_jk, reads=[ps], writes=[gsl])
                    if hf == 1:
                        D(SP, T["G_s"][to], G[:].rearrange("p j t -> p (j t)"), G, reads=g_b, writes=[G])

                rload(0); rload(1)
                fr = {0: front(0)}
                gn = {0: gens(0, *fr[0])}
                Gs = {}
                for n in range(2 * NT_OWN):
                    to, hf = n // 2, n % 2
                    if hf == 0:
                        if to + 2 < NT_OWN:
                            rload(to + 2)
                        G = Gr.next()
                        g_b = [Buf("gsl") for _ in range(32)]
                        for gb_ in g_b:
                            gb_.last_w = G.last_w; gb_.readers = list(G.readers)
                        Gs[to] = (G, g_b)
                    if n + 1 < 2 * NT_OWN:
                        to1, hf1 = (n + 1) // 2, (n + 1) % 2
                        if hf1 == 0:
                            fr[to1] = front(to1)
                        gn[n + 1] = gens(hf1, *fr[to1])
                    OI, OJg = gn.pop(n)
                    G, g_b = Gs[to]
                    gpart(to, hf, OI, OJg, G, g_b)
                    if hf == 1:
                        Gs.pop(to); fr.pop(to)
                fw.end_phase()

        if "peer" in phases:
            with ExitStack() as es:
                GP = fw.sbuf(es, "GP", [128, 128, 512], BF16)
                xnT = fw.sbuf(es, "pxnT", [128, 16, 512], BF16)
                dTr = Ring(fw, es, "pdT", 5, [128, 16, 128], BF16)
                actr = Ring(fw, es, "pact", 3, [128, 512], BF16)
                upr = Ring(fw, es, "pup", 4, [128, 4, 512], BF16)
                x1r = Ring(fw, es, "px1", 2, [128, 512], F32)
                outr = Ring(fw, es, "pout", 2, [128, 512], F32)
                bank = Ring(fw, es, "pb", 8, [128, 512], F32, psum=True)
                gp_b = [Buf(f"gp{i}") for i in range(32)]
                fw.phase_bufs.extend(gp_b)
                NP = NTOK // 512

                def gload(ps_, jg, eng):
                    for k in range(4):
                        gsrc = T["G_s"][ps_ * 4 + k].rearrange("p (j t) -> p j t", j=128)
                        D(eng, GP[:, jg * 4:(jg + 1) * 4, k * 128:(k + 1) * 128], gsrc[:, jg * 4:(jg + 1) * 4, :], gp_b[jg],
                          writes=[gp_b[jg]])

                D(SP, xnT[:], T["xn2T_s"][:, :, 0:512], xnT, writes=[xnT])
                for jg in range(32):
                    gload(0, jg, SP if jg % 2 == 0 else POOL)
                for ps_ in range(NP):
                    for j in range(128):
                        dT = dTr.next()
                        D(SP, dT[:].rearrange("p a b -> p (a b)"), T["dT_s"][j], dT, writes=[dT])
                        hp = bank.next()
                        for c in range(16):
                            I(PE, nc.tensor.matmul, hp[:], dT[:, c, :], xnT[:, c, :], start=(c == 0), stop=(c == 15),
                              reads=[dT, xnT], writes=[hp])
                        a = actr.next()
                        I(ACT, nc.scalar.activation, a[:], hp[:], AF.Gelu_apprx_tanh, reads=[hp], writes=[a])
                        I(DVE, nc.vector.tensor_tensor, GP[:, j, :], GP[:, j, :], a[:], ALU.mult, reads=[a, gp_b[j // 4]],
                          writes=[gp_b[j // 4]])
                    if ps_ + 1 < NP:
                        D(SP, xnT[:], T["xn2T_s"][:, :, (ps_ + 1) * 512:(ps_ + 2) * 512], xnT, writes=[xnT])
                    for q in range(4):
                        accs = [bank.next() for _ in range(4)]
                        for jg in range(32):
                            ut = upr.next()
                            D(SP, ut[:], T["up_s"][jg * 4:(jg + 1) * 4, :, q * 512:(q + 1) * 512].rearrange("j p d -> p j d"),
                              ut, writes=[ut])
                            for jj in range(4):
                                j = jg * 4 + jj
                                for k in range(4):
                                    I(PE, nc.tensor.matmul, accs[k][:], GP[:, j, k * 128:(k + 1) * 128], ut[:, jj, :],
                                      start=(j == 0), stop=(j == 127), reads=[gp_b[jg], ut], writes=[accs[k]])
                            if q == 3 and ps_ + 1 < NP:
                                gload(ps_ + 1, jg, POOL)
                        for k in range(4):
                            r0 = ps_ * 512 + k * 128
                            x1 = x1r.next(); o = outr.next()
                            D(SP, x1[:], T["x1_s"][r0:r0 + 128, q * 512:(q + 1) * 512], x1, writes=[x1])
                            I(DVE, nc.vector.tensor_tensor, o[:], accs[k][:], x1[:], ALU.add, reads=[accs[k], x1], writes=[o])
                            D(ACT, T["out"][r0:r0 + 128, q * 512:(q + 1) * 512], o[:], o, reads=[o])
                fw.end_phase()
        fw.barrier()
    return nc, fw


_CACHE = {}


def make_in_maps(inp, ncores=8, phases=PHASES):
    f = lambda a: np.ascontiguousarray(np.asarray(a, dtype=np.float32))
    x = f(inp["x"])
    cst = host_consts()
    shared = {
        "cst": cst,
        "g1": f(inp["norm1_g"][0].reshape(16, 128).T),
        "g2": f(inp["norm2_g"][0].reshape(16, 128).T),
        "w_in": f(inp["w_in"][0]),
        "up_f": f(inp["gla_up_f"][0]), "up_b": f(inp["gla_up_b"][0]),
        "bias_f": f(inp["gla_bias_f"][0].reshape(1, 256)), "bias_b": f(inp["gla_bias_b"][0].reshape(1, 256)),
        "out_g": f(inp["gla_out_g"][0].reshape(1, 512)),
        "gq": f(inp["q_norm_g"][0].reshape(1, 128)), "gk": f(inp["k_norm_g"][0].reshape(1, 128)),
        "w_out": f(inp["w_out"][0]), "wq": f(inp["peer_w_query"][0]),
        "subk": f(inp["peer_sub_keys"][0].reshape(16, 128, 128)),
    }
    if "tables" in phases:
        shared["down"] = f(inp["peer_down"][0])
        shared["up"] = f(inp["peer_up"][0])
    maps = []
    for c in range(ncores):
        b, p = c // 4, c % 4
        xe = np.zeros((NEXT, D_MODEL), np.float32)
        lo = p * NTOK - HALO
        hi = lo + NEXT
        slo, shi = max(lo, 0), min(hi, SEQ)
        xe[slo - lo:shi - lo] = x[b, slo:shi]
        m = dict(shared)
        m["xe"] = xe
        m["aux"] = host_aux(p)
        maps.append(m)
    return maps


def kernel(**inputs):
    if "nc" not in _CACHE:
        _CACHE["nc"] = build_program()[0]
    nc = _CACHE["nc"]
    maps = make_in_maps(inputs)
    res = run_bass_kernel_spmd(nc, maps, core_ids=list(range(8)))
    out = np.zeros((2, SEQ, D_MODEL), np.float32)
    for c in range(8):
        b, p = c // 4, c % 4
        out[b, p * NTOK:(p + 1) * NTOK] = np.asarray(res.results[c]["out"], dtype=np.float32)
    return out
```

```python
import numpy as np
from contextlib import ExitStack
import concourse.bass as bass
import concourse.mybir as mybir
from concourse.bass_utils import run_bass_kernel_spmd

F32 = mybir.dt.float32
BF16 = mybir.dt.bfloat16
U32 = mybir.dt.uint32
AF = mybir.ActivationFunctionType
ALU = mybir.AluOpType
AX = mybir.AxisListType

D_MODEL = 2048
SEQ = 16384
NTOK = 4096
HALO = 1024
NEXT = NTOK + 2 * HALO
NT_EXT = NEXT // 128
NT_OWN = NTOK // 128
HT = HALO // 128
IN_W = 6176
EPS = 1e-6
NEG = -1e30

PHASES = ("w_in", "tables", "stage1", "gla", "dil", "wout", "gmat", "peer")
DEBUG_OUTS = ()


class SemRec:
    def __init__(self, sem):
        self.sem = sem
        self.cnt = 0


class Ev:
    __slots__ = ("rec", "val", "eng")

    def __init__(self, rec, val, eng):
        self.rec = rec
        self.val = val
        self.eng = eng


class Buf:
    def __init__(self, name, t=None):
        self.name = name
        self.t = t
        self.last_w = None
        self.readers = []
        self.dma = None

    def __getitem__(self, k):
        return self.t[k]


class Eng:
    def __init__(self, fw, name, obj, sem):
        self.fw = fw
        self.name = name
        self.obj = obj
        self.rec = SemRec(sem)
        self.waited = {}

    def wait_ev(self, ev):
        rec = ev.rec
        if ev.val is None:
            val = rec.cnt * 16
        else:
            val = ev.val
            if ev.eng is self and (self.name == "pe" or not self.fw.same_engine_sync):
                return
        if val <= 0:
            return
        if self.waited.get(id(rec), 0) >= val:
            return
        self.waited[id(rec)] = val
        self.obj.wait_ge(rec.sem, val)
        self.fw.n_waits += 1


class FW:
    def __init__(self, nc, n_dma_sems=88, same_engine_sync=True):
        self.nc = nc
        self.same_engine_sync = same_engine_sync
        self.n_waits = 0
        self.n_inst = 0
        self.pe = Eng(self, "pe", nc.tensor, nc.alloc_semaphore("sem_pe"))
        self.act = Eng(self, "act", nc.scalar, nc.alloc_semaphore("sem_act"))
        self.dve = Eng(self, "dve", nc.vector, nc.alloc_semaphore("sem_dve"))
        self.pool = Eng(self, "pool", nc.gpsimd, nc.alloc_semaphore("sem_pool"))
        self.sp = Eng(self, "sp", nc.sync, nc.alloc_semaphore("sem_sp"))
        self.engs = [self.pe, self.act, self.dve, self.pool, self.sp]
        self.free_dma = [SemRec(nc.alloc_semaphore(f"sem_d{i}")) for i in range(n_dma_sems)]
        self.all_dma = list(self.free_dma)
        self.phase_bufs = []

    def sbuf(self, es, name, shape, dtype):
        t = es.enter_context(self.nc.sbuf_tensor("sb_" + name, list(shape), dtype))
        b = Buf(name, t)
        self.phase_bufs.append(b)
        return b

    def psum(self, es, name, shape, dtype=F32):
        t = es.enter_context(self.nc.psum_tensor("ps_" + name, list(shape), dtype))
        b = Buf(name, t)
        self.phase_bufs.append(b)
        return b

    def _deps(self, reads, writes):
        deps = []
        for b in reads:
            if b.last_w is not None:
                deps.append(b.last_w)
        for b in writes:
            if b.last_w is not None:
                deps.append(b.last_w)
            deps.extend(b.readers)
        return deps

    def _record(self, ev, reads, writes):
        for b in writes:
            b.last_w = ev
            b.readers = []
        for b in reads:
            if b in writes:
                continue
            b.readers = [r for r in b.readers if r.rec is not ev.rec]
            b.readers.append(ev)

    def I(self, eng, fn, *args, reads=(), writes=(), **kw):
        for ev in self._deps(reads, writes):
            eng.wait_ev(ev)
        inst = fn(*args, **kw)
        eng.rec.cnt += 1
        inst.then_inc(eng.rec.sem, 1)
        self.n_inst += 1
        self._record(Ev(eng.rec, eng.rec.cnt, eng), reads, writes)
        return inst

    def D(self, eng, out, in_, sb, reads=(), writes=(), **kw):
        for ev in self._deps(reads, writes):
            eng.wait_ev(ev)
        if sb.dma is None:
            sb.dma = self.free_dma.pop()
        rec = sb.dma
        inst = eng.obj.dma_start(out=out, in_=in_, **kw)
        inst.then_inc(rec.sem, 16)
        rec.cnt += 1
        self.n_inst += 1
        self._record(Ev(rec, None, eng), reads, writes)
        return inst

    def barrier(self):
        for e in self.engs:
            for o in self.engs:
                if o is e or o.rec.cnt == 0:
                    continue
                e.wait_ev(Ev(o.rec, o.rec.cnt, o))
            for rec in self.all_dma:
                if rec.cnt:
                    e.wait_ev(Ev(rec, None, None))

    def end_phase(self):
        self.barrier()
        for b in self.phase_bufs:
            if b.dma is not None:
                self.free_dma.append(b.dma)
                b.dma = None
        self.phase_bufs = []


class Ring:
    def __init__(self, fw, es, name, n, shape, dtype, psum=False):
        mk = fw.psum if psum else fw.sbuf
        self.bufs = [mk(es, f"{name}{i}", shape, dtype) for i in range(n)]
        self.i = 0

    def next(self):
        b = self.bufs[self.i % len(self.bufs)]
        self.i += 1
        return b


def bc(ap, steps):
    return bass.AP(ap.tensor, ap.offset, [list(ap.ap[0])] + [[s, c] for (s, c) in steps])


C_LINCL, C_USTRICT, C_UINCL, C_LSTRICT, C_IDENT, C_IOTA, C_BM, C_ONES, C_CMASK = [k * 128 for k in range(9)]
C_COLS = 8 * 128 + 17 * 128


def host_consts():
    j = np.arange(128)[:, None]
    i = np.arange(128)[None, :]
    c = np.zeros((128, C_COLS), np.float32)
    c[:, C_LINCL:C_LINCL + 128] = (j <= i)
    c[:, C_USTRICT:C_USTRICT + 128] = (j > i)
    c[:, C_UINCL:C_UINCL + 128] = (j >= i)
    c[:, C_LSTRICT:C_LSTRICT + 128] = (j < i)
    c[:, C_IDENT:C_IDENT + 128] = (j == i)
    c[:, C_IOTA:C_IOTA + 128] = np.broadcast_to(i, (128, 128))
    c[:, C_BM:C_BM + 128] = ((j // 16) == (i // 16))
    c[:, C_ONES:C_ONES + 128] = 1.0
    for t in range(17):
        off = (t - 8) * 128 + j - i
        m = (np.abs(off) <= 64).astype(np.float32)
        m += ((off % 4 == 0) & (np.abs(off) <= 256))
        m += ((off % 16 == 0) & (np.abs(off) <= 1024))
        c[:, C_CMASK + t * 128:C_CMASK + (t + 1) * 128] = m
    return c


def host_aux(p):
    pos = np.arange(NEXT) + p * NTOK - HALO
    half = 16
    inv_freq = (np.float32(500000.0) ** (-np.arange(half, dtype=np.float32) / np.float32(half))).astype(np.float32)
    ang = pos.astype(np.float32)[:, None] * inv_freq[None, :]
    a = np.zeros((NEXT, 33), np.float32)
    a[:, 0:16] = np.cos(ang)
    a[:, 16:32] = np.sin(ang)
    a[:, 32] = ((pos >= 0) & (pos < SEQ))
    return a


def build_program(phases=PHASES, debug_outs=DEBUG_OUTS):
    nc = bass.Bass("TRN2", target_bir_lowering=False)
    fw = FW(nc)
    I, D = fw.I, fw.D
    PE, ACT, DVE, POOL, SP = fw.pe, fw.act, fw.dve, fw.pool, fw.sp
    T = {}

    def din(name, shape, dt=F32):
        T[name] = nc.dram_tensor(name, list(shape), dt, kind="ExternalInput").ap()

    def dscr(name, shape, dt):
        kind = "ExternalOutput" if name in debug_outs else "Internal"
        T[name] = nc.dram_tensor(name, list(shape), dt, kind=kind).ap()

    din("xe", [NEXT, D_MODEL])
    din("cst", [128, C_COLS])
    din("aux", [NEXT, 33])
    din("g1", [128, 16])
    din("g2", [128, 16])
    din("w_in", [D_MODEL, IN_W])
    din("up_f", [16, 256]); din("up_b", [16, 256]); din("bias_f", [1, 256]); din("bias_b", [1, 256])
    din("out_g", [1, 512]); din("gq", [1, 128]); din("gk", [1, 128])
    din("w_out", [D_MODEL, D_MODEL]); din("wq", [D_MODEL, D_MODEL])
    din("subk", [16, 128, 128])
    if "tables" in phases:
        din("down", [128 * 128, D_MODEL]); din("up", [128 * 128, D_MODEL])
    T["out"] = nc.dram_tensor("out", [NTOK, D_MODEL], F32, kind="ExternalOutput").ap()

    dscr("wi_s", [128, 16, IN_W], BF16)
    dscr("qdT_s", [12, 128, NTOK], BF16)
    dscr("kdT_s", [12, 128, NEXT], BF16)
    dscr("vd_s", [NEXT, 12 * 129], BF16)
    dscr("glaT_s", [NT_EXT, 128, 8 * 128], BF16)
    dscr("kst_s", [NEXT, 512], BF16)
    dscr("va_s", [NEXT, 512], BF16)
    dscr("dec_s", [NT_EXT, 128, 4], F32)
    dscr("rs_s", [NTOK, 512], BF16)
    dscr("mixed_s", [NTOK, D_MODEL], BF16)
    dscr("x1_s", [NTOK, D_MODEL], F32)
    dscr("xn2T_s", [128, 16, NTOK], BF16)
    dscr("r3_s", [NTOK, 384], F32)
    dscr("G_s", [NT_OWN, 128, 128 * 128], BF16)
    if "tables" in phases:
        dscr("dT_s", [128, 128, 2048], BF16)
        dscr("up_s", [128, 128, 2048], BF16)

    with ExitStack() as es0:
        cst = fw.sbuf(es0, "cst", [128, C_COLS], F32)
        identb = fw.sbuf(es0, "identb", [128, 128], BF16)
        trib = fw.sbuf(es0, "trib", [128, 256], BF16)
        D(SP, cst[:], T["cst"], cst, writes=[cst])
        I(DVE, nc.vector.tensor_copy, identb[:], cst[:, C_IDENT:C_IDENT + 128], reads=[cst], writes=[identb])
        I(DVE, nc.vector.tensor_copy, trib[:], cst[:, C_LINCL:C_LINCL + 256], reads=[cst], writes=[trib])
        identf = cst
        fw.end_phase()
        fw.phase_bufs = []

        def rstd_from_ss(ss_ap, n, ss_buf, out_buf, out_ap, eng_recip=DVE):
            I(ACT, nc.scalar.activation, out_ap, ss_ap, AF.Sqrt, scale=1.0 / n, bias=eps_col[:, 0:1],
              reads=[ss_buf, eps_col], writes=[out_buf])
            I(eng_recip, nc.vector.reciprocal, out_ap, out_ap, reads=[out_buf], writes=[out_buf])

        eps_col = fw.sbuf(es0, "eps_col", [128, 1], F32)
        I(DVE, nc.vector.memset, eps_col[:], EPS, writes=[eps_col])

        if "w_in" in phases:
            with ExitStack() as es:
                st = Ring(fw, es, "wst", 2, [128, IN_W], F32)
                bf = Ring(fw, es, "wbf", 2, [128, IN_W], BF16)
                for c in range(16):
                    s = st.next(); b = bf.next()
                    D(SP, s[:], T["w_in"][c * 128:(c + 1) * 128, :], s, writes=[s])
                    if c % 2 == 0:
                        I(ACT, nc.scalar.copy, b[:], s[:], reads=[s], writes=[b])
                    else:
                        I(DVE, nc.vector.tensor_copy, b[:], s[:], reads=[s], writes=[b])
                    D(SP, T["wi_s"][:, c, :], b[:], b, reads=[b])
                fw.end_phase()

        def tables_gen(es):
            dn_r = Ring(fw, es, "dn", 3, [128, 2048], F32)
            up_r = Ring(fw, es, "upf", 3, [128, 2048], F32)
            dnb_r = Ring(fw, es, "dnb", 3, [128, 2048], BF16)
            upb_r = Ring(fw, es, "upb", 2, [128, 2048], BF16)
            dT_r = Ring(fw, es, "dTt", 2, [128, 2048], BF16)
            pT_r = Ring(fw, es, "pTt", 1, [128, 2048], BF16, psum=True)
            down_v = T["down"].rearrange("(i j) d -> j i d", j=128)
            up_v = T["up"].rearrange("(i j) d -> j i d", j=128)
            loaded = {}

            def tload(j):
                dn = dn_r.next(); upf = up_r.next()
                D(SP, dn[:], down_v[j], dn, writes=[dn])
                D(SP, upf[:], up_v[j], upf, writes=[upf])
                loaded[j] = (dn, upf)

            tload(0); tload(1)
            prev = None
            for j in range(129):
                cur = None
                if j < 128:
                    if j + 2 < 128:
                        tload(j + 2)
                    dn, upf = loaded.pop(j)
                    dnb = dnb_r.next(); upb = upb_r.next()
                    I(POOL, nc.gpsimd.tensor_copy, dnb[:], dn[:], reads=[dn], writes=[dnb])
                    I(ACT, nc.scalar.copy, upb[:], upf[:], reads=[upf], writes=[upb])
                    D(POOL, T["up_s"][j], upb[:], upb, reads=[upb])
                    cur = (j, dnb)
                if prev is not None:
                    pj, pdnb = prev
                    pT = pT_r.next()
                    for c in range(16):
                        I(PE, nc.tensor.transpose, pT[:, c * 128:(c + 1) * 128], pdnb[:, c * 128:(c + 1) * 128], identb[:],
                          reads=[pdnb, identb], writes=[pT])
                    dT = dT_r.next()
                    I(ACT, nc.scalar.copy, dT[:], pT[:], reads=[pT], writes=[dT])
                    D(POOL, T["dT_s"][pj], dT[:], dT, reads=[dT])
                prev = cur
                yield

        if "tables" in phases and "dil" not in phases:
            with ExitStack() as es:
                for _ in tables_gen(es):
                    pass
                fw.end_phase()

        if "stage1" in phases:
            with ExitStack() as es:
                g1col = fw.sbuf(es, "g1col", [128, 16], F32)
                gq_b = fw.sbuf(es, "gq_b", [128, 128], F32)
                gk_b = fw.sbuf(es, "gk_b", [128, 128], F32)
                upb = fw.sbuf(es, "upbx", [33, 512], F32)
                D(SP, g1col[:], T["g1"], g1col, writes=[g1col])
                D(SP, gq_b[:], T["gq"].partition_broadcast(128), gq_b, writes=[gq_b])
                D(SP, gk_b[:], T["gk"].partition_broadcast(128), gk_b, writes=[gk_b])
                I(ACT, nc.scalar.mul, gq_b[:], gq_b[:], 128.0 ** -0.5, reads=[gq_b], writes=[gq_b])
                I(DVE, nc.vector.memset, upb[:], 0.0, writes=[upb])
                D(SP, upb[0:16, 0:256], T["up_f"], upb, writes=[upb])
                D(SP, upb[16:32, 256:512], T["up_b"], upb, writes=[upb])
                D(SP, upb[32:33, 0:256], T["bias_f"], upb, writes=[upb])
                D(SP, upb[32:33, 256:512], T["bias_b"], upb, writes=[upb])

                xr = Ring(fw, es, "x", 2, [128, 2048], F32)
                junk = fw.sbuf(es, "junk", [128, 2048], BF16)
                xbr = Ring(fw, es, "xb", 2, [128, 2048], BF16)
                ssr = Ring(fw, es, "ss", 4, [128, 1], F32)
                xnTr = Ring(fw, es, "xnT", 2, [128, 16, 512], BF16)
                slabr = Ring(fw, es, "slab", 2, [128, 16, 512], BF16)
                gTr = Ring(fw, es, "gT", 2, [33, 512], F32)
                for b in gTr.bufs:
                    I(DVE, nc.vector.memset, b[32:33, :], 1.0, writes=[b])
                auxr = Ring(fw, es, "aux", 8, [128, 33], F32)
                tabr = Ring(fw, es, "tab", 8, [128, 2, 2, 32], F32)
                Er = Ring(fw, es, "E", 4, [128, 3, 512], F32)
                vdr = Ring(fw, es, "vd", 3, [128, 4, 129], BF16)
                sqr = Ring(fw, es, "sq", 2, [128, 512], F32)
                ss4r = Ring(fw, es, "ss4", 2, [128, 4], F32)
                qnr = Ring(fw, es, "qn", 2, [128, 4, 128], F32)
                qsmr = Ring(fw, es, "qsm", 2, [128, 4, 32], F32)
                rtr = Ring(fw, es, "rt", 2, [128, 4, 4, 16], F32)
                qrr = Ring(fw, es, "qr", 3, [128, 4, 128], BF16)
                TTr = Ring(fw, es, "TT", 2, [128, 4, 128], BF16)
                e1r = Ring(fw, es, "e1", 2, [128, 512], F32)
                spr = Ring(fw, es, "sp", 2, [128, 512], F32)
                decr = Ring(fw, es, "dec", 2, [128, 4], F32)
                QKr = Ring(fw, es, "QK", 3, [128, 4, 256], BF16)
                kstr = Ring(fw, es, "kst", 2, [128, 2, 256], BF16)
                var = Ring(fw, es, "va", 2, [128, 512], BF16)
                rsr = Ring(fw, es, "rs", 2, [128, 512], BF16)
                GTr = Ring(fw, es, "GT", 2, [128, 8, 128], BF16)
                pTx = Ring(fw, es, "pTx", 1, [128, 2048], BF16, psum=True)
                main = Ring(fw, es, "mp", 4, [128, 512], F32, psum=True)
                pTs = Ring(fw, es, "pTs", 2, [128, 1024], BF16, psum=True)

                def load_slab(c0, n):
                    s = slabr.next()
                    D(SP, s[:, :, 0:n], T["wi_s"][:, :, c0:c0 + n], s, writes=[s])
                    return s

                pending = []

                def defer(fn):
                    pending.append(fn)

                def flush(keep=0):
                    while len(pending) > keep:
                        pending.pop(0)()

                def proj(slab, xnT, k, n=512):
                    ps = main.next()
                    for c in range(16):
                        I(PE, nc.tensor.matmul, ps[:, 0:n], xnT[:, c, k * 128:(k + 1) * 128], slab[:, c, 0:n],
                          start=(c == 0), stop=(c == 15), reads=[xnT, slab], writes=[ps])
                    flush(keep=1)
                    return ps

                for g in range(NT_EXT // 4):
                    tiles = [4 * g + k for k in range(4)]
                    own = (HT <= tiles[0] < HT + NT_OWN)
                    xnT = xnTr.next()
                    auxs = []
                    tabs = []
                    for k, te in enumerate(tiles):
                        x = xr.next(); aux = auxr.next(); auxs.append(aux)
                        D(SP, x[:], T["xe"][te * 128:(te + 1) * 128, :], x, writes=[x])
                        D(POOL, aux[:], T["aux"][te * 128:(te + 1) * 128, :], aux, writes=[aux])
                        tb = tabr.next(); tabs.append(tb)
                        for wi_, g_b_ in enumerate((gq_b, gk_b)):
                            I(POOL, nc.gpsimd.tensor_tensor, tb[:, wi_, 0, :].rearrange("p (a b) -> p a b", a=2),
                              bc(aux[:, 0:16], [(0, 2), (1, 16)]), g_b_[:, 0:32].rearrange("p (a b) -> p a b", a=2), ALU.mult,
                              reads=[aux, g_b_], writes=[tb])
                            I(DVE, nc.vector.scalar_tensor_tensor, tb[:, wi_, 1, 0:16], aux[:, 16:32], -1.0, g_b_[:, 16:32],
                              ALU.mult, ALU.mult, reads=[aux, g_b_], writes=[tb])
                            I(POOL, nc.gpsimd.tensor_tensor, tb[:, wi_, 1, 16:32], aux[:, 16:32], g_b_[:, 0:16], ALU.mult,
                              reads=[aux, g_b_], writes=[tb])
                        ss = ssr.next()
                        I(ACT, nc.scalar.activation, junk[:], x[:], AF.Square, accum_out=ss[:, 0:1],
                          reads=[x], writes=[junk, ss])
                        rstd_from_ss(ss[:, 0:1], 2048.0, ss, ss, ss[:, 0:1])
                        xb = xbr.next()
                        I(DVE, nc.vector.tensor_scalar, xb[:], x[:], ss[:, 0:1], None, ALU.mult,
                          reads=[x, ss], writes=[xb])
                        pT = pTx.next()
                        for c in range(16):
                            I(PE, nc.tensor.transpose, pT[:, c * 128:(c + 1) * 128], xb[:, c * 128:(c + 1) * 128],
                              identb[:], reads=[xb, identb], writes=[pT])
                        I(DVE, nc.vector.tensor_tensor, xnT[:, :, k * 128:(k + 1) * 128],
                          pT[:].rearrange("p (c t) -> p c t", c=16), bc(g1col[:], [(1, 16), (0, 128)]), ALU.mult,
                          reads=[pT, g1col], writes=[xnT])

                    slab = load_slab(1024, 32)
                    gps = main.next()
                    for c in range(16):
                        I(PE, nc.tensor.matmul, gps[0:32, :], slab[:, c, 0:32], xnT[:, c, :],
                          start=(c == 0), stop=(c == 15), reads=[xnT, slab], writes=[gps])
                    gT = gTr.next()
                    I(ACT, nc.scalar.copy, gT[0:32, :], gps[0:32, :], reads=[gps], writes=[gT])
                    Es = []
                    for k, te in enumerate(tiles):
                        zps = main.next()
                        I(PE, nc.tensor.matmul, zps[:], gT[0:33, k * 128:(k + 1) * 128], upb[0:33, :],
                          start=True, stop=True, reads=[gT, upb], writes=[zps])
                        e1 = e1r.next(); sp = spr.next()
                        I(ACT, nc.scalar.activation, e1[:], zps[:], AF.Exp, scale=-1.0, reads=[zps], writes=[e1])
                        I(ACT, nc.scalar.activation, sp[:], e1[:], AF.Ln, bias=1.0, reads=[e1], writes=[sp])
                        cum = main.next()
                        I(PE, nc.tensor.matmul, cum[:, 0:256], cst[:, C_LINCL:C_LINCL + 128], sp[:, 0:256],
                          start=True, stop=True, reads=[cst, sp], writes=[cum])
                        I(PE, nc.tensor.matmul, cum[:, 256:512], cst[:, C_UINCL:C_UINCL + 128], sp[:, 256:512],
                          start=True, stop=True, reads=[cst, sp], writes=[cum])
                        rem = main.next()
                        I(PE, nc.tensor.matmul, rem[:, 0:256], cst[:, C_USTRICT:C_USTRICT + 128], sp[:, 0:256],
                          start=True, stop=True, reads=[cst, sp], writes=[rem])
                        I(PE, nc.tensor.matmul, rem[:, 256:512], cst[:, C_LSTRICT:C_LSTRICT + 128], sp[:, 256:512],
                          start=True, stop=True, reads=[cst, sp], writes=[rem])
                        E = Er.next(); Es.append(E)
                        I(ACT, nc.scalar.activation, E[:, 0, :], cum[:], AF.Exp, scale=-1.0 / 16, reads=[cum], writes=[E])
                        I(ACT, nc.scalar.activation, E[:, 1, :], cum[:], AF.Exp, scale=1.0 / 16, reads=[cum], writes=[E])
                        I(ACT, nc.scalar.activation, E[:, 2, :], rem[:], AF.Exp, scale=-1.0 / 16, reads=[rem], writes=[E])
                        tot = main.next()
                        for cc in range(4):
                            I(PE, nc.tensor.matmul, tot[:, cc:cc + 1], sp[:, cc * 128:(cc + 1) * 128],
                              cst[:, C_ONES:C_ONES + 1], start=True, stop=True, reads=[cst, sp], writes=[tot])
                        dec = decr.next()
                        I(ACT, nc.scalar.activation, dec[:], tot[:, 0:4], AF.Exp, scale=-1.0 / 16, reads=[tot], writes=[dec])
                        D(ACT, T["dec_s"][te], dec[:], dec, reads=[dec])

                    slab = load_slab(0, 512)
                    for k, te in enumerate(tiles):
                        ps = proj(slab, xnT, k)
                        E = Es[k]
                        QK = QKr.next(); kst = kstr.next()
                        qa = bc(ps[:, 0:256], [(0, 2), (1, 256)])
                        ka = bc(ps[:, 256:512], [(0, 2), (1, 256)])
                        I(DVE, nc.vector.scalar_tensor_tensor, QK[:, 0:2, :], qa, 0.125,
                          E[:, 0, :].rearrange("p (a b) -> p a b", a=2), ALU.mult, ALU.mult,
                          reads=[ps, E], writes=[QK])
                        I(DVE, nc.vector.tensor_tensor, QK[:, 2:4, :], ka, E[:, 1, :].rearrange("p (a b) -> p a b", a=2),
                          ALU.mult, reads=[ps, E], writes=[QK])
                        I(DVE, nc.vector.tensor_tensor, kst[:], ka, E[:, 2, :].rearrange("p (a b) -> p a b", a=2),
                          ALU.mult, reads=[ps, E], writes=[kst])
                        D(POOL, T["kst_s"][te * 128:(te + 1) * 128, :], kst[:].rearrange("p a b -> p (a b)"), kst, reads=[kst])
                        def tail_qk(QK=QK, te=te):
                            pT = pTs.next()
                            QKf = QK[:].rearrange("p a b -> p (a b)")
                            for b8 in range(8):
                                I(PE, nc.tensor.transpose, pT[:, b8 * 128:(b8 + 1) * 128], QKf[:, b8 * 128:(b8 + 1) * 128],
                                  identb[:], reads=[QK, identb], writes=[pT])
                            GT = GTr.next()
                            I(ACT, nc.scalar.copy, GT[:].rearrange("p a b -> p (a b)"), pT[:], reads=[pT], writes=[GT])
                            D(ACT, T["glaT_s"][te], GT[:].rearrange("p a b -> p (a b)"), GT, reads=[GT])
                        defer(tail_qk)

                    slab = load_slab(512, 512)
                    for k, te in enumerate(tiles):
                        ps = proj(slab, xnT, k)
                        va = var.next()
                        I(ACT, nc.scalar.copy, va[:], ps[:], reads=[ps], writes=[va])
                        D(ACT, T["va_s"][te * 128:(te + 1) * 128, :], va[:], va, reads=[va])

                    if own:
                        slab = load_slab(1056, 512)
                        for k, te in enumerate(tiles):
                            ps = proj(slab, xnT, k)
                            rs = rsr.next()
                            I(ACT, nc.scalar.activation, rs[:], ps[:], AF.Silu, reads=[ps], writes=[rs])
                            to = te - HT
                            D(ACT, T["rs_s"][to * 128:(to + 1) * 128, :], rs[:], rs, reads=[rs])

                    for which in ("q", "k"):
                        if which == "q" and not own:
                            continue
                        base = 1568 if which == "q" else 3104
                        g_b = gq_b if which == "q" else gk_b
                        for hg in range(3):
                            slab = load_slab(base + hg * 512, 512)
                            for k, te in enumerate(tiles):
                                ps = proj(slab, xnT, k)
                                ps3 = ps[:].rearrange("p (h d) -> p h d", h=4)
                                sq = sqr.next(); ss4 = ss4r.next()
                                I(ACT, nc.scalar.activation, sq[:], ps[:], AF.Square, reads=[ps], writes=[sq])
                                I(DVE, nc.vector.tensor_reduce, ss4[:], sq[:].rearrange("p (h d) -> p h d", h=4), AX.X, ALU.add,
                                  reads=[sq], writes=[ss4])
                                rstd_from_ss(ss4[:], 128.0, ss4, ss4, ss4[:])
                                qn = qnr.next(); qr = qrr.next(); qsm = qsmr.next(); rt = rtr.next()
                                I(DVE, nc.vector.tensor_tensor, qn[:], ps3, bc(ss4[:], [(1, 4), (0, 128)]), ALU.mult,
                                  reads=[ps, ss4], writes=[qn])
                                I(DVE, nc.vector.tensor_tensor, qr[:], qn[:], bc(g_b[:], [(0, 4), (1, 128)]), ALU.mult,
                                  reads=[qn, g_b], writes=[qr])
                                tb = tabs[k]
                                wi_ = 0 if which == "q" else 1
                                TA = tb[:, wi_, 0, :]; TB = tb[:, wi_, 1, :]
                                I(DVE, nc.vector.tensor_tensor, qsm[:], qn[:, :, 0:32], bc(TA, [(0, 4), (1, 32)]), ALU.mult,
                                  reads=[qn, tb], writes=[qsm])
                                I(POOL, nc.gpsimd.tensor_tensor, rt[:, :, 0, :], qn[:, :, 16:32], bc(TB[:, 0:16], [(0, 4), (1, 16)]), ALU.mult,
                                  reads=[qn, tb], writes=[rt])
                                I(POOL, nc.gpsimd.tensor_tensor, rt[:, :, 1, :], qn[:, :, 0:16], bc(TB[:, 16:32], [(0, 4), (1, 16)]), ALU.mult,
                                  reads=[qn, tb], writes=[rt])
                                I(DVE, nc.vector.tensor_tensor, qr[:, :, 0:32].rearrange("p h (a b) -> p h a b", a=2),
                                  qsm[:].rearrange("p h (a b) -> p h a b", a=2), rt[:, :, 0:2, :], ALU.add,
                                  reads=[qsm, rt], writes=[qr])
                                def tail_d(qr=qr, te=te, hg=hg, which=which):
                                    pT = pTs.next()
                                    for h in range(4):
                                        I(PE, nc.tensor.transpose, pT[:, h * 128:(h + 1) * 128], qr[:, h, :], identb[:],
                                          reads=[qr, identb], writes=[pT])
                                    TT = TTr.next()
                                    I(ACT, nc.scalar.copy, TT[:].rearrange("p a b -> p (a b)"), pT[:, 0:512], reads=[pT], writes=[TT])
                                    if which == "q":
                                        to = te - HT
                                        dst = T["qdT_s"][hg * 4:(hg + 1) * 4, :, to * 128:(to + 1) * 128]
                                    else:
                                        dst = T["kdT_s"][hg * 4:(hg + 1) * 4, :, te * 128:(te + 1) * 128]
                                    D(ACT, dst.rearrange("h p t -> p h t"), TT[:], TT, reads=[TT])
                                defer(tail_d)

                    for hg in range(3):
                        slab = load_slab(4640 + hg * 512, 512)
                        for k, te in enumerate(tiles):
                            ps = proj(slab, xnT, k)
                            vd = vdr.next()
                            I(ACT, nc.scalar.copy, vd[:, :, 0:128], ps[:].rearrange("p (h d) -> p h d", h=4),
                              reads=[ps], writes=[vd])
                            I(DVE, nc.vector.tensor_copy, vd[:, :, 128:129], bc(auxs[k][:, 32:33], [(0, 4), (1, 1)]),
                              reads=[auxs[k]], writes=[vd])
                            D(POOL, T["vd_s"][te * 128:(te + 1) * 128, hg * 516:(hg + 1) * 516], vd[:].rearrange("p h d -> p (h d)"),
                              vd, reads=[vd])
                flush()
                fw.end_phase()

        if "gla" in phases:
            with ExitStack() as es:
                og_b = fw.sbuf(es, "og_b", [128, 512], F32)
                D(SP, og_b[:], T["out_g"].partition_broadcast(128), og_b, writes=[og_b])
                st_f = fw.sbuf(es, "st_f", [128, 2, 128], F32)
                st_fb = fw.sbuf(es, "st_fb", [128, 2, 128], BF16)
                st_b = fw.sbuf(es, "st_b", [128, 2, 128], F32)
                st_bb = fw.sbuf(es, "st_bb", [128, 2, 128], BF16)
                SR = fw.sbuf(es, "SR", [128, NT_OWN, 2, 128], BF16)
                for b in (st_f, st_fb, st_b, st_bb):
                    I(DVE, nc.vector.memset, b[:], 0.0, writes=[b])
                kstr = Ring(fw, es, "gkst", 3, [128, 512], BF16)
                var = Ring(fw, es, "gva", 3, [128, 512], BF16)
                decr = Ring(fw, es, "gdec", 3, [128, 4], F32)
                GTr = Ring(fw, es, "gGT", 2, [128, 8, 128], BF16)
                rsr = Ring(fw, es, "grs", 2, [128, 512], BF16)
                Smr = Ring(fw, es, "Sm", 2, [128, 8, 128], BF16)
                sqr = Ring(fw, es, "gsq", 2, [128, 512], F32)
                ss4r = Ring(fw, es, "gss4", 2, [128, 4], F32)
                onr = Ring(fw, es, "gon", 2, [128, 512], F32)
                mixr = Ring(fw, es, "gmix", 2, [128, 512], BF16)
                inc_r = Ring(fw, es, "inc", 2, [128, 2, 128], F32, psum=True)
                S_r = Ring(fw, es, "Sps", 1, [128, 8, 128], F32, psum=True)
                o_r = Ring(fw, es, "ops", 2, [128, 4, 128], F32, psum=True)
                dummy = fw.psum(es, "dummy", [128, 128], F32)

                def load_tile(te, need_out):
                    kst = kstr.next(); va = var.next(); dec = decr.next()
                    D(SP, kst[:], T["kst_s"][te * 128:(te + 1) * 128, :], kst, writes=[kst])
                    D(SP, va[:], T["va_s"][te * 128:(te + 1) * 128, :], va, writes=[va])
                    D(SP, dec[:], T["dec_s"][te], dec, writes=[dec])
                    GT = rs = None
                    if need_out:
                        GT = GTr.next(); rs = rsr.next()
                        to = te - HT
                        D(SP, GT[:].rearrange("p a b -> p (a b)"), T["glaT_s"][te], GT, writes=[GT])
                        D(SP, rs[:], T["rs_s"][to * 128:(to + 1) * 128, :], rs, writes=[rs])
                    return kst, va, dec, GT, rs

                def state_update(st, stb, kst, va, dec, d):
                    inc = inc_r.next()
                    for h in range(4):
                        c, hh = h // 2, h % 2
                        I(PE, nc.tensor.matmul, inc[hh * 64:(hh + 1) * 64, c, :],
                          kst[:, d * 256 + h * 64:d * 256 + (h + 1) * 64], va[:, h * 128:(h + 1) * 128],
                          start=True, stop=True, reads=[kst, va], writes=[inc])
                    for c in range(2):
                        I(DVE, nc.vector.scalar_tensor_tensor, st[:, c, :], st[:, c, :], dec[:, 2 * d + c:2 * d + c + 1],
                          inc[:, c, :], ALU.mult, ALU.add, reads=[st, dec, inc], writes=[st])
                    I(ACT, nc.scalar.copy, stb[:], st[:], reads=[st], writes=[stb])

                for te in range(NT_EXT - 1, HT - 1, -1):
                    kst, va, dec, _, _ = load_tile(te, False)
                    if te < HT + NT_OWN:
                        I(ACT, nc.scalar.copy, SR[:, te - HT, :, :], st_bb[:], reads=[st_bb], writes=[SR])
                    state_update(st_b, st_bb, kst, va, dec, 1)

                for te in range(0, HT + NT_OWN):
                    need = te >= HT
                    kst, va, dec, GT, rs = load_tile(te, need)
                    if need:
                        to = te - HT
                        Sps = S_r.next()
                        for hh in range(2):
                            if hh == 1:
                                I(PE, nc.tensor.matmul, dummy[:], GT[:, 0, :], GT[:, 1, :], start=True, stop=True,
                                  reads=[GT], writes=[dummy])
                            for d in range(2):
                                for c in range(2):
                                    h = 2 * c + hh
                                    kT = GT[hh * 64:(hh + 1) * 64, (2 + d) * 2 + c, :]
                                    qT = GT[hh * 64:(hh + 1) * 64, d * 2 + c, :]
                                    I(PE, nc.tensor.matmul, Sps[:, d * 4 + h, :], kT, qT, start=True, stop=True,
                                      reads=[GT], writes=[Sps])
                        Sm = Smr.next()
                        for d in range(2):
                            I(DVE, nc.vector.tensor_tensor, Sm[:, d * 4:(d + 1) * 4, :], Sps[:, d * 4:(d + 1) * 4, :],
                              bc(trib[:, d * 128:(d + 1) * 128], [(0, 4), (1, 128)]), ALU.mult,
                              reads=[Sps, trib], writes=[Sm])
                        ops = o_r.next()
                        for h in range(4):
                            c, hh = h // 2, h % 2
                            vh = va[:, h * 128:(h + 1) * 128]
                            I(PE, nc.tensor.matmul, ops[:, h, :], Sm[:, h, :], vh, start=True, stop=False,
                              reads=[Sm, va], writes=[ops])
                            I(PE, nc.tensor.matmul, ops[:, h, :], Sm[:, 4 + h, :], vh, start=False, stop=False,
                              reads=[Sm, va], writes=[ops])
                            I(PE, nc.tensor.matmul, ops[:, h, :], GT[hh * 64:(hh + 1) * 64, 0 * 2 + c, :],
                              st_fb[hh * 64:(hh + 1) * 64, c, :], start=False, stop=False,
                              reads=[GT, st_fb], writes=[ops])
                            I(PE, nc.tensor.matmul, ops[:, h, :], GT[hh * 64:(hh + 1) * 64, 1 * 2 + c, :],
                              SR[hh * 64:(hh + 1) * 64, to, c, :], start=False, stop=True,
                              reads=[GT, SR], writes=[ops])
                        opf = ops[:].rearrange("p h d -> p (h d)")
                        sq = sqr.next(); ss4 = ss4r.next(); on = onr.next(); mix = mixr.next()
                        I(ACT, nc.scalar.activation, sq[:], opf, AF.Square, reads=[ops], writes=[sq])
                        I(DVE, nc.vector.tensor_reduce, ss4[:], sq[:].rearrange("p (h d) -> p h d", h=4), AX.X, ALU.add,
                          reads=[sq], writes=[ss4])
                        rstd_from_ss(ss4[:], 128.0, ss4, ss4, ss4[:])
                        I(DVE, nc.vector.tensor_tensor, on[:].rearrange("p (h d) -> p h d", h=4), ops[:],
                          bc(ss4[:], [(1, 4), (0, 128)]), ALU.mult, reads=[ops, ss4], writes=[on])
                        I(DVE, nc.vector.tensor_tensor, on[:], on[:], og_b[:], ALU.mult, reads=[on, og_b], writes=[on])
                        I(DVE, nc.vector.tensor_tensor, mix[:], on[:], rs[:], ALU.mult, reads=[on, rs], writes=[mix])
                        D(POOL, T["mixed_s"][to * 128:(to + 1) * 128, 0:512], mix[:], mix, reads=[mix])
                    state_update(st_f, st_fb, kst, va, dec, 0)
                fw.end_phase()

        if "dil" in phases:
            with ExitStack() as es:
                cmaskb = fw.sbuf(es, "cmaskb", [128, 17 * 128], BF16)
                I(DVE, nc.vector.tensor_copy, cmaskb[:], cst[:, C_CMASK:C_CMASK + 17 * 128], reads=[cst], writes=[cmaskb])
                kTr = Ring(fw, es, "dkT", 2, [128, NEXT], BF16)
                vr = Ring(fw, es, "dv", 2, [128, NT_EXT, 129], BF16)
                qTr = Ring(fw, es, "dqT", 2, [128, NTOK], BF16)
                exr = Ring(fw, es, "dex", 4, [128, 512], BF16)
                pmr = Ring(fw, es, "dpm", 5, [128, 512], BF16)
                rdr = Ring(fw, es, "drd", 3, [128, 1], F32)
                oor = Ring(fw, es, "doo", 3, [128, 128], BF16)
                S_r = Ring(fw, es, "dS", 3, [128, 512], F32, psum=True)
                o_r = Ring(fw, es, "dO", 2, [128, 129], F32, psum=True)
                tg = tables_gen(es) if "tables" in phases else None
                dstep = 0
                vd_v = T["vd_s"].rearrange("(t p) (h d) -> h p t d", p=128, h=12)
                def dil_load(h):
                    kT = kTr.next(); v = vr.next(); qT = qTr.next()
                    D(SP, kT[:], T["kdT_s"][h], kT, writes=[kT])
                    D(SP, qT[:], T["qdT_s"][h], qT, writes=[qT])
                    for v4 in range(4):
                        D(SP, v[:, v4 * 12:(v4 + 1) * 12, :], vd_v[h][:, v4 * 12:(v4 + 1) * 12, :], v, writes=[v])
                    return kT, v, qT
                nxt = dil_load(0)
                dpend = []
                for h in range(12):
                    kT, v, qT = nxt
                    while dpend:
                        dpend.pop(0)()
                    if h + 1 < 12:
                        nxt = dil_load(h + 1)
                    for qi in range(NT_OWN):
                        dstep += 1
                        if tg is not None and dstep % 3 == 0:
                            next(tg, None)
                        ops = o_r.next()
                        kts = list(range(qi, qi + 17))
                        for g0 in range(0, 17, 4):
                            grp = kts[g0:g0 + 4]
                            n = len(grp)
                            Sps = S_r.next()
                            for k, kt in enumerate(grp):
                                I(PE, nc.tensor.matmul, Sps[:, k * 128:(k + 1) * 128], kT[:, kt * 128:(kt + 1) * 128],
                                  qT[:, qi * 128:(qi + 1) * 128], start=True, stop=True, reads=[kT, qT], writes=[Sps])
                            ex = exr.next(); pm = pmr.next()
                            I(ACT, nc.scalar.activation, ex[:, 0:n * 128], Sps[:, 0:n * 128], AF.Exp, reads=[Sps], writes=[ex])
                            I(DVE, nc.vector.tensor_tensor, pm[:, 0:n * 128], ex[:, 0:n * 128],
                              cmaskb[:, g0 * 128:(g0 + n) * 128], ALU.mult, reads=[ex, cmaskb], writes=[pm])

                            def pv(grp=grp, g0=g0, pm=pm, ops=ops, v=v, qi=qi, h=h):
                                for k, kt in enumerate(grp):
                                    I(PE, nc.tensor.matmul, ops[:, 0:129], pm[:, k * 128:(k + 1) * 128], v[:, kt, :],
                                      start=(g0 + k == 0), stop=(g0 + k == 16), reads=[pm, v], writes=[ops])
                                if g0 + len(grp) == 17:
                                    rd = rdr.next(); oo = oor.next()
                                    I(DVE, nc.vector.reciprocal, rd[:], ops[:, 128:129], reads=[ops], writes=[rd])
                                    I(DVE, nc.vector.tensor_scalar, oo[:], ops[:, 0:128], rd[:, 0:1], None, ALU.mult,
                                      reads=[ops, rd], writes=[oo])
                                    D(POOL, T["mixed_s"][qi * 128:(qi + 1) * 128, 512 + h * 128:512 + (h + 1) * 128], oo[:], oo,
                                      reads=[oo])
                            dpend.append(pv)
                            while len(dpend) > 2:
                                dpend.pop(0)()
                while dpend:
                    dpend.pop(0)()
                if tg is not None:
                    for _ in tg:
                        pass
                fw.end_phase()

        if "wout" in phases:
            with ExitStack() as es:
                Wo = fw.sbuf(es, "Wo", [128, 16, 2048], BF16)
                g2col = fw.sbuf(es, "g2col", [128, 16], F32)
                D(SP, g2col[:], T["g2"], g2col, writes=[g2col])
                with ExitStack() as es2:
                    wst = Ring(fw, es2, "wst2", 2, [128, 2048], F32)
                    for c in range(16):
                        s_ = wst.next()
                        D(SP, s_[:], T["w_out"][c * 128:(c + 1) * 128, :], s_, writes=[s_])
                        if c % 2 == 0:
                            I(ACT, nc.scalar.copy, Wo[:, c, :], s_[:], reads=[s_], writes=[Wo])
                        else:
                            I(DVE, nc.vector.tensor_copy, Wo[:, c, :], s_[:], reads=[s_], writes=[Wo])
                    fw.barrier()
                mxr = Ring(fw, es, "mx", 3, [128, 2048], BF16)
                mTr = Ring(fw, es, "mT", 3, [128, 16, 128], BF16)
                xr = Ring(fw, es, "x5", 3, [128, 2048], F32)
                ssr = Ring(fw, es, "ss5", 3, [128, 1], F32)
                xb2r = Ring(fw, es, "xb2", 2, [128, 2048], BF16)
                xnTr = Ring(fw, es, "xn2T", 2, [128, 16, 128], BF16)
                pTx = Ring(fw, es, "pT5", 2, [128, 2048], BF16, psum=True)
                main = Ring(fw, es, "mp5", 4, [128, 512], F32, psum=True)
                st = {}

                def stepA(to):
                    te = to + HT
                    mx = mxr.next(); x = xr.next()
                    D(SP, mx[:], T["mixed_s"][to * 128:(to + 1) * 128, :], mx, writes=[mx])
                    D(SP, x[:], T["xe"][te * 128:(te + 1) * 128, :], x, writes=[x])
                    pT = pTx.next()
                    for c in range(16):
                        I(PE, nc.tensor.transpose, pT[:, c * 128:(c + 1) * 128], mx[:, c * 128:(c + 1) * 128], identb[:],
                          reads=[mx, identb], writes=[pT])
                    mT = mTr.next()
                    I(ACT, nc.scalar.copy, mT[:].rearrange("p a b -> p (a b)"), pT[:], reads=[pT], writes=[mT])
                    st[to] = (mT, x)

                def stepB(to):
                    mT, x1 = st[to]
                    for q in range(4):
                        ps = main.next()
                        for c in range(16):
                            I(PE, nc.tensor.matmul, ps[:], mT[:, c, :], Wo[:, c, q * 512:(q + 1) * 512],
                              start=(c == 0), stop=(c == 15), reads=[mT, Wo], writes=[ps])
                        I(DVE, nc.vector.tensor_tensor, x1[:, q * 512:(q + 1) * 512], ps[:], x1[:, q * 512:(q + 1) * 512], ALU.add,
                          reads=[ps, x1], writes=[x1])
                    D(ACT, T["x1_s"][to * 128:(to + 1) * 128, :], x1[:], x1, reads=[x1])
                    ss = ssr.next()
                    xb2 = xb2r.next()
                    I(ACT, nc.scalar.activation, xb2[:], x1[:], AF.Square, accum_out=ss[:, 0:1], reads=[x1], writes=[xb2, ss])
                    rstd_from_ss(ss[:, 0:1], 2048.0, ss, ss, ss[:, 0:1])
                    I(ACT, nc.scalar.activation, xb2[:], x1[:], AF.Copy, scale=ss[:, 0:1], reads=[x1, ss], writes=[xb2])
                    st[to] = (xb2,)

                def stepC(to):
                    (xb2,) = st.pop(to)
                    pT = pTx.next()
                    for c in range(16):
                        I(PE, nc.tensor.transpose, pT[:, c * 128:(c + 1) * 128], xb2[:, c * 128:(c + 1) * 128], identb[:],
                          reads=[xb2, identb], writes=[pT])
                    xnT = xnTr.next()
                    I(DVE, nc.vector.tensor_tensor, xnT[:], pT[:].rearrange("p (c t) -> p c t", c=16),
                      bc(g2col[:], [(1, 16), (0, 128)]), ALU.mult, reads=[pT, g2col], writes=[xnT])
                    D(ACT, T["xn2T_s"][:, :, to * 128:(to + 1) * 128], xnT[:], xnT, reads=[xnT])

                for n in range(NT_OWN + 2):
                    if n < NT_OWN:
                        stepA(n)
                    if 0 <= n - 1 < NT_OWN:
                        stepB(n - 1)
                    if 0 <= n - 2 < NT_OWN:
                        stepC(n - 2)
                fw.end_phase()

            with ExitStack() as es:
                Wq = fw.sbuf(es, "Wq", [128, 16, 2048], BF16)
                KT = fw.sbuf(es, "KT", [128, 16, 128], F32)
                with ExitStack() as es2:
                    wst = Ring(fw, es2, "wst3", 2, [128, 2048], F32)
                    for c in range(16):
                        s_ = wst.next()
                        D(SP, s_[:], T["wq"][c * 128:(c + 1) * 128, :], s_, writes=[s_])
                        if c % 2 == 0:
                            I(ACT, nc.scalar.copy, Wq[:, c, :], s_[:], reads=[s_], writes=[Wq])
                        else:
                            I(DVE, nc.vector.tensor_copy, Wq[:, c, :], s_[:], reads=[s_], writes=[Wq])
                    kps = fw.psum(es2, "kps", [128, 512], F32)
                    for g4 in range(4):
                        s_ = wst.next()
                        D(SP, s_[:, 0:512].rearrange("p (a b) -> p a b", a=4),
                          T["subk"][g4 * 4:(g4 + 1) * 4].rearrange("a k d -> k a d"), s_, writes=[s_])
                        for a_ in range(4):
                            I(PE, nc.tensor.transpose, kps[:, a_ * 128:(a_ + 1) * 128], s_[:, a_ * 128:(a_ + 1) * 128],
                              identf[:, C_IDENT:C_IDENT + 128], reads=[s_, cst], writes=[kps])
                        I(ACT, nc.scalar.copy, KT[:, g4 * 4:(g4 + 1) * 4, :].rearrange("p a b -> p (a b)"), kps[:],
                          reads=[kps], writes=[KT])
                    fw.barrier()
                xn4r = Ring(fw, es, "xn4", 2, [128, 16, 512], BF16)
                qryTr = Ring(fw, es, "qryT", 1, [128, 16, 512], F32)
                scr = Ring(fw, es, "sc", 2, [128, 16, 128], F32)
                topv = fw.sbuf(es, "topv", [128, 16, 16], F32)
                topi = fw.sbuf(es, "topi", [128, 16, 16], U32)
                topif_r = Ring(fw, es, "topif", 2, [128, 16, 16], F32)
                wkA = fw.sbuf(es, "wkA", [128, 128], F32)
                wkB = fw.sbuf(es, "wkB", [128, 128], F32)
                cand = fw.sbuf(es, "cand", [128, 8, 16, 16], F32)
                wk2A = fw.sbuf(es, "wk2A", [128, 256], F32)
                wk2B = fw.sbuf(es, "wk2B", [128, 256], F32)
                tvb = [Buf(f"tv{i}") for i in range(16)]
                tib = [Buf(f"ti{i}") for i in range(16)]
                cmb = [Buf(f"cm{i}") for i in range(8)]
                cm = fw.sbuf(es, "cm", [128, 8, 16], F32)
                cpos = fw.sbuf(es, "cpos", [128, 8, 16], U32)
                cpb = [Buf(f"cp{i}") for i in range(8)]
                abu = fw.sbuf(es, "abu", [128, 2, 8, 16], U32)
                ab = fw.sbuf(es, "ab", [128, 2, 8, 16], F32)
                eq = fw.sbuf(es, "eq", [128, 8, 16, 16], F32)
                gts = fw.sbuf(es, "gts", [128, 8, 16], F32)
                R_r = Ring(fw, es, "R3", 2, [128, 3, 128], F32)
                Zs = fw.sbuf(es, "Zs", [128, 8], F32)
                main = Ring(fw, es, "mp6", 6, [128, 512], F32, psum=True)
                for g in range(NT_OWN // 4):
                    xn4 = xn4r.next()
                    D(SP, xn4[:], T["xn2T_s"][:, :, g * 512:(g + 1) * 512], xn4, writes=[xn4])
                    qryT = qryTr.next()
                    for hc in range(16):
                        ps = main.next()
                        for c in range(16):
                            I(PE, nc.tensor.matmul, ps[:], Wq[:, c, hc * 128:(hc + 1) * 128], xn4[:, c, :],
                              start=(c == 0), stop=(c == 15), reads=[Wq, xn4], writes=[ps])
                        I(ACT, nc.scalar.copy, qryT[:, hc, :], ps[:], reads=[ps], writes=[qryT])
                    for k in range(4):
                        to = g * 4 + k
                        sc = scr.next()
                        for g4 in range(4):
                            ps = main.next()
                            for a_ in range(4):
                                hc = g4 * 4 + a_
                                I(PE, nc.tensor.matmul, ps[:, a_ * 128:(a_ + 1) * 128], qryT[:, hc, k * 128:(k + 1) * 128], KT[:, hc, :],
                                  start=True, stop=True, reads=[qryT, KT], writes=[ps])
                            I(ACT, nc.scalar.copy, sc[:, g4 * 4:(g4 + 1) * 4, :].rearrange("p a b -> p (a b)"), ps[:],
                              reads=[ps], writes=[sc])
                        for hp in range(0, 16, 2):
                            pr = [(hp, wkA, tvb[hp], tib[hp]), (hp + 1, wkB, tvb[hp + 1], tib[hp + 1])]
                            for (hc, wk_, tv_, ti_) in pr:
                                I(DVE, nc.vector.max, topv[:, hc, 0:8], sc[:, hc, :], reads=[sc], writes=[tv_])
                            for (hc, wk_, tv_, ti_) in pr:
                                I(DVE, nc.vector.max_index, topi[:, hc, 0:8], topv[:, hc, 0:8], sc[:, hc, :], reads=[sc, tv_], writes=[ti_])
                            for (hc, wk_, tv_, ti_) in pr:
                                I(DVE, nc.vector.match_replace, wk_[:], topv[:, hc, 0:8], sc[:, hc, :], NEG, reads=[sc, tv_], writes=[wk_])
                            for (hc, wk_, tv_, ti_) in pr:
                                I(DVE, nc.vector.max, topv[:, hc, 8:16], wk_[:], reads=[wk_], writes=[tv_])
                            for (hc, wk_, tv_, ti_) in pr:
                                I(DVE, nc.vector.max_index, topi[:, hc, 8:16], topv[:, hc, 8:16], wk_[:], reads=[wk_, tv_], writes=[ti_])
                        topif = topif_r.next()
                        I(DVE, nc.vector.tensor_copy, topif[:], topi[:], reads=tib, writes=[topif])
                        tv = topv[:]
                        v1 = bass.AP(tv.tensor, tv.offset, [list(tv.ap[0]), [32, 8], [1, 16], [0, 16]])
                        v2 = bass.AP(tv.tensor, tv.offset + 16, [list(tv.ap[0]), [32, 8], [0, 16], [1, 16]])
                        I(POOL, nc.gpsimd.tensor_tensor, cand[:], v1, v2, ALU.add, reads=tvb, writes=[cand])
                        for h2 in range(0, 8, 2):
                            pr = [(h2, wk2A, cmb[h2]), (h2 + 1, wk2B, cmb[h2 + 1])]
                            chs = {h: cand[:, h, :, :].rearrange("p a b -> p (a b)") for (h, _, _) in pr}
                            for (h, w2, cb) in pr:
                                I(DVE, nc.vector.max, cm[:, h, 0:8], chs[h], reads=[cand], writes=[cb])
                            for (h, w2, cb) in pr:
                                I(DVE, nc.vector.max_index, cpos[:, h, 0:8], cm[:, h, 0:8], chs[h], reads=[cand, cb], writes=[cpb[h]])
                            for (h, w2, cb) in pr:
                                I(DVE, nc.vector.match_replace, w2[:], cm[:, h, 0:8], chs[h], NEG, reads=[cand, cb], writes=[w2])
                            for (h, w2, cb) in pr:
                                I(DVE, nc.vector.max, cm[:, h, 8:16], w2[:], reads=[w2], writes=[cb])
                            for (h, w2, cb) in pr:
                                I(DVE, nc.vector.max_index, cpos[:, h, 8:16], cm[:, h, 8:16], w2[:], reads=[w2, cb], writes=[cpb[h]])
                        I(DVE, nc.vector.tensor_scalar, abu[:, 0, :, :], cpos[:], 4, None, ALU.logical_shift_right, reads=cpb, writes=[abu])
                        I(DVE, nc.vector.tensor_scalar, abu[:, 1, :, :], cpos[:], 15, None, ALU.bitwise_and, reads=cpb, writes=[abu])
                        I(DVE, nc.vector.tensor_copy, ab[:], abu[:], reads=[abu], writes=[ab])
                        R = R_r.next()
                        tfv = topif[:]
                        io16 = bc(cst[:, C_IOTA:C_IOTA + 16], [(0, 8), (0, 16), (1, 16)])
                        for c_ in range(2):
                            abv = ab[:, c_, :, :]
                            I(DVE, nc.vector.tensor_tensor, eq[:], io16, bc(abv, [(16, 8), (1, 16), (0, 16)]), ALU.is_equal,
                              reads=[cst, ab], writes=[eq])
                            idx_b = bass.AP(tfv.tensor, tfv.offset + 16 * c_, [list(tfv.ap[0]), [32, 8], [0, 16], [1, 16]])
                            I(POOL, nc.gpsimd.tensor_tensor, eq[:], eq[:], idx_b, ALU.mult, reads=[eq, topif], writes=[eq])
                            I(DVE, nc.vector.tensor_reduce, R[:, c_, :].rearrange("p (h r) -> p h r", h=8), eq[:], AX.X, ALU.add,
                              reads=[eq], writes=[R])
                        I(POOL, nc.gpsimd.tensor_tensor, gts[:], cm[:], bc(cm[:, :, 0:1], [(16, 8), (0, 16)]), ALU.subtract,
                          reads=cmb, writes=[gts])
                        I(ACT, nc.scalar.activation, gts[:], gts[:], AF.Exp, reads=[gts], writes=[gts])
                        I(DVE, nc.vector.tensor_reduce, Zs[:], gts[:], AX.X, ALU.add, reads=[gts], writes=[Zs])
                        I(DVE, nc.vector.reciprocal, Zs[:], Zs[:], reads=[Zs], writes=[Zs])
                        I(DVE, nc.vector.tensor_tensor, R[:, 2, :].rearrange("p (h r) -> p h r", h=8), gts[:],
                          bc(Zs[:], [(1, 8), (0, 16)]), ALU.mult, reads=[gts, Zs], writes=[R])
                        D(ACT, T["r3_s"][to * 128:(to + 1) * 128, :], R[:].rearrange("p a b -> p (a b)"), R, reads=[R])
                fw.end_phase()

        if "gmat" in phases:
            with ExitStack() as es:
                Rr = Ring(fw, es, "gR", 3, [128, 3, 128], F32)
                RT_r = Ring(fw, es, "gRT", 2, [128, 3, 128], F32)
                gtb_r = Ring(fw, es, "gtb", 2, [128, 128], BF16)
                RTb_r = Ring(fw, es, "gRTb", 2, [128, 2, 128], BF16)
                iotab = fw.sbuf(es, "iotab", [128, 128], BF16)
                I(DVE, nc.vector.tensor_copy, iotab[:], cst[:, C_IOTA:C_IOTA + 128], reads=[cst], writes=[iotab])
                OI_r = Ring(fw, es, "OI", 2, [128, 64, 128], BF16)
                OJ_r = Ring(fw, es, "OJ", 2, [128, 64, 128], BF16)
                OJg_r = Ring(fw, es, "OJg", 2, [128, 64, 128], BF16)
                Gr = Ring(fw, es, "Gall", 2, [128, 128, 128], BF16)
                pR = fw.psum(es, "pR", [128, 3, 128], F32)
                cps = Ring(fw, es, "cps", 4, [128, 4, 128], F32, psum=True)
                iota = cst[:, C_IOTA:C_IOTA + 128]

                Rl = {}

                def rload(to):
                    R = Rr.next()
                    D(SP, R[:].rearrange("p a b -> p (a b)"), T["r3_s"][to * 128:(to + 1) * 128, :], R, writes=[R])
                    Rl[to] = R

                def front(to):
                    R = Rl.pop(to); RT = RT_r.next(); gtb = gtb_r.next()
                    for c in range(3):
                        I(PE, nc.tensor.transpose, pR[:, c, :], R[:, c, :], identf[:, C_IDENT:C_IDENT + 128],
                          reads=[R, cst], writes=[pR])
                    I(ACT, nc.scalar.copy, RT[:].rearrange("p a b -> p (a b)"), pR[:].rearrange("p a b -> p (a b)"),
                      reads=[pR], writes=[RT])
                    I(ACT, nc.scalar.copy, gtb[:], RT[:, 2, :], reads=[RT], writes=[gtb])
                    RTb = RTb_r.next()
                    I(ACT, nc.scalar.copy, RTb[:].rearrange("p a b -> p (a b)"), RT[:, 0:2, :].rearrange("p a b -> p (a b)"),
                      reads=[RT], writes=[RTb])
                    return RTb, gtb

                def gens(hf, RT, gtb):
                    t0h = hf * 64
                    OI = OI_r.next(); OJ = OJ_r.next(); OJg = OJg_r.next()
                    I(DVE, nc.vector.tensor_tensor, OI[:], bc(iotab[:], [(0, 64), (1, 128)]),
                      bc(RT[:, 0, t0h:t0h + 64], [(1, 64), (0, 128)]), ALU.is_equal, reads=[iotab, RT], writes=[OI])
                    I(DVE, nc.vector.tensor_tensor, OJ[:], bc(iotab[:], [(0, 64), (1, 128)]),
                      bc(RT[:, 1, t0h:t0h + 64], [(1, 64), (0, 128)]), ALU.is_equal, reads=[iotab, RT], writes=[OJ])
                    I(POOL, nc.gpsimd.tensor_tensor, OJg[:], OJ[:], bc(gtb[:, t0h:t0h + 64], [(1, 64), (0, 128)]), ALU.mult,
                      reads=[OJ, gtb], writes=[OJg])
                    return OI, OJg

                def gpart(to, hf, OI, OJg, G, g_b):
                    t0h = hf * 64
                    for t4 in range(16):
                        ps = cps.next()
                        for k in range(4):
                            t = t4 * 4 + k
                            I(PE, nc.tensor.matmul, ps[:, k, :], OI[:, t, :], OJg[:, t, :], start=True, stop=True,
                              reads=[OI, OJg], writes=[ps])
                        gv = G[:]
                        g_out = bass.AP(gv.tensor, gv.offset + t0h + t4 * 4, [list(gv.ap[0]), [128, 128], [1, 4]])
                        ps_jk = ps[:].rearrange("p k j -> p j k")
                        gsl = g_b[hf * 16 + t4]
                        I(ACT, nc.scalar.copy, g_out, ps_jk, reads=[ps], writes=[gsl])
                    if hf == 1:
                        D(SP, T["G_s"][to], G[:].rearrange("p j t -> p (j t)"), G, reads=g_b, writes=[G])

                rload(0); rload(1)
                fr = {0: front(0)}
                gn = {0: gens(0, *fr[0])}
                Gs = {}
                for n in range(2 * NT_OWN):
                    to, hf = n // 2, n % 2
                    if hf == 0:
                        if to + 2 < NT_OWN:
                            rload(to + 2)
                        G = Gr.next()
                        g_b = [Buf("gsl") for _ in range(32)]
                        for gb_ in g_b:
                            gb_.last_w = G.last_w; gb_.readers = list(G.readers)
                        Gs[to] = (G, g_b)
                    if n + 1 < 2 * NT_OWN:
                        to1, hf1 = (n + 1) // 2, (n + 1) % 2
                        if hf1 == 0:
                            fr[to1] = front(to1)
                        gn[n + 1] = gens(hf1, *fr[to1])
                    OI, OJg = gn.pop(n)
                    G, g_b = Gs[to]
                    gpart(to, hf, OI, OJg, G, g_b)
                    if hf == 1:
                        Gs.pop(to); fr.pop(to)
                fw.end_phase()

        if "peer" in phases:
            with ExitStack() as es:
                GP = fw.sbuf(es, "GP", [128, 128, 512], BF16)
                xnT = fw.sbuf(es, "pxnT", [128, 16, 512], BF16)
                dTr = Ring(fw, es, "pdT", 5, [128, 16, 128], BF16)
                actr = Ring(fw, es, "pact", 3, [128, 512], BF16)
                upr = Ring(fw, es, "pup", 4, [128, 4, 512], BF16)
                x1r = Ring(fw, es, "px1", 2, [128, 512], F32)
                outr = Ring(fw, es, "pout", 2, [128, 512], F32)
                bank = Ring(fw, es, "pb", 8, [128, 512], F32, psum=True)
                gp_b = [Buf(f"gp{i}") for i in range(32)]
                fw.phase_bufs.extend(gp_b)
                NP = NTOK // 512

                def gload(ps_, jg, eng):
                    for k in range(4):
                        gsrc = T["G_s"][ps_ * 4 + k].rearrange("p (j t) -> p j t", j=128)
                        D(eng, GP[:, jg * 4:(jg + 1) * 4, k * 128:(k + 1) * 128], gsrc[:, jg * 4:(jg + 1) * 4, :], gp_b[jg],
                          writes=[gp_b[jg]])

                D(SP, xnT[:], T["xn2T_s"][:, :, 0:512], xnT, writes=[xnT])
                for jg in range(32):
                    gload(0, jg, SP if jg % 2 == 0 else POOL)
                for ps_ in range(NP):
                    for j in range(128):
                        dT = dTr.next()
                        D(SP, dT[:].rearrange("p a b -> p (a b)"), T["dT_s"][j], dT, writes=[dT])
                        hp = bank.next()
                        for c in range(16):
                            I(PE, nc.tensor.matmul, hp[:], dT[:, c, :], xnT[:, c, :], start=(c == 0), stop=(c == 15),
                              reads=[dT, xnT], writes=[hp])
                        a = actr.next()
                        I(ACT, nc.scalar.activation, a[:], hp[:], AF.Gelu_apprx_tanh, reads=[hp], writes=[a])
                        I(DVE, nc.vector.tensor_tensor, GP[:, j, :], GP[:, j, :], a[:], ALU.mult, reads=[a, gp_b[j // 4]],
                          writes=[gp_b[j // 4]])
                    if ps_ + 1 < NP:
                        D(SP, xnT[:], T["xn2T_s"][:, :, (ps_ + 1) * 512:(ps_ + 2) * 512], xnT, writes=[xnT])
                    for q in range(4):
                        accs = [bank.next() for _ in range(4)]
                        for jg in range(32):
                            ut = upr.next()
                            D(SP, ut[:], T["up_s"][jg * 4:(jg + 1) * 4, :, q * 512:(q + 1) * 512].rearrange("j p d -> p j d"),
                              ut, writes=[ut])
                            for jj in range(4):
                                j = jg * 4 + jj
                                for k in range(4):
                                    I(PE, nc.tensor.matmul, accs[k][:], GP[:, j, k * 128:(k + 1) * 128], ut[:, jj, :],
                                      start=(j == 0), stop=(j == 127), reads=[gp_b[jg], ut], writes=[accs[k]])
                            if q == 3 and ps_ + 1 < NP:
                                gload(ps_ + 1, jg, POOL)
                        for k in range(4):
                            r0 = ps_ * 512 + k * 128
                            x1 = x1r.next(); o = outr.next()
                            D(SP, x1[:], T["x1_s"][r0:r0 + 128, q * 512:(q + 1) * 512], x1, writes=[x1])
                            I(DVE, nc.vector.tensor_tensor, o[:], accs[k][:], x1[:], ALU.add, reads=[accs[k], x1], writes=[o])
                            D(ACT, T["out"][r0:r0 + 128, q * 512:(q + 1) * 512], o[:], o, reads=[o])
                fw.end_phase()
        fw.barrier()
    return nc, fw


_CACHE = {}


def make_in_maps(inp, ncores=8, phases=PHASES):
    f = lambda a: np.ascontiguousarray(np.asarray(a, dtype=np.float32))
    x = f(inp["x"])
    cst = host_consts()
    shared = {
        "cst": cst,
        "g1": f(inp["norm1_g"][0].reshape(16, 128).T),
        "g2": f(inp["norm2_g"][0].reshape(16, 128).T),
        "w_in": f(inp["w_in"][0]),
        "up_f": f(inp["gla_up_f"][0]), "up_b": f(inp["gla_up_b"][0]),
        "bias_f": f(inp["gla_bias_f"][0].reshape(1, 256)), "bias_b": f(inp["gla_bias_b"][0].reshape(1, 256)),
        "out_g": f(inp["gla_out_g"][0].reshape(1, 512)),
        "gq": f(inp["q_norm_g"][0].reshape(1, 128)), "gk": f(inp["k_norm_g"][0].reshape(1, 128)),
        "w_out": f(inp["w_out"][0]), "wq": f(inp["peer_w_query"][0]),
        "subk": f(inp["peer_sub_keys"][0].reshape(16, 128, 128)),
    }
    if "tables" in phases:
        shared["down"] = f(inp["peer_down"][0])
        shared["up"] = f(inp["peer_up"][0])
    maps = []
    for c in range(ncores):
        b, p = c // 4, c % 4
        xe = np.zeros((NEXT, D_MODEL), np.float32)
        lo = p * NTOK - HALO
        hi = lo + NEXT
        slo, shi = max(lo, 0), min(hi, SEQ)
        xe[slo - lo:shi - lo] = x[b, slo:shi]
        m = dict(shared)
        m["xe"] = xe
        m["aux"] = host_aux(p)
        maps.append(m)
    return maps


def kernel(**inputs):
    if "nc" not in _CACHE:
        _CACHE["nc"] = build_program()[0]
    nc = _CACHE["nc"]
    maps = make_in_maps(inputs)
    res = run_bass_kernel_spmd(nc, maps, core_ids=list(range(8)))
    out = np.zeros((2, SEQ, D_MODEL), np.float32)
    for c in range(8):
        b, p = c // 4, c % 4
        out[b, p * NTOK:(p + 1) * NTOK] = np.asarray(res.results[c]["out"], dtype=np.float32)
    return out
```
